# Optimizing a Trainium2 kernel written in Bass

```python
import jax
import jax.numpy as jnp
from jax import lax
import numpy as np


D_MODEL = 2048
BATCH = 4
SEQ = 4096
DEPTH = 2

CHUNK = 64
D_MIX = D_MODEL
D_MLSTM = D_MIX // 2
MLSTM_HEADS = 4
MLSTM_HEAD_DIM = D_MLSTM // MLSTM_HEADS
MLSTM_CONV = 4
D_ATTN = D_MIX - D_MLSTM
ATTN_HEADS = 8
ATTN_HEAD_DIM = D_ATTN // ATTN_HEADS
LEFT_CHUNKS = 8
BAND = (LEFT_CHUNKS + 1) * CHUNK
MAX_REL = 256
CONV_WIDTH = 31
D_FF = 4 * D_MODEL
N_EVEN = (DEPTH + 1) // 2
N_ODD = DEPTH // 2
EPS = 1e-6
D_IN_PROJ = 4 * D_MLSTM + 2 * MLSTM_HEADS + 3 * D_ATTN
SPLIT_POINTS = (2 * D_MLSTM, 3 * D_MLSTM, 4 * D_MLSTM, 4 * D_MLSTM + MLSTM_HEADS,
                4 * D_MLSTM + 2 * MLSTM_HEADS, 4 * D_MLSTM + 2 * MLSTM_HEADS + D_ATTN,
                4 * D_MLSTM + 2 * MLSTM_HEADS + 2 * D_ATTN)

kernel_name = 'hybrid_mlstm_chunkattn_conformer'


def rmsnorm(x, g):
    xf = x.astype(jnp.float32)
    y = xf * lax.rsqrt(jnp.mean(xf * xf, axis=-1, keepdims=True) + EPS)
    return (y * g.astype(jnp.float32)).astype(x.dtype)


def layernorm(x, g, b):
    xf = x.astype(jnp.float32)
    mu = jnp.mean(xf, axis=-1, keepdims=True)
    var = jnp.mean(jnp.square(xf - mu), axis=-1, keepdims=True)
    y = (xf - mu) * lax.rsqrt(var + EPS)
    return (y * g.astype(jnp.float32) + b.astype(jnp.float32)).astype(x.dtype)


def causal_dwconv(x, w, b):
    K, C = w.shape
    y = lax.conv_general_dilated(x, w[:, None, :].astype(x.dtype), window_strides=(1,),
                                 padding=[(K - 1, 0)],
                                 dimension_numbers=('NWC', 'WIO', 'NWC'),
                                 feature_group_count=C)
    return y + b.astype(x.dtype)


def mlstm_chunkwise(q, k, v, i_pre, f_pre):
    B_, S, H, Dk = q.shape
    Dv = v.shape[-1]
    NC = S // CHUNK
    f32 = jnp.float32

    def chunks(t):
        t = t.astype(f32).reshape((B_, NC, CHUNK, H) + t.shape[3:])
        return jnp.moveaxis(t, (1, 3), (0, 2))

    qc = chunks(q)
    kc = chunks(k) * (Dk ** -0.5)
    vc = chunks(v)
    ic = chunks(i_pre)
    lfc = chunks(jax.nn.log_sigmoid(f_pre.astype(f32)))
    tri = jnp.tril(jnp.ones((CHUNK, CHUNK), dtype=bool))

    def step(carry, xs):
        C, n, m = carry
        qt, kt, vt, it, lft = xs
        b = jnp.cumsum(lft, axis=-1)
        g = b[..., -1]
        dlog = jnp.where(tri, b[..., :, None] - b[..., None, :] + it[..., None, :], -jnp.inf)
        inter_log = b + m[..., None]
        m_t = jnp.maximum(inter_log, jnp.max(dlog, axis=-1))
        w_intra = jnp.exp(dlog - m_t[..., None])
        w_inter = jnp.exp(inter_log - m_t)
        s = jnp.einsum('bhtk,bhsk->bhts', qt, kt) * w_intra
        num = (jnp.einsum('bhts,bhsv->bhtv', s, vt)
               + w_inter[..., None] * jnp.einsum('bhtk,bhvk->bhtv', qt, C))
        den = jnp.sum(s, axis=-1) + w_inter * jnp.einsum('bhtk,bhk->bht', qt, n)
        h = num / jnp.maximum(jnp.abs(den), jnp.exp(-m_t))[..., None]
        a = g[..., None] - b + it
        m_new = jnp.maximum(g + m, jnp.max(a, axis=-1))
        decay = jnp.exp(g + m - m_new)
        w_state = jnp.exp(a - m_new[..., None])
        C_new = decay[..., None, None] * C + jnp.einsum('bhsv,bhsk->bhvk', vt * w_state[..., None], kt)
        n_new = decay[..., None] * n + jnp.einsum('bhs,bhsk->bhk', w_state, kt)
        return (C_new, n_new, m_new), h

    init = (jnp.zeros((B_, H, Dv, Dk), f32), jnp.zeros((B_, H, Dk), f32), jnp.zeros((B_, H), f32))
    _, hs = lax.scan(step, init, (qc, kc, vc, ic, lfc))
    return jnp.moveaxis(hs, (0, 2), (1, 3)).reshape(B_, S, H, Dv)


def chunked_rel_attention(q, k, v, rel_bias):
    B_, S, H, Dh = q.shape
    NC = S // CHUNK
    band = jnp.arange(NC)[:, None] + jnp.arange(LEFT_CHUNKS + 1)[None, :]

    def band_gather(t):
        t = t.reshape(B_, NC, CHUNK, H, Dh)
        t = jnp.pad(t, ((0, 0), (LEFT_CHUNKS, 0), (0, 0), (0, 0), (0, 0)))
        return t[:, band].reshape(B_, NC, BAND, H, Dh)

    qc = q.reshape(B_, NC, CHUNK, H, Dh)
    kb = band_gather(k)
    vb = band_gather(v)
    scores = jnp.einsum('bnqhd,bnkhd->bhnqk', qc, kb).astype(jnp.float32) * (Dh ** -0.5)
    q_off = jnp.arange(CHUNK)
    k_off = ((jnp.arange(LEFT_CHUNKS + 1) - LEFT_CHUNKS)[:, None] * CHUNK
             + jnp.arange(CHUNK)[None, :]).reshape(-1)
    dist = jnp.clip(q_off[:, None] - k_off[None, :], -MAX_REL, MAX_REL) + MAX_REL
    bias = rel_bias.astype(jnp.float32)[:, dist]
    key_valid = jnp.repeat(band >= LEFT_CHUNKS, CHUNK, axis=1)
    scores = jnp.where(key_valid[None, None, :, None, :], scores + bias[:, None], -jnp.inf)
    p = jax.nn.softmax(scores, axis=-1).astype(v.dtype)
    out = jnp.einsum('bhnqk,bnkhd->bnqhd', p, vb)
    return out.reshape(B_, S, H * Dh)


def mlstm_attn_mixer(u, w_in, qk_conv_w, qk_conv_b, igate_b, fgate_b, mlstm_norm_g, rel_bias, w_out):
    B_, S, _ = u.shape
    proj = u @ w_in
    qk_a, v_a, o_a, i_a, f_a, q_b, k_b, v_b = jnp.split(proj, SPLIT_POINTS, axis=-1)
    qk_a = jax.nn.silu(causal_dwconv(qk_a, qk_conv_w, qk_conv_b))
    q_a, k_a = jnp.split(qk_a, 2, axis=-1)
    ha = lambda t: t.reshape(B_, S, MLSTM_HEADS, MLSTM_HEAD_DIM)
    h_a = mlstm_chunkwise(ha(q_a), ha(k_a), ha(v_a),
                          i_a.astype(jnp.float32) + igate_b.astype(jnp.float32),
                          f_a.astype(jnp.float32) + fgate_b.astype(jnp.float32))
    h_a = h_a * lax.rsqrt(jnp.mean(h_a * h_a, axis=-1, keepdims=True) + EPS)
    h_a = h_a * mlstm_norm_g.astype(jnp.float32).reshape(MLSTM_HEADS, MLSTM_HEAD_DIM)
    h_a = (jax.nn.sigmoid(o_a.astype(jnp.float32)) * h_a.reshape(B_, S, D_MLSTM)).astype(u.dtype)
    hb = lambda t: t.reshape(B_, S, ATTN_HEADS, ATTN_HEAD_DIM)
    h_b = chunked_rel_attention(hb(q_b), hb(k_b), hb(v_b), rel_bias)
    return jnp.concatenate([h_a, h_b], axis=-1) @ w_out


def conformer_conv(u, pw1_w, pw1_b, dw_w, dw_b, ln_g, ln_b, pw2_w, pw2_b):
    a, g = jnp.split(u @ pw1_w + pw1_b, 2, axis=-1)
    z = a * jax.nn.sigmoid(g)
    z = causal_dwconv(z, dw_w, dw_b)
    z = jax.nn.silu(layernorm(z, ln_g, ln_b))
    return z @ pw2_w + pw2_b


def sq_relu_mlp(u, w1, w2):
    return jnp.square(jax.nn.relu(u @ w1)) @ w2


def setup_inputs(seed: int = 0) -> dict:
    key = jax.random.key(seed)
    ks = jax.random.split(key, 24)
    D = D_MODEL

    def nrm(k, shape, scale):
        return jax.random.normal(k, shape, jnp.float32) * scale

    return {
        'x': nrm(ks[0], (BATCH, SEQ, D), 1.0),
        'mixer_norm_g': 1.0 + nrm(ks[1], (DEPTH, D), 0.02),
        'mix_w_in': nrm(ks[2], (N_EVEN, D, D_IN_PROJ), D ** -0.5),
        'qk_conv_w': nrm(ks[3], (N_EVEN, MLSTM_CONV, 2 * D_MLSTM), MLSTM_CONV ** -0.5),
        'qk_conv_b': nrm(ks[4], (N_EVEN, 2 * D_MLSTM), 0.02),
        'igate_b': nrm(ks[5], (N_EVEN, MLSTM_HEADS), 0.1),
        'fgate_b': jnp.linspace(3.0, 6.0, MLSTM_HEADS, dtype=jnp.float32)[None, :]
                   + nrm(ks[6], (N_EVEN, MLSTM_HEADS), 0.1),
        'mlstm_norm_g': 1.0 + nrm(ks[7], (N_EVEN, D_MLSTM), 0.02),
        'rel_bias': nrm(ks[8], (N_EVEN, ATTN_HEADS, 2 * MAX_REL + 1), 0.1),
        'mix_w_out': nrm(ks[9], (N_EVEN, D_MIX, D), D_MIX ** -0.5),
        'conv_pw1_w': nrm(ks[10], (N_ODD, D, 2 * D), D ** -0.5),
        'conv_pw1_b': nrm(ks[11], (N_ODD, 2 * D), 0.02),
        'conv_dw_w': nrm(ks[12], (N_ODD, CONV_WIDTH, D), CONV_WIDTH ** -0.5),
        'conv_dw_b': nrm(ks[13], (N_ODD, D), 0.02),
        'conv_ln_g': 1.0 + nrm(ks[14], (N_ODD, D), 0.02),
        'conv_ln_b': nrm(ks[15], (N_ODD, D), 0.02),
        'conv_pw2_w': nrm(ks[16], (N_ODD, D, D), D ** -0.5),
        'conv_pw2_b': nrm(ks[17], (N_ODD, D), 0.02),
        'mlp_norm_g': 1.0 + nrm(ks[18], (DEPTH, D), 0.02),
        'mlp_w1': nrm(ks[19], (DEPTH, D, D_FF), D ** -0.5),
        'mlp_w2': nrm(ks[20], (DEPTH, D_FF, D), D_FF ** -0.5),
        'final_norm_g': 1.0 + nrm(ks[21], (D,), 0.02),
    }


def reference(x, mixer_norm_g, mix_w_in, qk_conv_w, qk_conv_b, igate_b, fgate_b, mlstm_norm_g,
              rel_bias, mix_w_out, conv_pw1_w, conv_pw1_b, conv_dw_w, conv_dw_b, conv_ln_g,
              conv_ln_b, conv_pw2_w, conv_pw2_b, mlp_norm_g, mlp_w1, mlp_w2, final_norm_g):
    h = x
    for layer in range(DEPTH):
        u = rmsnorm(h, mixer_norm_g[layer])
        if layer % 2 == 0:
            e = layer // 2
            h = h + mlstm_attn_mixer(u, mix_w_in[e], qk_conv_w[e], qk_conv_b[e], igate_b[e],
                                     fgate_b[e], mlstm_norm_g[e], rel_bias[e], mix_w_out[e])
        else:
            o = layer // 2
            h = h + conformer_conv(u, conv_pw1_w[o], conv_pw1_b[o], conv_dw_w[o], conv_dw_b[o],
                                   conv_ln_g[o], conv_ln_b[o], conv_pw2_w[o], conv_pw2_b[o])
        u = rmsnorm(h, mlp_norm_g[layer])
        h = h + sq_relu_mlp(u, mlp_w1[layer], mlp_w2[layer])
    return rmsnorm(h, final_norm_g)
```

```python
import numpy as np
from contextlib import ExitStack
import concourse.bass as bass
import concourse.mybir as mybir
from concourse.bass_utils import run_bass_kernel_spmd

F32 = mybir.dt.float32
BF16 = mybir.dt.bfloat16
AF = mybir.ActivationFunctionType
ALU = mybir.AluOpType
AX = mybir.AxisListType


class Res:
    __slots__ = ("name", "w", "r", "dsem")

    def __init__(self, name):
        self.name = name
        self.w = None
        self.r = {}
        self.dsem = None


class Buf(Res):
    __slots__ = ("ap", "parts", "nbytes")

    def __init__(self, name, ap):
        super().__init__(name)
        self.ap = ap
        self.parts = {}

    def part(self, key):
        p = self.parts.get(key)
        if p is None:
            p = Res("%s/%s" % (self.name, key))
            self.parts[key] = p
        return p


class Sched:
    ENG = ("pe", "act", "dve", "pool", "sp")

    def __init__(self, nc, es):
        self.nc = nc
        self.es = es
        self.eng = {"pe": nc.tensor, "act": nc.scalar, "dve": nc.vector, "pool": nc.gpsimd, "sp": nc.sync}
        self.sem = {}
        self.cnt = {}
        for e in ("pe", "act", "dve", "pool"):
            self.sem[e] = es.enter_context(nc.semaphore("s_" + e))
            self.cnt[e] = 0
        self.known = {e: {} for e in self.ENG}
        self.dsems = {}
        self.finals = []
        self.ninst = 0
        self._rot = {}

    def sb(self, name, shape, dtype):
        t = self.es.enter_context(self.nc.sbuf_tensor(name, list(shape), dtype))
        return Buf(name, t)

    def psum(self, name, shape, dtype=F32):
        t = self.es.enter_context(self.nc.psum_tensor(name, list(shape), dtype))
        return Buf(name, t)

    def pool_of(self, name, bufs):
        self._rot[name] = [bufs, 0]

    def rot(self, name):
        p = self._rot[name]
        b = p[0][p[1] % len(p[0])]
        p[1] += 1
        return b

    def _dsem(self, res):
        if res.dsem is None:
            nm = "d%d" % len(self.dsems)
            s = self.es.enter_context(self.nc.semaphore(nm))
            res.dsem = [nm, s, 0]
            self.dsems[nm] = res.dsem
        return res.dsem

    def _semof(self, key):
        return self.sem[key] if key in self.sem else self.dsems[key][1]

    def _waits(self, e, reads, writes):
        need = {}
        for r in reads:
            if r.w is not None:
                k, v = r.w
                if need.get(k, 0) < v:
                    need[k] = v
        for w in writes:
            if w.w is not None:
                k, v = w.w
                if need.get(k, 0) < v:
                    need[k] = v
            for k, v in w.r.items():
                if need.get(k, 0) < v:
                    need[k] = v
        kn = self.known[e]
        eng = self.eng[e]
        for k, v in need.items():
            if kn.get(k, 0) < v:
                eng.wait_ge(self._semof(k), v)
                kn[k] = v
                self.ninst += 1

    def _mark(self, key, val, reads, writes):
        for r in reads:
            if r.r.get(key, 0) < val:
                r.r[key] = val
        for w in writes:
            w.w = (key, val)
            w.r = {}

    def op(self, e, fn, reads=(), writes=()):
        self._waits(e, reads, writes)
        ins = fn(self.eng[e])
        self.cnt[e] += 1
        ins.then_inc(self.sem[e], 1)
        self._mark(e, self.cnt[e], reads, writes)
        self.ninst += 1
        return ins

    def pe_group(self, fns, reads=(), writes=()):
        self._waits("pe", reads, writes)
        ins = None
        for fn in fns:
            ins = fn(self.eng["pe"])
            self.ninst += 1
        self.cnt["pe"] += 1
        ins.then_inc(self.sem["pe"], 1)
        self._mark("pe", self.cnt["pe"], reads, writes)

    def mm(self, out, pairs, reads=(), out_ap=None):
        oap = out.ap[:] if out_ap is None else out_ap
        n = len(pairs)
        fns = []
        for i, (l, r) in enumerate(pairs):
            fns.append(lambda pe, l=l, r=r, i=i: pe.matmul(oap, l, r, start=(i == 0), stop=(i == n - 1)))
        self.pe_group(fns, reads=reads, writes=[out])

    def dma(self, q, out_ap, in_ap, reads=(), writes=(), final=False, semres=None):
        self._waits(q, reads, writes)
        sr = semres if semres is not None else (writes[0] if writes else reads[0])
        ds = self._dsem(sr)
        self.eng[q].dma_start(out=out_ap, in_=in_ap).then_inc(ds[1], 16)
        ds[2] += 16
        self._mark(ds[0], ds[2], reads, writes)
        self.ninst += 1
        if final:
            self.finals.append((ds[0], ds[2]))

    def finish(self):
        kn = self.known["sp"]
        for k, v in self.finals:
            if kn.get(k, 0) < v:
                self.eng["sp"].wait_ge(self._semof(k), v)
                kn[k] = v


    def barrier(self, engs=("pe", "act", "dve", "sp")):
        for e in engs:
            kn = self.known[e]
            for k in ("pe", "act", "dve", "pool"):
                v = self.cnt[k]
                if k != e and v > 0 and kn.get(k, 0) < v:
                    self.eng[e].wait_ge(self.sem[k], v)
                    kn[k] = v
            for nm, ds in self.dsems.items():
                if ds[2] > 0 and kn.get(nm, 0) < ds[2]:
                    self.eng[e].wait_ge(ds[1], ds[2])
                    kn[nm] = ds[2]


D = 2048
KC = 16
DFF = 8192
T = 1152
NT = 9
VS = 4096
DIN = 7176
EPS = 1e-6
NEG = -30000.0

VO = {}
_o = 0
for _n, _w in (("g_mix0", 16), ("g_mix1", 16), ("g_mlp0", 16), ("g_mlp1", 16), ("g_fin", 16),
               ("qk_cb", 16), ("qk_cw", 64), ("ml_g", 8), ("pw1_b", 32), ("dw_w", 496),
               ("dw_b", 16), ("ln_g", 16), ("ln_b", 16), ("pw2_b", 16), ("gate_b", 8),
               ("flag", 1), ("maskc", 1), ("eps", 1), ("one", 1), ("nl16", 1), ("zero", 1)):
    VO[_n] = _o
    _o += _w
NV = _o


def _fm(v):
    v = np.asarray(v, np.float32)
    return np.ascontiguousarray(v.reshape(-1, 128).T)


def run_interleaved(gens, depth):
    active = []
    it = iter(gens)
    while True:
        while len(active) < depth:
            g = next(it, None)
            if g is None:
                break
            active.append(g)
        if not active:
            break
        for g in list(active):
            try:
                next(g)
            except StopIteration:
                active.remove(g)


def build_program(dbg=False):
    nc = bass.Bass("TRN2", target_bir_lowering=False)

    def din(name, shape):
        return nc.dram_tensor(name, list(shape), F32, kind="ExternalInput").ap()

    x_d = din("x_ext", [VS, D])
    vec_d = din("vecs", [128, NV])
    cm_d = din("cmat", [128, 4, 128])
    b2_d = din("bias2", [8, 128, 640])
    win_d = din("w_in", [D, DIN])
    wout_d = din("w_out", [D, D])
    pw1_d = din("pw1", [D, 2 * D])
    pw2_d = din("pw2", [D, D])
    w1_d = din("w1", [2, D, DFF])
    w2_d = din("w2", [2, DFF, D])
    out_d = nc.dram_tensor("out", [2048, D], F32, kind="ExternalOutput").ap()
    dbg_d = None
    if dbg:
        dbg_d = nc.dram_tensor("dbg", [2, 128, 16, T], F32, kind="ExternalOutput").ap()

    winv = win_d.rearrange("(k p) n -> p k n", p=128)
    woutv = wout_d.rearrange("(k p) n -> p k n", p=128)
    pw1v = pw1_d.rearrange("(k p) n -> p k n", p=128)
    pw2v = pw2_d.rearrange("(k p) n -> p k n", p=128)
    w1v = [w1_d[l].rearrange("(k p) n -> p k n", p=128) for l in range(2)]
    w2v = [w2_d[l].rearrange("(k p) n -> p k n", p=128) for l in range(2)]

    es = ExitStack()
    with es:
        S = Sched(nc, es)
        vec = S.sb("vec", [128, NV], F32)
        ident_f = S.sb("ident_f", [128, 128], F32)
        ones_f = S.sb("ones_f", [128, 128], F32)
        U_f = S.sb("U_f", [128, 128], F32)
        mask16 = S.sb("mask16", [128, 128], F32)
        ident_b = S.sb("ident_b", [128, 128], BF16)
        ones_b = S.sb("ones_b", [128, 128], BF16)
        KVk = S.sb("KVk", [128, 8, 512], BF16)
        KVv = S.sb("KVv", [128, 8, 4, 128], BF16)
        Tst = S.sb("Tst", [128, 4, 2, 257], F32)
        Cbf = S.sb("Cbf", [128, 4, 2, 258], BF16)
        egp = S.sb("egp", [128, 4], F32)
        prehalo = S.sb("prehalo", [128, 16, 4], BF16)
        st1 = S.sb("st1", [128, 256], F32)
        st2 = S.sb("st2", [128, 256], F32)
        sm = S.sb("sm", [128, 8], F32)
        sm2 = [S.sb("sm2_%d" % i, [128, 4], F32) for i in range(3)]
        NSLOT = 2
        wslots = [S.sb("wslot%d" % i, [128, 4096], BF16) for i in range(NSLOT)]
        R_h = S.sb("R_h", [128, 16 * T], F32)
        R_u = S.sb("R_u", [128, 8 * T], F32)
        R_m = S.sb("R_m", [128, 8 * T], F32)
        R_s = S.sb("R_s", [128, 2048], F32)
        big = [S.psum("pbig%d" % i, [128, 512]) for i in range(3)]
        wide = S.psum("pwide", [128, 1024])
        misc = [S.psum("pmisc%d" % i, [128, 512]) for i in range(3)]
        S.pool_of("big", big)
        S.pool_of("misc", misc)
        S.pool_of("sm2", sm2)

        def V(c):
            return vec.ap[:, c:c + 1]

        def view(reg, off, shape, dt, name):
            nel = 1
            for d_ in shape[1:]:
                nel *= d_
            if dt == BF16:
                assert off % 4 == 0
                words = (nel + 1) // 2
                a = reg.ap[:, off // 4: off // 4 + words].bitcast(BF16)[:, 0:nel]
                nb = words * 4
            else:
                a = reg.ap[:, off // 4: off // 4 + nel]
                nb = nel * 4
            if len(shape) == 3:
                a = a.rearrange("p (a b) -> p a b", a=shape[1])
            elif len(shape) == 4:
                a = a.rearrange("p (a b c) -> p a b c", a=shape[1], b=shape[2])
            b = Buf(name, a)
            b.nbytes = nb
            return b

        class Arena:
            def __init__(self, reg, size):
                self.reg, self.size, self.off = reg, size, 0

            def alloc(self, shape, dt, name):
                b = view(self.reg, self.off, shape, dt, name)
                self.off += (b.nbytes + 31) // 32 * 32
                assert self.off <= self.size, (name, self.off, self.size)
                return b

        hT = view(R_h, 0, [128, 16, T], F32, "hT")
        uT = view(R_u, 0, [128, 16, T], BF16, "uT")
        mixT = view(R_m, 0, [128, 16, T], BF16, "mixT")
        sq = view(R_s, 0, [128, 16, 256], BF16, "sq")
        rl = [view(R_s, 0, [128, 512], F32, "rl0"), view(R_s, 2048, [128, 512], F32, "rl1")]
        dg31 = view(R_s, 0, [128, 31, 128], BF16, "dg31")
        S.pool_of("rl", rl)

        def hp(k):
            return hT.part(k)

        HP = [hT.part(k) for k in range(16)]
        UP = [uT.part(k) for k in range(16)]
        MP = [mixT.part(k) for k in range(16)]

        S.dma("sp", vec.ap[:], vec_d[:, :], writes=[vec])
        S.dma("sp", ident_f.ap[:], cm_d[:, 0, :], writes=[ident_f])
        S.dma("sp", ones_f.ap[:], cm_d[:, 1, :], writes=[ones_f])
        S.dma("sp", U_f.ap[:], cm_d[:, 2, :], writes=[U_f])
        S.dma("sp", mask16.ap[:], cm_d[:, 3, :], writes=[mask16])
        S.dma("pool", ident_b.ap[:], cm_d[:, 0, :], writes=[ident_b])
        S.dma("pool", ones_b.ap[:], cm_d[:, 1, :], writes=[ones_b])
        S.op("dve", lambda e: e.memset(Tst.ap[:], 0.0), writes=[Tst])
        S.op("dve", lambda e: e.memset(Cbf.ap[:], 0.0), writes=[Cbf])
        S.op("dve", lambda e: e.memset(egp.ap[:], 1.0), writes=[egp])
        S.op("dve", lambda e: e.memset(prehalo.ap[:], 0.0), writes=[prehalo])
        S.op("dve", lambda e: e.memset(KVk.ap[:], 0.0), writes=[KVk])
        S.op("dve", lambda e: e.memset(KVv.ap[:], 0.0), writes=[KVv])

        class WStream:
            def __init__(self):
                self.plan = []
                self.issued = 0
                self.taken = 0

            def add(self, tag, ap, kc, cols):
                self.plan.append((tag, ap, kc, cols))

            def _issue(self):
                tag, ap, kc, cols = self.plan[self.issued]
                slot = wslots[self.issued % NSLOT]
                v = slot.ap[:, 0:kc * cols].rearrange("p (k c) -> p k c", k=kc)
                S.dma("pool", v, ap, writes=[slot])
                self.issued += 1

            def next(self, tag):
                while self.issued <= self.taken:
                    self._issue()
                ptag, ap, kc, cols = self.plan[self.taken]
                assert ptag == tag, (ptag, tag)
                slot = wslots[self.taken % NSLOT]
                v = slot.ap[:, 0:kc * cols].rearrange("p (k c) -> p k c", k=kc)
                self.taken += 1
                return slot, v

            def prefetch(self):
                while self.issued < min(len(self.plan), self.taken + NSLOT):
                    self._issue()

        W = WStream()

        def plan_tile(full, attn):
            W.add("gates", winv[:, :, 4096:4104], 16, 8)
            for hd in range(4):
                if full:
                    W.add("mq%d" % hd, winv[:, :, hd * 256:(hd + 1) * 256], 16, 256)
                W.add("mk%d" % hd, winv[:, :, 1024 + hd * 256:1024 + (hd + 1) * 256], 16, 256)
                W.add("mv%d" % hd, winv[:, :, 2048 + hd * 256:2048 + (hd + 1) * 256], 16, 256)
                if full:
                    W.add("mo%d" % hd, winv[:, :, 3072 + hd * 256:3072 + (hd + 1) * 256], 16, 256)
            if attn:
                for pr in range(4):
                    if full:
                        W.add("aq%d" % pr, winv[:, :, 4104 + pr * 256:4104 + (pr + 1) * 256], 16, 256)
                    W.add("ak%d" % pr, winv[:, :, 5128 + pr * 256:5128 + (pr + 1) * 256], 16, 256)
                    W.add("av%d" % pr, winv[:, :, 6152 + pr * 256:6152 + (pr + 1) * 256], 16, 256)
            if full and dbg != 2:
                for s_ in range(8):
                    W.add("wo%d" % s_, woutv[:, :, s_ * 256:(s_ + 1) * 256], 16, 256)
                plan_mlp(0)
                for s_ in range(8):
                    W.add("p1a%d" % s_, pw1v[:, :, s_ * 256:(s_ + 1) * 256], 16, 256)
                    W.add("p1g%d" % s_, pw1v[:, :, 2048 + s_ * 256:2048 + (s_ + 1) * 256], 16, 256)
                for s_ in range(8):
                    W.add("p2%d" % s_, pw2v[:, :, s_ * 256:(s_ + 1) * 256], 16, 256)
                plan_mlp(1)

        def plan_mlp(l):
            for g in range(8):
                for s4 in range(4):
                    c = g * 1024 + s4 * 256
                    W.add("w1_%d_%d_%d" % (l, g, s4), w1v[l][:, :, c:c + 256], 16, 256)
                for s4 in range(4):
                    W.add("w2_%d_%d_%d" % (l, g, s4), w2v[l][:, g * 8:(g + 1) * 8, s4 * 512:(s4 + 1) * 512], 8, 512)

        def ACT(out, in_, func, reads, writes, bias=None, scale=None, accum=None):
            kw = {}
            if bias is not None:
                kw["bias"] = bias
            if scale is not None:
                kw["scale"] = scale
            if accum is not None:
                kw["accum_out"] = accum
            S.op("act", lambda e: e.activation(out=out, in_=in_, func=func, **kw), reads, writes)

        def TT(out, in0, in1, op, reads, writes, eng="dve"):
            S.op(eng, lambda e: e.tensor_tensor(out=out, in0=in0, in1=in1, op=op), reads, writes)

        def TS(out, in0, s1, op0, reads, writes, s2=None, op1=None, eng="dve"):
            if op1 is None:
                S.op(eng, lambda e: e.tensor_scalar(out=out, in0=in0, scalar1=s1, scalar2=None, op0=op0), reads, writes)
            else:
                S.op(eng, lambda e: e.tensor_scalar(out=out, in0=in0, scalar1=s1, scalar2=s2, op0=op0, op1=op1), reads, writes)

        def STT(out, in0, scalar, in1, op0, op1, reads, writes):
            S.op("dve", lambda e: e.scalar_tensor_tensor(out=out, in0=in0, scalar=scalar, in1=in1, op0=op0, op1=op1), reads, writes)

        def RECIP(out, in_, reads, writes):
            S.op("dve", lambda e: e.reciprocal(out=out, in_=in_), reads, writes)

        def TRANS(ps, items, reads, f32=False):
            pv = ps.ap[:, 0:512] if f32 else ps.ap[:, 0:512].bitcast(BF16)
            idn = ident_f if f32 else ident_b
            fns = []
            for (c0, a, n) in items:
                fns.append(lambda pe, c0=c0, a=a, n=n: pe.transpose(pv[:, c0:c0 + n], a, idn.ap[:]))
            S.pe_group(fns, reads=list(reads) + [idn], writes=[ps])
            return pv

        def subs_from(c_lo):
            if c_lo == 0:
                return [(0, 512), (512, 512), (1024, 128)]
            return [(128, 512), (640, 512)]

        def norm_fm(src, sres, c0, w, gofs, dst, dres, dc0):
            ACT(sq.ap[:, :, 0:w], src[:, :, c0:c0 + w], AF.Square, sres, [sq])
            ps = S.rot("misc")
            S.mm(ps, [(ones_b.ap[:], sq.ap[:, k, 0:w]) for k in range(16)], reads=[sq, ones_b], out_ap=ps.ap[:, 0:w])
            ACT(st2.ap[:, 0:w], ps.ap[:, 0:w], AF.Sqrt, [ps, vec], [st2], bias=V(VO["eps"]), scale=1.0 / D)
            RECIP(st2.ap[:, 0:w], st2.ap[:, 0:w], [st2], [st2])
            for k in range(16):
                STT(dst[:, k, dc0:dc0 + w], src[:, k, c0:c0 + w], V(gofs + k), st2.ap[:, 0:w], ALU.mult, ALU.mult,
                    [sres[k] if len(sres) == 16 else sres[0], st2, vec], [dres[k] if len(dres) == 16 else dres[0]])

        def proj_fm(slot, sv, kc, nm, src, sreads, subs, evac, k0=0):
            for mi in range(nm):
                for (c0, w) in subs:
                    ps = S.rot("big")
                    S.mm(ps, [(sv[:, k, mi * 128:(mi + 1) * 128], src[:, k0 + k, c0:c0 + w]) for k in range(kc)],
                         reads=[slot] + list(sreads), out_ap=ps.ap[:, 0:w])
                    evac(mi, c0, w, ps)
            W.prefetch()

        def proj_tm(slot, sv, cols, ntt, evac):
            for tt in range(ntt):
                ps = S.rot("big")
                S.mm(ps, [(uT.ap[:, k, tt * 128:(tt + 1) * 128], sv[:, k, 0:cols]) for k in range(16)],
                     reads=[slot] + UP, out_ap=ps.ap[:, 0:cols])
                evac(tt, ps)
            W.prefetch()

        def load_xT(tok0, tt, dst, dres, dc0, xs_pool):
            xs = S.rot(xs_pool)
            S.dma("sp", xs.ap[:], x_d[tok0 + tt * 128: tok0 + (tt + 1) * 128, :], writes=[xs])
            for q4 in range(4):
                ps = S.rot("misc")
                TRANS(ps, [(j * 128, xs.ap[:, (q4 * 4 + j) * 128:(q4 * 4 + j + 1) * 128], 128) for j in range(4)], [xs], f32=True)
                o = dst[:, q4 * 4:(q4 + 1) * 4, dc0:dc0 + 128]
                i_ = ps.ap[:, 0:512].rearrange("p (a b) -> p a b", a=4)
                if q4 % 2 == 0:
                    ACT(o, i_, AF.Copy, [ps], dres[q4 * 4:(q4 + 1) * 4] if len(dres) == 16 else dres)
                else:
                    S.op("dve", lambda e, o=o, i_=i_: e.tensor_copy(out=o, in_=i_), [ps], dres[q4 * 4:(q4 + 1) * 4] if len(dres) == 16 else dres)

        def tile(tok0, ntt, full, attn, snap_tt, upd_last, flag_tts, is_A, out0, dbg_i=None):
            Tn = ntt * 128
            subs = [(c0, min(512, Tn - c0)) for c0 in range(0, Tn, 512)]
            S.barrier()
            ar = Arena(R_h, 16 * T * 4)
            xs_b = [ar.alloc([128, 2048], F32, "xs%d" % i) for i in range(2)]
            xT_b = [ar.alloc([128, 16, 128], F32, "xT%d" % i) for i in range(2)]
            S.pool_of("xs", xs_b)
            for tt in range(ntt):
                xT = xT_b[tt % 2]
                load_xT(tok0, tt, xT.ap, [xT], 0, "xs")
                norm_fm(xT.ap, [xT], 0, 128, VO["g_mix0"], uT.ap, UP, tt * 128)
            S.barrier()
            ar = Arena(R_h, 16 * T * 4)
            LF = ar.alloc([128, NT, 4], F32, "LF")
            Acol = ar.alloc([128, NT, 4], F32, "Acol")
            EMB = ar.alloc([128, NT, 4], F32, "EMB")
            EG = ar.alloc([128, NT, 4], F32, "EG")
            EG16 = ar.alloc([128, NT, 4], F32, "EG16")
            gt = ar.alloc([128, 16], F32, "gt")
            slot, sv = W.next("gates")
            for tt in range(ntt):
                ps = S.rot("misc")
                S.mm(ps, [(uT.ap[:, k, tt * 128:(tt + 1) * 128], sv[:, k, 0:8]) for k in range(16)],
                     reads=[slot] + UP, out_ap=ps.ap[:, 0:8])
                TT(gt.ap[:, 0:8], ps.ap[:, 0:8], vec.ap[:, VO["gate_b"]:VO["gate_b"] + 8], ALU.add, [ps, vec], [gt])
                ACT(gt.ap[:, 8:12], gt.ap[:, 4:8], AF.Exp, [gt], [gt], scale=-1.0)
                ACT(gt.ap[:, 8:12], gt.ap[:, 8:12], AF.Ln, [gt, vec], [gt], bias=V(VO["one"]), scale=1.0)
                TS(LF.ap[:, tt, :], gt.ap[:, 8:12], -1.0, ALU.mult, [gt], [LF])
                ps2 = S.rot("misc")
                S.pe_group([
                    lambda pe, ps2=ps2, tt=tt: pe.matmul(ps2.ap[:, 0:4], U_f.ap[:], LF.ap[:, tt, :], start=True, stop=True),
                    lambda pe, ps2=ps2, tt=tt: pe.matmul(ps2.ap[:, 4:8], ones_f.ap[:], LF.ap[:, tt, :], start=True, stop=True),
                ], reads=[U_f, ones_f, LF], writes=[ps2])
                TT(gt.ap[:, 12:16], gt.ap[:, 0:4], ps2.ap[:, 0:4], ALU.subtract, [gt, ps2], [gt])
                ACT(Acol.ap[:, tt, :], gt.ap[:, 12:16], AF.Exp, [gt], [Acol])
                if tt in flag_tts:
                    TS(Acol.ap[:, tt, :], Acol.ap[:, tt, :], V(VO["flag"]), ALU.mult, [Acol, vec], [Acol])
                ACT(EMB.ap[:, tt, :], ps2.ap[:, 0:4], AF.Exp, [ps2], [EMB], scale=-1.0)
                ACT(EG.ap[:, tt, :], ps2.ap[:, 4:8], AF.Exp, [ps2], [EG])
                ACT(EG16.ap[:, tt, :], ps2.ap[:, 4:8], AF.Exp, [ps2, vec], [EG16], bias=V(VO["nl16"]), scale=1.0)
            W.prefetch()
            ar_base = ar.off

            if True:
                ar.off = ar_base
                preq = ar.alloc([128, 2, T + 4], BF16, "preq")
                prek = ar.alloc([128, 2, T + 4], BF16, "prek")
                qT = ar.alloc([128, 2, T], BF16, "qT")
                kT = ar.alloc([128, 2, T], BF16, "kT")
                kTM = ar.alloc([128, NT, 256], BF16, "kTM")
                va = ar.alloc([128, NT, 258], BF16, "va")
                sigo = ar.alloc([128, NT, 256], BF16, "sigo")
                hn = [ar.alloc([128, 256], BF16, "hn%d" % i) for i in range(2)]
                PTb = [ar.alloc([128, 128], BF16, "PT%d" % i) for i in range(2)]
                dg = [ar.alloc([128, 4, 128], BF16, "dg%d" % i) for i in range(2)]
                junk = ar.alloc([128, 256], BF16, "junk")
            for hd in range(4):

                def conv_silu(pre, fbase, dst, dgi):
                    for mi in range(2):
                        f = fbase + mi
                        d_ = dg[(dgi + mi) % 2]
                        for j in range(4):
                            TS(d_.ap[:, j, :], ident_b.ap[:], V(VO["qk_cw"] + f * 4 + j), ALU.mult, [ident_b, vec], [d_])
                        for (c0, w) in subs:
                            ps = S.rot("big")
                            S.mm(ps, [(d_.ap[:, j, :], pre.ap[:, mi, c0 + j + 1:c0 + j + 1 + w]) for j in range(4)],
                                 reads=[d_, pre], out_ap=ps.ap[:, 0:w])
                            ACT(dst.ap[:, mi, c0:c0 + w], ps.ap[:, 0:w], AF.Silu, [ps, vec], [dst], bias=V(VO["qk_cb"] + f), scale=1.0)

                def proj_pre(tag, pre, fbase):
                    slot, sv = W.next(tag)
                    S.op("dve", lambda e: e.tensor_copy(out=pre.ap[:, :, 0:4], in_=prehalo.ap[:, fbase:fbase + 2, :]), [prehalo], [pre])

                    def ev(mi, c0, w, ps):
                        ACT(pre.ap[:, mi, 4 + c0:4 + c0 + w], ps.ap[:, 0:w], AF.Copy, [ps], [pre])
                    proj_fm(slot, sv, 16, 2, uT.ap, UP, subs, ev)
                    if snap_tt is not None:
                        st_ = (snap_tt + 1) * 128
                        S.op("dve", lambda e: e.tensor_copy(out=prehalo.ap[:, fbase:fbase + 2, :], in_=pre.ap[:, :, st_:st_ + 4]), [pre], [prehalo])

                if full:
                    proj_pre("mq%d" % hd, preq, hd * 2)
                    conv_silu(preq, hd * 2, qT, 0)
                proj_pre("mk%d" % hd, prek, 8 + hd * 2)
                conv_silu(prek, 8 + hd * 2, kT, 0)
                for tt in range(ntt):
                    ps = S.rot("misc")
                    pv = TRANS(ps, [(kt * 128, kT.ap[:, kt, tt * 128:(tt + 1) * 128], 128) for kt in range(2)], [kT])
                    S.op("dve", lambda e, pv=pv, tt=tt: e.tensor_copy(out=kTM.ap[:, tt, :], in_=pv[:, 0:256]), [ps], [kTM])
                slot, sv = W.next("mv%d" % hd)

                def ev_v(tt, ps):
                    ACT(va.ap[:, tt, 0:256], ps.ap[:, 0:256], AF.Copy, [ps, Acol], [va], scale=Acol.ap[:, tt, hd:hd + 1])
                    S.op("dve", lambda e: e.tensor_copy(out=va.ap[:, tt, 256:257], in_=Acol.ap[:, tt, hd:hd + 1]), [Acol], [va])
                proj_tm(slot, sv, 256, ntt, ev_v)
                if full:
                    slot, sv = W.next("mo%d" % hd)

                    def ev_o(tt, ps):
                        ACT(sigo.ap[:, tt, :], ps.ap[:, 0:256], AF.Sigmoid, [ps], [sigo])
                    proj_tm(slot, sv, 256, ntt, ev_o)
                egprev = egp.ap[:, hd:hd + 1]
                egres = egp
                for tt in range(ntt):
                    tsl = slice(tt * 128, (tt + 1) * 128)
                    if full:
                        ps = S.rot("misc")
                        S.mm(ps, [(kT.ap[:, kt, tsl], qT.ap[:, kt, tsl]) for kt in range(2)], reads=[kT, qT], out_ap=ps.ap[:, 0:128])
                        PT = PTb[tt % 2]
                        TT(PT.ap[:], ps.ap[:, 0:128], mask16.ap[:], ALU.mult, [ps, mask16], [PT])
                        psx = S.rot("misc")
                        S.mm(psx, [(PT.ap[:], va.ap[:, tt, 0:257])] + [(qT.ap[:, kt, tsl], Cbf.ap[:, hd, kt, 0:257]) for kt in range(2)],
                             reads=[PT, va, qT, Cbf], out_ap=psx.ap[:, 0:257])
                        ACT(sm.ap[:, 0:1], psx.ap[:, 256:257], AF.Abs, [psx], [sm])
                        TT(sm.ap[:, 0:1], sm.ap[:, 0:1], EMB.ap[:, tt, hd:hd + 1], ALU.max, [sm, EMB], [sm])
                        RECIP(sm.ap[:, 1:2], sm.ap[:, 0:1], [sm], [sm])
                        ACT(junk.ap[:], psx.ap[:, 0:256], AF.Square, [psx, sm], [junk, sm], scale=sm.ap[:, 1:2], accum=sm.ap[:, 2:3])
                        ACT(sm.ap[:, 3:4], sm.ap[:, 2:3], AF.Sqrt, [sm, vec], [sm], bias=V(VO["eps"]), scale=1.0 / 256)
                        RECIP(sm.ap[:, 4:5], sm.ap[:, 3:4], [sm], [sm])
                        TT(sm.ap[:, 5:6], sm.ap[:, 4:5], sm.ap[:, 1:2], ALU.mult, [sm], [sm])
                        h_ = hn[tt % 2]
                        STT(h_.ap[:], psx.ap[:, 0:256], sm.ap[:, 5:6], sigo.ap[:, tt, :], ALU.mult, ALU.mult, [psx, sm, sigo], [h_])
                        ps3 = S.rot("misc")
                        pv = TRANS(ps3, [(vt * 128, h_.ap[:, vt * 128:(vt + 1) * 128], 128) for vt in range(2)], [h_])
                        for vt in range(2):
                            ACT(mixT.ap[:, hd * 2 + vt, tsl], pv[:, vt * 128:(vt + 1) * 128], AF.Copy, [ps3, vec], [MP[hd * 2 + vt]],
                                scale=V(VO["ml_g"] + hd * 2 + vt))
                    if tt <= upd_last:
                        S.pe_group([
                            lambda pe, kt=kt, tt=tt: pe.matmul(wide.ap[:, kt * 512:kt * 512 + 257], kTM.ap[:, tt, kt * 128:(kt + 1) * 128],
                                                               va.ap[:, tt, 0:257], start=True, stop=True)
                            for kt in range(2)], reads=[kTM, va], writes=[wide])
                        for kt in range(2):
                            STT(Tst.ap[:, hd, kt, :], Tst.ap[:, hd, kt, :], egprev, wide.ap[:, kt * 512:kt * 512 + 257],
                                ALU.mult, ALU.add, [Tst, wide, egres], [Tst])
                        for kt in range(2):
                            ACT(Cbf.ap[:, hd, kt, 0:257], Tst.ap[:, hd, kt, :], AF.Copy, [Tst, EG16], [Cbf], scale=EG16.ap[:, tt, hd:hd + 1])
                        egprev = EG.ap[:, tt, hd:hd + 1]
                        egres = EG
                if snap_tt is not None:
                    S.op("dve", lambda e: e.tensor_copy(out=egp.ap[:, hd:hd + 1], in_=EG.ap[:, snap_tt, hd:hd + 1]), [EG], [egp])

            S.barrier()
            if attn:
                ar.off = ar_base
                qTb = [ar.alloc([128, T], BF16, "qTb%d" % i) for i in range(2)]
                kbuf = [ar.alloc([128, 512 + T], BF16, "kbuf%d" % i) for i in range(2)]
                vbuf = [ar.alloc([128, 4 + NT, 128], BF16, "vbuf%d" % i) for i in range(2)]
                b2 = [ar.alloc([128, 640], F32, "b2_%d" % i) for i in range(2)]
                sc = [ar.alloc([128, 640], F32, "sc%d" % i) for i in range(2)]
                pex = [ar.alloc([128, 640], F32, "pex%d" % i) for i in range(2)]
                pn = [ar.alloc([128, 640], BF16, "pn%d" % i) for i in range(2)]
                pTt = [ar.alloc([128, 640], BF16, "pT%d" % i) for i in range(2)]
                asubs = subs if full else [s_ for s_ in subs if s_[0] >= 512]
                att0 = 0 if full else 4
                for pr in range(4):
                    for hh in range(2):
                        h = pr * 2 + hh
                        S.op("dve", lambda e, hh=hh, h=h: e.tensor_copy(out=kbuf[hh].ap[:, 0:512], in_=KVk.ap[:, h, :]), [KVk], [kbuf[hh]])
                        S.op("dve", lambda e, hh=hh, h=h: e.tensor_copy(out=vbuf[hh].ap[:, 0:4, :], in_=KVv.ap[:, h, :, :]), [KVv], [vbuf[hh]])
                        if full:
                            S.dma("sp", b2[hh].ap[:], b2_d[h], writes=[b2[hh]])
                    if full:
                        slot, sv = W.next("aq%d" % pr)

                        def ev_q(mi, c0, w, ps):
                            ACT(qTb[mi].ap[:, c0:c0 + w], ps.ap[:, 0:w], AF.Copy, [ps], [qTb[mi]], scale=float(128 ** -0.5))
                        proj_fm(slot, sv, 16, 2, uT.ap, UP, subs, ev_q)
                    slot, sv = W.next("ak%d" % pr)

                    def ev_k(mi, c0, w, ps):
                        S.op("dve", lambda e: e.tensor_copy(out=kbuf[mi].ap[:, 512 + c0:512 + c0 + w], in_=ps.ap[:, 0:w]), [ps], [kbuf[mi]])
                    proj_fm(slot, sv, 16, 2, uT.ap, UP, asubs, ev_k)
                    slot, sv = W.next("av%d" % pr)
                    for tt in range(att0, ntt):
                        ps = S.rot("big")
                        S.mm(ps, [(uT.ap[:, k, tt * 128:(tt + 1) * 128], sv[:, k, 0:256]) for k in range(16)],
                             reads=[slot] + UP, out_ap=ps.ap[:, 0:256])
                        for hh in range(2):
                            ACT(vbuf[hh].ap[:, 4 + tt, :], ps.ap[:, hh * 128:(hh + 1) * 128], AF.Copy, [ps], [vbuf[hh]])
                    W.prefetch()
                    if full:
                        def unit(u, hh):
                            h = pr * 2 + hh
                            i2 = (u * 2 + hh) % 2
                            q_ = qTb[hh].ap[:, u * 128:(u + 1) * 128]
                            S.pe_group([
                                lambda pe: pe.matmul(wide.ap[:, 0:512], q_, kbuf[hh].ap[:, u * 128:u * 128 + 512], start=True, stop=True),
                                lambda pe: pe.matmul(wide.ap[:, 512:640], q_, kbuf[hh].ap[:, u * 128 + 512:u * 128 + 640], start=True, stop=True),
                            ], reads=[qTb[hh], kbuf[hh]], writes=[wide])
                            s_ = sc[i2]
                            TT(s_.ap[:], wide.ap[:, 0:640], b2[hh].ap[:], ALU.add, [wide, b2[hh]], [s_])
                            if is_A and u <= 4:
                                nm_ = 640 - u * 128
                                TS(s_.ap[:, 0:nm_], s_.ap[:, 0:nm_], V(VO["maskc"]), ALU.add, [s_, vec], [s_])
                            yield
                            m_ = S.rot("sm2")
                            S.op("dve", lambda e: e.tensor_reduce(out=m_.ap[:, 0:1], in_=s_.ap[:], axis=AX.X, op=ALU.max, negate=True), [s_], [m_])
                            p_ = pex[i2]
                            ACT(p_.ap[:], s_.ap[:], AF.Exp, [s_, m_], [p_, m_], bias=m_.ap[:, 0:1], scale=1.0, accum=m_.ap[:, 1:2])
                            yield
                            RECIP(m_.ap[:, 2:3], m_.ap[:, 1:2], [m_], [m_])
                            n_ = pn[i2]
                            TS(n_.ap[:], p_.ap[:], m_.ap[:, 2:3], ALU.mult, [p_, m_], [n_])
                            yield
                            psT = S.rot("misc")
                            pv = TRANS(psT, [(j * 128, n_.ap[:, j * 128:(j + 1) * 128], 128) for j in range(5)], [n_])
                            t_ = pTt[i2]
                            ACT(t_.ap[:], pv[:, 0:640], AF.Copy, [psT], [t_])
                            yield
                            pso = S.rot("misc")
                            S.mm(pso, [(vbuf[hh].ap[:, u + j, :], t_.ap[:, j * 128:(j + 1) * 128]) for j in range(5)],
                                 reads=[vbuf[hh], t_], out_ap=pso.ap[:, 0:128])
                            S.op("dve", lambda e: e.tensor_copy(out=mixT.ap[:, 8 + h, u * 128:(u + 1) * 128], in_=pso.ap[:, 0:128]), [pso], [MP[8 + h]])
                            yield
                        run_interleaved([unit(u, hh) for u in range(ntt) for hh in range(2)], 2)
                    if snap_tt is not None:
                        sk = (snap_tt + 1) * 128
                        for hh in range(2):
                            h = pr * 2 + hh
                            S.op("dve", lambda e, hh=hh, h=h: e.tensor_copy(out=KVk.ap[:, h, :], in_=kbuf[hh].ap[:, sk:sk + 512]), [kbuf[hh]], [KVk])
                            S.op("dve", lambda e, hh=hh, h=h: e.tensor_copy(out=KVv.ap[:, h, :, :], in_=vbuf[hh].ap[:, snap_tt + 1:snap_tt + 5, :]), [vbuf[hh]], [KVv])
            if not full:
                return
            S.barrier()
            if dbg == 2:
                S.dma("pool", dbg_d[dbg_i], mixT.ap[:], reads=MP, final=True, semres=mixT)
                return
            aru = Arena(R_u, 8 * T * 4)
            xs2 = [aru.alloc([128, 2048], F32, "xsb%d" % i) for i in range(2)]
            S.pool_of("xs2", xs2)
            for tt in range(ntt):
                load_xT(tok0, tt, hT.ap, HP, tt * 128, "xs2")
            for s_ in range(8):
                slot, sv = W.next("wo%d" % s_)

                def ev_o2(mi, c0, w, ps, s_=s_):
                    d_ = s_ * 2 + mi
                    TT(hT.ap[:, d_, c0:c0 + w], ps.ap[:, 0:w], hT.ap[:, d_, c0:c0 + w], ALU.add, [ps, HP[d_]], [HP[d_]])
                proj_fm(slot, sv, 16, 2, mixT.ap, MP, subs, ev_o2)
            S.barrier()
            if dbg_i is not None and dbg_d is not None:
                S.dma("sp", dbg_d[dbg_i], hT.ap[:], reads=HP, final=True, semres=hT)
            mlp(0, 0)
            for c0 in range(0, T, 256):
                w = min(256, T - c0)
                norm_fm(hT.ap, HP, c0, w, VO["g_mix1"], uT.ap, UP, c0)
            S.barrier()
            zT = mixT
            for s_ in range(8):
                slotA, svA = W.next("p1a%d" % s_)
                slotG, svG = W.next("p1g%d" % s_)
                for mi in range(2):
                    f = s_ * 2 + mi
                    for (c0, w) in subs:
                        psA = S.rot("big")
                        S.mm(psA, [(svA[:, k, mi * 128:(mi + 1) * 128], uT.ap[:, k, c0:c0 + w]) for k in range(16)], reads=[slotA] + UP, out_ap=psA.ap[:, 0:w])
                        psG = S.rot("big")
                        S.mm(psG, [(svG[:, k, mi * 128:(mi + 1) * 128], uT.ap[:, k, c0:c0 + w]) for k in range(16)], reads=[slotG] + UP, out_ap=psG.ap[:, 0:w])
                        sg = S.rot("rl")
                        ACT(sg.ap[:, 0:w], psG.ap[:, 0:w], AF.Sigmoid, [psG, vec], [sg], bias=V(VO["pw1_b"] + 16 + f), scale=1.0)
                        STT(zT.ap[:, f, c0:c0 + w], psA.ap[:, 0:w], V(VO["pw1_b"] + f), sg.ap[:, 0:w], ALU.add, ALU.mult, [psA, sg, vec], [MP[f]])
                W.prefetch()
            if is_A:
                TS(zT.ap[:, :, 96:128], zT.ap[:, :, 96:128], V(VO["flag"]), ALU.mult, MP + [vec], MP)
            S.barrier()
            yT = uT
            csubs = subs_from(128)
            for f in range(16):
                for j in range(31):
                    TS(dg31.ap[:, j, :], ident_b.ap[:], V(VO["dw_w"] + f * 31 + j), ALU.mult, [ident_b, vec], [dg31],
                       eng="dve")
                for (c0, w) in csubs:
                    ps = S.rot("big")
                    S.mm(ps, [(dg31.ap[:, j, :], zT.ap[:, f, c0 - 30 + j:c0 - 30 + j + w]) for j in range(31)], reads=[dg31, MP[f]], out_ap=ps.ap[:, 0:w])
                    ACT(yT.ap[:, f, c0:c0 + w], ps.ap[:, 0:w], AF.Identity, [ps, vec], [UP[f]], bias=V(VO["dw_b"] + f), scale=1.0)
            S.barrier()
            sT = mixT
            for c0 in range(128, T, 256):
                w = 256
                ACT(sq.ap[:, :, 0:w], yT.ap[:, :, c0:c0 + w], AF.Square, UP, [sq])
                psm = S.rot("misc")
                S.mm(psm, [(ones_b.ap[:], yT.ap[:, k, c0:c0 + w]) for k in range(16)], reads=UP + [ones_b], out_ap=psm.ap[:, 0:w])
                psq = S.rot("misc")
                S.mm(psq, [(ones_b.ap[:], sq.ap[:, k, 0:w]) for k in range(16)], reads=[sq, ones_b], out_ap=psq.ap[:, 0:w])
                TS(st1.ap[:, 0:w], psm.ap[:, 0:w], 1.0 / D, ALU.mult, [psm], [st1])
                TT(st2.ap[:, 0:w], st1.ap[:, 0:w], st1.ap[:, 0:w], ALU.mult, [st1], [st2])
                STT(st2.ap[:, 0:w], psq.ap[:, 0:w], 1.0 / D, st2.ap[:, 0:w], ALU.mult, ALU.subtract, [psq, st2], [st2])
                ACT(st2.ap[:, 0:w], st2.ap[:, 0:w], AF.Sqrt, [st2, vec], [st2], bias=V(VO["eps"]), scale=1.0)
                RECIP(st2.ap[:, 0:w], st2.ap[:, 0:w], [st2], [st2])
                for f in range(16):
                    t_ = S.rot("rl")
                    TT(t_.ap[:, 0:w], yT.ap[:, f, c0:c0 + w], st1.ap[:, 0:w], ALU.subtract, [UP[f], st1], [t_])
                    TT(t_.ap[:, 0:w], t_.ap[:, 0:w], st2.ap[:, 0:w], ALU.mult, [t_, st2], [t_])
                    ACT(sT.ap[:, f, c0:c0 + w], t_.ap[:, 0:w], AF.Silu, [t_, vec], [MP[f]], bias=V(VO["ln_b"] + f), scale=V(VO["ln_g"] + f))
            S.barrier()
            for s_ in range(8):
                slot, sv = W.next("p2%d" % s_)

                def ev_p2(mi, c0, w, ps, s_=s_):
                    d_ = s_ * 2 + mi
                    STT(hT.ap[:, d_, c0:c0 + w], ps.ap[:, 0:w], V(VO["pw2_b"] + d_), hT.ap[:, d_, c0:c0 + w], ALU.add, ALU.add, [ps, HP[d_], vec], [HP[d_]])
                proj_fm(slot, sv, 16, 2, sT.ap, MP, csubs, ev_p2)
            S.barrier()
            mlp(1, 128)
            S.barrier()
            aru = Arena(R_u, 8 * T * 4)
            oT = [aru.alloc([128, 16, 128], F32, "oT%d" % i) for i in range(2)]
            ob = [aru.alloc([128, 2048], F32, "ob%d" % i) for i in range(2)]
            for tt in range(1, ntt):
                o_ = oT[tt % 2]
                norm_fm(hT.ap, HP, tt * 128, 128, VO["g_fin"], o_.ap, [o_], 0)
                b_ = ob[tt % 2]
                for q4 in range(4):
                    ps = S.rot("misc")
                    TRANS(ps, [(j * 128, o_.ap[:, q4 * 4 + j, :], 128) for j in range(4)], [o_], f32=True)
                    if q4 % 2 == 0:
                        ACT(b_.ap[:, q4 * 512:(q4 + 1) * 512], ps.ap[:, 0:512], AF.Copy, [ps], [b_])
                    else:
                        S.op("dve", lambda e, b_=b_, ps=ps, q4=q4: e.tensor_copy(out=b_.ap[:, q4 * 512:(q4 + 1) * 512], in_=ps.ap[:, 0:512]), [ps], [b_])
                r0 = out0 + (tt - 1) * 128
                S.dma("sp", out_d[r0:r0 + 128, :], b_.ap[:], reads=[b_], final=True)

        def mlp(l, c_lo):
            msubs = subs_from(c_lo)
            gofs = VO["g_mlp0"] if l == 0 else VO["g_mlp1"]
            for c0 in range(c_lo, T, 256):
                norm_fm(hT.ap, HP, c0, min(256, T - c0), gofs, uT.ap, UP, c0)
            S.barrier()
            hid = [view(R_m, 0, [128, 8, T], BF16, "hid0"), view(R_m, 8 * T * 2, [128, 8, T], BF16, "hid1")]
            for g in range(8):
                hb = hid[g % 2]
                for s4 in range(4):
                    slot, sv = W.next("w1_%d_%d_%d" % (l, g, s4))

                    def ev1(mi, c0, w, ps, s4=s4, hb=hb):
                        r_ = S.rot("rl")
                        ACT(r_.ap[:, 0:w], ps.ap[:, 0:w], AF.Relu, [ps], [r_])
                        TT(hb.ap[:, s4 * 2 + mi, c0:c0 + w], ps.ap[:, 0:w], r_.ap[:, 0:w], ALU.mult, [ps, r_], [hb])
                    proj_fm(slot, sv, 16, 2, uT.ap, UP, msubs, ev1)
                for s4 in range(4):
                    slot, sv = W.next("w2_%d_%d_%d" % (l, g, s4))

                    def ev2(mi, c0, w, ps, s4=s4):
                        d_ = s4 * 4 + mi
                        TT(hT.ap[:, d_, c0:c0 + w], ps.ap[:, 0:w], hT.ap[:, d_, c0:c0 + w], ALU.add, [ps, HP[d_]], [HP[d_]])
                    proj_fm(slot, sv, 8, 4, hb.ap, [hb], msubs, ev2)

        plan_tile(False, False)
        plan_tile(False, True)
        plan_tile(True, True)
        if dbg != 2:
            plan_tile(True, True)
        tile(0, 6, False, False, 5, 5, set(range(6)), False, None)
        tile(768, 9, False, True, 8, 8, set(range(9)), False, None)
        tile(1920, 9, True, True, 7, 7, {0}, True, 0, dbg_i=0)
        if dbg != 2:
            tile(2944, 9, True, True, None, 7, set(), False, 1024, dbg_i=1)
        S.barrier(("sp",))
        S.finish()
        assert W.taken == len(W.plan), (W.taken, len(W.plan))
    return nc


def _host_consts():
    cm = np.zeros((128, 4, 128), np.float32)
    cm[:, 0, :] = np.eye(128, dtype=np.float32)
    cm[:, 1, :] = 1.0
    u = np.triu(np.ones((128, 128), np.float32))
    cm[:, 2, :] = u
    cm[:, 3, :] = u * (1.0 / 16.0)
    return cm


def _bias2(rel_bias):
    qi = np.arange(128)
    kj = np.arange(640)
    qc = qi // 64
    kc = kj // 64
    qpos = qi
    kpos = (kc - 8) * 64 + (kj % 64)
    dist = np.clip(qpos[:, None] - kpos[None, :], -256, 256) + 256
    vis = (kc[None, :] >= qc[:, None]) & (kc[None, :] <= qc[:, None] + 8)
    out = np.empty((8, 128, 640), np.float32)
    for h in range(8):
        out[h] = np.where(vis, rel_bias[h][dist], np.float32(NEG))
    return out


_NC_CACHE = {}


def kernel(**inputs):
    dbg = inputs.pop("_dbg", False)
    x = np.asarray(inputs["x"], np.float32)
    f = lambda n: np.asarray(inputs[n], np.float32)
    vecs = np.zeros((128, NV), np.float32)

    def put(name, arr):
        arr = np.asarray(arr, np.float32)
        vecs[:, VO[name]:VO[name] + arr.shape[1]] = arr

    put("g_mix0", _fm(f("mixer_norm_g")[0]))
    put("g_mix1", _fm(f("mixer_norm_g")[1]))
    put("g_mlp0", _fm(f("mlp_norm_g")[0]))
    put("g_mlp1", _fm(f("mlp_norm_g")[1]))
    put("g_fin", _fm(f("final_norm_g")))
    put("qk_cb", _fm(f("qk_conv_b")[0]))
    cw = f("qk_conv_w")[0]
    put("qk_cw", np.ascontiguousarray(cw.T.reshape(16, 128, 4).transpose(1, 0, 2)).reshape(128, 64))
    put("ml_g", _fm(f("mlstm_norm_g")[0]))
    put("pw1_b", _fm(f("conv_pw1_b")[0]))
    dw = f("conv_dw_w")[0]
    put("dw_w", np.ascontiguousarray(dw.T.reshape(16, 128, 31).transpose(1, 0, 2)).reshape(128, 496))
    put("dw_b", _fm(f("conv_dw_b")[0]))
    put("ln_g", _fm(f("conv_ln_g")[0]))
    put("ln_b", _fm(f("conv_ln_b")[0]))
    put("pw2_b", _fm(f("conv_pw2_b")[0]))
    gb = np.concatenate([f("igate_b")[0], f("fgate_b")[0]])[None, :]
    put("gate_b", np.broadcast_to(gb, (128, 8)))
    vecs[:, VO["eps"]] = EPS
    vecs[:, VO["one"]] = 1.0
    vecs[:, VO["nl16"]] = -np.log(16.0)
    vecs[:, VO["zero"]] = 0.0
    cm = _host_consts()
    b2 = _bias2(f("rel_bias")[0])
    w_in = np.ascontiguousarray(f("mix_w_in")[0])
    w_out = np.ascontiguousarray(f("mix_w_out")[0])
    pw1 = np.ascontiguousarray(f("conv_pw1_w")[0])
    pw2 = np.ascontiguousarray(f("conv_pw2_w")[0])
    w1 = np.ascontiguousarray(f("mlp_w1"))
    w2 = np.ascontiguousarray(f("mlp_w2"))
    in_maps = []
    for c in range(8):
        b, half = c // 2, c % 2
        xe = np.zeros((VS, D), np.float32)
        if half == 0:
            xe[2048:] = x[b, :2048]
        else:
            xe[:] = x[b]
        v = vecs.copy()
        v[:, VO["flag"]] = float(half)
        v[:, VO["maskc"]] = 0.0 if half == 1 else NEG
        in_maps.append({"x_ext": xe, "vecs": v, "cmat": cm, "bias2": b2, "w_in": w_in, "w_out": w_out,
                        "pw1": pw1, "pw2": pw2, "w1": w1, "w2": w2})
    key = dbg
    if key not in _NC_CACHE:
        _NC_CACHE[key] = build_program(dbg)
    nc = _NC_CACHE[key]
    res = run_bass_kernel_spmd(nc, in_maps, core_ids=list(range(8)))
    out = np.empty((4, 4096, D), np.float32)
    for c in range(8):
        b, half = c // 2, c % 2
        out[b, half * 2048:(half + 1) * 2048] = res.results[c]["out"]
    if dbg:
        return out, [res.results[c]["dbg"] for c in range(8)]
    return out
```

```python
import numpy as np
from contextlib import ExitStack
import concourse.bass as bass
import concourse.mybir as mybir
from concourse.bass_utils import run_bass_kernel_spmd

F32 = mybir.dt.float32
BF16 = mybir.dt.bfloat16
AF = mybir.ActivationFunctionType
ALU = mybir.AluOpType
AX = mybir.AxisListType


class Res:
    __slots__ = ("name", "w", "r", "dsem")

    def __init__(self, name):
        self.name = name
        self.w = None
        self.r = {}
        self.dsem = None


class Buf(Res):
    __slots__ = ("ap", "parts", "nbytes")

    def __init__(self, name, ap):
        super().__init__(name)
        self.ap = ap
        self.parts = {}

    def part(self, key):
        p = self.parts.get(key)
        if p is None:
            p = Res("%s/%s" % (self.name, key))
            self.parts[key] = p
        return p


class Sched:
    ENG = ("pe", "act", "dve", "pool", "sp")

    def __init__(self, nc, es):
        self.nc = nc
        self.es = es
        self.eng = {"pe": nc.tensor, "act": nc.scalar, "dve": nc.vector, "pool": nc.gpsimd, "sp": nc.sync}
        self.sem = {}
        self.cnt = {}
        for e in ("pe", "act", "dve", "pool"):
            self.sem[e] = es.enter_context(nc.semaphore("s_" + e))
            self.cnt[e] = 0
        self.known = {e: {} for e in self.ENG}
        self.dsems = {}
        self.finals = []
        self.ninst = 0
        self._rot = {}

    def sb(self, name, shape, dtype):
        t = self.es.enter_context(self.nc.sbuf_tensor(name, list(shape), dtype))
        return Buf(name, t)

    def psum(self, name, shape, dtype=F32):
        t = self.es.enter_context(self.nc.psum_tensor(name, list(shape), dtype))
        return Buf(name, t)

    def pool_of(self, name, bufs):
        self._rot[name] = [bufs, 0]

    def rot(self, name):
        p = self._rot[name]
        b = p[0][p[1] % len(p[0])]
        p[1] += 1
        return b

    def _dsem(self, res):
        if res.dsem is None:
            nm = "d%d" % len(self.dsems)
            s = self.es.enter_context(self.nc.semaphore(nm))
            res.dsem = [nm, s, 0]
            self.dsems[nm] = res.dsem
        return res.dsem

    def _semof(self, key):
        return self.sem[key] if key in self.sem else self.dsems[key][1]

    def _waits(self, e, reads, writes):
        need = {}
        for r in reads:
            if r.w is not None:
                k, v = r.w
                if need.get(k, 0) < v:
                    need[k] = v
        for w in writes:
            if w.w is not None:
                k, v = w.w
                if need.get(k, 0) < v:
                    need[k] = v
            for k, v in w.r.items():
                if need.get(k, 0) < v:
                    need[k] = v
        kn = self.known[e]
        eng = self.eng[e]
        for k, v in need.items():
            if kn.get(k, 0) < v:
                eng.wait_ge(self._semof(k), v)
                kn[k] = v
                self.ninst += 1

    def _mark(self, key, val, reads, writes):
        for r in reads:
            if r.r.get(key, 0) < val:
                r.r[key] = val
        for w in writes:
            w.w = (key, val)
            w.r = {}

    def op(self, e, fn, reads=(), writes=()):
        self._waits(e, reads, writes)
        ins = fn(self.eng[e])
        self.cnt[e] += 1
        ins.then_inc(self.sem[e], 1)
        self._mark(e, self.cnt[e], reads, writes)
        self.ninst += 1
        return ins

    def pe_group(self, fns, reads=(), writes=()):
        self._waits("pe", reads, writes)
        ins = None
        for fn in fns:
            ins = fn(self.eng["pe"])
            self.ninst += 1
        self.cnt["pe"] += 1
        ins.then_inc(self.sem["pe"], 1)
        self._mark("pe", self.cnt["pe"], reads, writes)

    def mm(self, out, pairs, reads=(), out_ap=None):
        oap = out.ap[:] if out_ap is None else out_ap
        n = len(pairs)
        fns = []
        for i, (l, r) in enumerate(pairs):
            fns.append(lambda pe, l=l, r=r, i=i: pe.matmul(oap, l, r, start=(i == 0), stop=(i == n - 1)))
        self.pe_group(fns, reads=reads, writes=[out])

    def dma(self, q, out_ap, in_ap, reads=(), writes=(), final=False, semres=None):
        self._waits(q, reads, writes)
        sr = semres if semres is not None else (writes[0] if writes else reads[0])
        ds = self._dsem(sr)
        self.eng[q].dma_start(out=out_ap, in_=in_ap).then_inc(ds[1], 16)
        ds[2] += 16
        self._mark(ds[0], ds[2], reads, writes)
        self.ninst += 1
        if final:
            self.finals.append((ds[0], ds[2]))

    def finish(self):
        kn = self.known["sp"]
        for k, v in self.finals:
            if kn.get(k, 0) < v:
                self.eng["sp"].wait_ge(self._semof(k), v)
                kn[k] = v


    def barrier(self, engs=("pe", "act", "dve", "sp")):
        for e in engs:
            kn = self.known[e]
            for k in ("pe", "act", "dve", "pool"):
                v = self.cnt[k]
                if k != e and v > 0 and kn.get(k, 0) < v:
                    self.eng[e].wait_ge(self.sem[k], v)
                    kn[k] = v
            for nm, ds in self.dsems.items():
                if ds[2] > 0 and kn.get(nm, 0) < ds[2]:
                    self.eng[e].wait_ge(ds[1], ds[2])
                    kn[nm] = ds[2]


D = 2048
KC = 16
DFF = 8192
T = 1152
NT = 9
VS = 4096
DIN = 7176
EPS = 1e-6
NEG = -30000.0

VO = {}
_o = 0
for _n, _w in (("g_mix0", 16), ("g_mix1", 16), ("g_mlp0", 16), ("g_mlp1", 16), ("g_fin", 16),
               ("qk_cb", 16), ("qk_cw", 64), ("ml_g", 8), ("pw1_b", 32), ("dw_w", 496),
               ("dw_b", 16), ("ln_g", 16), ("ln_b", 16), ("pw2_b", 16), ("gate_b", 8),
               ("flag", 1), ("maskc", 1), ("eps", 1), ("one", 1), ("nl16", 1), ("zero", 1)):
    VO[_n] = _o
    _o += _w
NV = _o


def _fm(v):
    v = np.asarray(v, np.float32)
    return np.ascontiguousarray(v.reshape(-1, 128).T)


def run_interleaved(gens, depth):
    active = []
    it = iter(gens)
    while True:
        while len(active) < depth:
            g = next(it, None)
            if g is None:
                break
            active.append(g)
        if not active:
            break
        for g in list(active):
            try:
                next(g)
            except StopIteration:
                active.remove(g)


def build_program(dbg=False):
    nc = bass.Bass("TRN2", target_bir_lowering=False)

    def din(name, shape):
        return nc.dram_tensor(name, list(shape), F32, kind="ExternalInput").ap()

    x_d = din("x_ext", [VS, D])
    vec_d = din("vecs", [128, NV])
    cm_d = din("cmat", [128, 4, 128])
    b2_d = din("bias2", [8, 128, 640])
    win_d = din("w_in", [D, DIN])
    wout_d = din("w_out", [D, D])
    pw1_d = din("pw1", [D, 2 * D])
    pw2_d = din("pw2", [D, D])
    w1_d = din("w1", [2, D, DFF])
    w2_d = din("w2", [2, DFF, D])
    out_d = nc.dram_tensor("out", [2048, D], F32, kind="ExternalOutput").ap()
    dbg_d = None
    if dbg:
        dbg_d = nc.dram_tensor("dbg", [2, 128, 16, T], F32, kind="ExternalOutput").ap()

    winv = win_d.rearrange("(k p) n -> p k n", p=128)
    woutv = wout_d.rearrange("(k p) n -> p k n", p=128)
    pw1v = pw1_d.rearrange("(k p) n -> p k n", p=128)
    pw2v = pw2_d.rearrange("(k p) n -> p k n", p=128)
    w1v = [w1_d[l].rearrange("(k p) n -> p k n", p=128) for l in range(2)]
    w2v = [w2_d[l].rearrange("(k p) n -> p k n", p=128) for l in range(2)]

    es = ExitStack()
    with es:
        S = Sched(nc, es)
        vec = S.sb("vec", [128, NV], F32)
        ident_f = S.sb("ident_f", [128, 128], F32)
        ones_f = S.sb("ones_f", [128, 128], F32)
        U_f = S.sb("U_f", [128, 128], F32)
        mask16 = S.sb("mask16", [128, 128], F32)
        ident_b = S.sb("ident_b", [128, 128], BF16)
        ones_b = S.sb("ones_b", [128, 128], BF16)
        KVk = S.sb("KVk", [128, 8, 512], BF16)
        KVv = S.sb("KVv", [128, 8, 4, 128], BF16)
        Tst = S.sb("Tst", [128, 4, 2, 257], F32)
        Cbf = S.sb("Cbf", [128, 4, 2, 258], BF16)
        egp = S.sb("egp", [128, 4], F32)
        prehalo = S.sb("prehalo", [128, 16, 4], BF16)
        st1 = S.sb("st1", [128, 256], F32)
        st2 = S.sb("st2", [128, 256], F32)
        sm = S.sb("sm", [128, 8], F32)
        sm2 = [S.sb("sm2_%d" % i, [128, 4], F32) for i in range(3)]
        NSLOT = 2
        wslots = [S.sb("wslot%d" % i, [128, 4096], BF16) for i in range(NSLOT)]
        R_h = S.sb("R_h", [128, 16 * T], F32)
        R_u = S.sb("R_u", [128, 8 * T], F32)
        R_m = S.sb("R_m", [128, 8 * T], F32)
        R_s = S.sb("R_s", [128, 2048], F32)
        big = [S.psum("pbig%d" % i, [128, 512]) for i in range(3)]
        wide = S.psum("pwide", [128, 1024])
        misc = [S.psum("pmisc%d" % i, [128, 512]) for i in range(2)]
        pxy = S.psum("pxy", [128, 512])
        S.pool_of("big", big)
        S.pool_of("misc", misc)
        S.pool_of("sm2", sm2)

        def V(c):
            return vec.ap[:, c:c + 1]

        def view(reg, off, shape, dt, name):
            nel = 1
            for d_ in shape[1:]:
                nel *= d_
            if dt == BF16:
                assert off % 4 == 0
                words = (nel + 1) // 2
                a = reg.ap[:, off // 4: off // 4 + words].bitcast(BF16)[:, 0:nel]
                nb = words * 4
            else:
                a = reg.ap[:, off // 4: off // 4 + nel]
                nb = nel * 4
            if len(shape) == 3:
                a = a.rearrange("p (a b) -> p a b", a=shape[1])
            elif len(shape) == 4:
                a = a.rearrange("p (a b c) -> p a b c", a=shape[1], b=shape[2])
            b = Buf(name, a)
            b.nbytes = nb
            return b

        class Arena:
            def __init__(self, reg, size):
                self.reg, self.size, self.off = reg, size, 0

            def alloc(self, shape, dt, name):
                b = view(self.reg, self.off, shape, dt, name)
                self.off += (b.nbytes + 31) // 32 * 32
                assert self.off <= self.size, (name, self.off, self.size)
                return b

        hT = view(R_h, 0, [128, 16, T], F32, "hT")
        uT = view(R_u, 0, [128, 16, T], BF16, "uT")
        mixT = view(R_m, 0, [128, 16, T], BF16, "mixT")
        sq = view(R_s, 0, [128, 16, 256], BF16, "sq")
        rl = [view(R_s, 0, [128, 512], F32, "rl0"), view(R_s, 2048, [128, 512], F32, "rl1")]
        dg31 = view(R_s, 0, [128, 31, 128], BF16, "dg31")
        S.pool_of("rl", rl)

        def hp(k):
            return hT.part(k)

        HP = [hT.part(k) for k in range(16)]
        UP = [uT.part(k) for k in range(16)]
        MP = [mixT.part(k) for k in range(16)]

        S.dma("sp", vec.ap[:], vec_d[:, :], writes=[vec])
        S.dma("sp", ident_f.ap[:], cm_d[:, 0, :], writes=[ident_f])
        S.dma("sp", ones_f.ap[:], cm_d[:, 1, :], writes=[ones_f])
        S.dma("sp", U_f.ap[:], cm_d[:, 2, :], writes=[U_f])
        S.dma("sp", mask16.ap[:], cm_d[:, 3, :], writes=[mask16])
        S.dma("pool", ident_b.ap[:], cm_d[:, 0, :], writes=[ident_b])
        S.dma("pool", ones_b.ap[:], cm_d[:, 1, :], writes=[ones_b])
        S.op("dve", lambda e: e.memset(Tst.ap[:], 0.0), writes=[Tst])
        S.op("dve", lambda e: e.memset(Cbf.ap[:], 0.0), writes=[Cbf])
        S.op("dve", lambda e: e.memset(egp.ap[:], 1.0), writes=[egp])
        S.op("dve", lambda e: e.memset(prehalo.ap[:], 0.0), writes=[prehalo])
        S.op("dve", lambda e: e.memset(KVk.ap[:], 0.0), writes=[KVk])
        S.op("dve", lambda e: e.memset(KVv.ap[:], 0.0), writes=[KVv])

        class WStream:
            def __init__(self):
                self.plan = []
                self.issued = 0
                self.taken = 0

            def add(self, tag, ap, kc, cols):
                self.plan.append((tag, ap, kc, cols))

            def _issue(self):
                tag, ap, kc, cols = self.plan[self.issued]
                slot = wslots[self.issued % NSLOT]
                v = slot.ap[:, 0:kc * cols].rearrange("p (k c) -> p k c", k=kc)
                S.dma("pool", v, ap, writes=[slot])
                self.issued += 1

            def next(self, tag):
                while self.issued <= self.taken:
                    self._issue()
                ptag, ap, kc, cols = self.plan[self.taken]
                assert ptag == tag, (ptag, tag)
                slot = wslots[self.taken % NSLOT]
                v = slot.ap[:, 0:kc * cols].rearrange("p (k c) -> p k c", k=kc)
                self.taken += 1
                return slot, v

            def prefetch(self):
                while self.issued < min(len(self.plan), self.taken + NSLOT):
                    self._issue()

        W = WStream()

        def plan_tile(full, attn):
            W.add("gates", winv[:, :, 4096:4104], 16, 8)
            for hd in range(4):
                if full:
                    W.add("mq%d" % hd, winv[:, :, hd * 256:(hd + 1) * 256], 16, 256)
                W.add("mk%d" % hd, winv[:, :, 1024 + hd * 256:1024 + (hd + 1) * 256], 16, 256)
                W.add("mv%d" % hd, winv[:, :, 2048 + hd * 256:2048 + (hd + 1) * 256], 16, 256)
                if full:
                    W.add("mo%d" % hd, winv[:, :, 3072 + hd * 256:3072 + (hd + 1) * 256], 16, 256)
            if attn:
                for pr in range(4):
                    if full:
                        W.add("aq%d" % pr, winv[:, :, 4104 + pr * 256:4104 + (pr + 1) * 256], 16, 256)
                    W.add("ak%d" % pr, winv[:, :, 5128 + pr * 256:5128 + (pr + 1) * 256], 16, 256)
                    W.add("av%d" % pr, winv[:, :, 6152 + pr * 256:6152 + (pr + 1) * 256], 16, 256)
            if full and dbg != 2:
                for s_ in range(8):
                    W.add("wo%d" % s_, woutv[:, :, s_ * 256:(s_ + 1) * 256], 16, 256)
                plan_mlp(0)
                for s_ in range(8):
                    W.add("p1a%d" % s_, pw1v[:, :, s_ * 256:(s_ + 1) * 256], 16, 256)
                    W.add("p1g%d" % s_, pw1v[:, :, 2048 + s_ * 256:2048 + (s_ + 1) * 256], 16, 256)
                for s_ in range(8):
                    W.add("p2%d" % s_, pw2v[:, :, s_ * 256:(s_ + 1) * 256], 16, 256)
                plan_mlp(1)

        def plan_mlp(l):
            for g in range(8):
                for s4 in range(4):
                    c = g * 1024 + s4 * 256
                    W.add("w1_%d_%d_%d" % (l, g, s4), w1v[l][:, :, c:c + 256], 16, 256)
                for s4 in range(4):
                    W.add("w2_%d_%d_%d" % (l, g, s4), w2v[l][:, g * 8:(g + 1) * 8, s4 * 512:(s4 + 1) * 512], 8, 512)

        def ACT(out, in_, func, reads, writes, bias=None, scale=None, accum=None):
            kw = {}
            if bias is not None:
                kw["bias"] = bias
            if scale is not None:
                kw["scale"] = scale
            if accum is not None:
                kw["accum_out"] = accum
            S.op("act", lambda e: e.activation(out=out, in_=in_, func=func, **kw), reads, writes)

        def TT(out, in0, in1, op, reads, writes, eng="dve"):
            S.op(eng, lambda e: e.tensor_tensor(out=out, in0=in0, in1=in1, op=op), reads, writes)

        def TS(out, in0, s1, op0, reads, writes, s2=None, op1=None, eng="dve"):
            if op1 is None:
                S.op(eng, lambda e: e.tensor_scalar(out=out, in0=in0, scalar1=s1, scalar2=None, op0=op0), reads, writes)
            else:
                S.op(eng, lambda e: e.tensor_scalar(out=out, in0=in0, scalar1=s1, scalar2=s2, op0=op0, op1=op1), reads, writes)

        def STT(out, in0, scalar, in1, op0, op1, reads, writes):
            S.op("dve", lambda e: e.scalar_tensor_tensor(out=out, in0=in0, scalar=scalar, in1=in1, op0=op0, op1=op1), reads, writes)

        def RECIP(out, in_, reads, writes):
            S.op("dve", lambda e: e.reciprocal(out=out, in_=in_), reads, writes)

        def TRANS(ps, items, reads, f32=False):
            pv = ps.ap[:, 0:512] if f32 else ps.ap[:, 0:512].bitcast(BF16)
            idn = ident_f if f32 else ident_b
            fns = []
            for (c0, a, n) in items:
                fns.append(lambda pe, c0=c0, a=a, n=n: pe.transpose(pv[:, c0:c0 + n], a, idn.ap[:]))
            S.pe_group(fns, reads=list(reads) + [idn], writes=[ps])
            return pv

        def subs_from(c_lo):
            if c_lo == 0:
                return [(0, 512), (512, 512), (1024, 128)]
            return [(128, 512), (640, 512)]

        def norm_fm(src, sres, c0, w, gofs, dst, dres, dc0):
            ACT(sq.ap[:, :, 0:w], src[:, :, c0:c0 + w], AF.Square, sres, [sq])
            ps = S.rot("misc")
            S.mm(ps, [(ones_b.ap[:], sq.ap[:, k, 0:w]) for k in range(16)], reads=[sq, ones_b], out_ap=ps.ap[:, 0:w])
            ACT(st2.ap[:, 0:w], ps.ap[:, 0:w], AF.Sqrt, [ps, vec], [st2], bias=V(VO["eps"]), scale=1.0 / D)
            RECIP(st2.ap[:, 0:w], st2.ap[:, 0:w], [st2], [st2])
            for k in range(16):
                STT(dst[:, k, dc0:dc0 + w], src[:, k, c0:c0 + w], V(gofs + k), st2.ap[:, 0:w], ALU.mult, ALU.mult,
                    [sres[k] if len(sres) == 16 else sres[0], st2, vec], [dres[k] if len(dres) == 16 else dres[0]])

        def proj_fm_g(slot, sv, kc, nm, src, sreads, subs, evac, k0=0):
            for mi in range(nm):
                for (c0, w) in subs:
                    ps = S.rot("big")
                    S.mm(ps, [(sv[:, k, mi * 128:(mi + 1) * 128], src[:, k0 + k, c0:c0 + w]) for k in range(kc)],
                         reads=[slot] + list(sreads), out_ap=ps.ap[:, 0:w])
                    evac(mi, c0, w, ps)
                    yield
            W.prefetch()

        def proj_fm(*a_, **k_):
            for _ in proj_fm_g(*a_, **k_):
                pass

        def proj_tm_g(slot, sv, cols, ntt, evac):
            for tt in range(ntt):
                ps = S.rot("big")
                S.mm(ps, [(uT.ap[:, k, tt * 128:(tt + 1) * 128], sv[:, k, 0:cols]) for k in range(16)],
                     reads=[slot] + UP, out_ap=ps.ap[:, 0:cols])
                evac(tt, ps)
                yield
            W.prefetch()

        def load_xT(tok0, tt, dst, dres, dc0, xs_pool):
            xs = S.rot(xs_pool)
            S.dma("sp", xs.ap[:], x_d[tok0 + tt * 128: tok0 + (tt + 1) * 128, :], writes=[xs])
            for q4 in range(4):
                ps = S.rot("misc")
                TRANS(ps, [(j * 128, xs.ap[:, (q4 * 4 + j) * 128:(q4 * 4 + j + 1) * 128], 128) for j in range(4)], [xs], f32=True)
                o = dst[:, q4 * 4:(q4 + 1) * 4, dc0:dc0 + 128]
                i_ = ps.ap[:, 0:512].rearrange("p (a b) -> p a b", a=4)
                if q4 % 2 == 0:
                    ACT(o, i_, AF.Copy, [ps], dres[q4 * 4:(q4 + 1) * 4] if len(dres) == 16 else dres)
                else:
                    S.op("dve", lambda e, o=o, i_=i_: e.tensor_copy(out=o, in_=i_), [ps], dres[q4 * 4:(q4 + 1) * 4] if len(dres) == 16 else dres)

        def tile(tok0, ntt, full, attn, snap_tt, upd_last, flag_tts, is_A, out0, dbg_i=None):
            Tn = ntt * 128
            subs = [(c0, min(512, Tn - c0)) for c0 in range(0, Tn, 512)]
            S.barrier()
            ar = Arena(R_h, 16 * T * 4)
            xs_b = [ar.alloc([128, 2048], F32, "xs%d" % i) for i in range(2)]
            xT_b = [ar.alloc([128, 16, 128], F32, "xT%d" % i) for i in range(2)]
            S.pool_of("xs", xs_b)
            for tt in range(ntt):
                xT = xT_b[tt % 2]
                load_xT(tok0, tt, xT.ap, [xT], 0, "xs")
                norm_fm(xT.ap, [xT], 0, 128, VO["g_mix0"], uT.ap, UP, tt * 128)
            S.barrier()
            ar = Arena(R_h, 16 * T * 4)
            LF = ar.alloc([128, NT, 4], F32, "LF")
            Acol = ar.alloc([128, NT, 4], F32, "Acol")
            EMB = ar.alloc([128, NT, 4], F32, "EMB")
            EG = ar.alloc([128, NT, 4], F32, "EG")
            EG16 = ar.alloc([128, NT, 4], F32, "EG16")
            gt = ar.alloc([128, 16], F32, "gt")
            slot, sv = W.next("gates")
            for tt in range(ntt):
                ps = S.rot("misc")
                S.mm(ps, [(uT.ap[:, k, tt * 128:(tt + 1) * 128], sv[:, k, 0:8]) for k in range(16)],
                     reads=[slot] + UP, out_ap=ps.ap[:, 0:8])
                TT(gt.ap[:, 0:8], ps.ap[:, 0:8], vec.ap[:, VO["gate_b"]:VO["gate_b"] + 8], ALU.add, [ps, vec], [gt])
                ACT(gt.ap[:, 8:12], gt.ap[:, 4:8], AF.Exp, [gt], [gt], scale=-1.0)
                ACT(gt.ap[:, 8:12], gt.ap[:, 8:12], AF.Ln, [gt, vec], [gt], bias=V(VO["one"]), scale=1.0)
                TS(LF.ap[:, tt, :], gt.ap[:, 8:12], -1.0, ALU.mult, [gt], [LF])
                ps2 = S.rot("misc")
                S.pe_group([
                    lambda pe, ps2=ps2, tt=tt: pe.matmul(ps2.ap[:, 0:4], U_f.ap[:], LF.ap[:, tt, :], start=True, stop=True),
                    lambda pe, ps2=ps2, tt=tt: pe.matmul(ps2.ap[:, 4:8], ones_f.ap[:], LF.ap[:, tt, :], start=True, stop=True),
                ], reads=[U_f, ones_f, LF], writes=[ps2])
                TT(gt.ap[:, 12:16], gt.ap[:, 0:4], ps2.ap[:, 0:4], ALU.subtract, [gt, ps2], [gt])
                ACT(Acol.ap[:, tt, :], gt.ap[:, 12:16], AF.Exp, [gt], [Acol])
                if tt in flag_tts:
                    TS(Acol.ap[:, tt, :], Acol.ap[:, tt, :], V(VO["flag"]), ALU.mult, [Acol, vec], [Acol])
                ACT(EMB.ap[:, tt, :], ps2.ap[:, 0:4], AF.Exp, [ps2], [EMB], scale=-1.0)
                ACT(EG.ap[:, tt, :], ps2.ap[:, 4:8], AF.Exp, [ps2], [EG])
                ACT(EG16.ap[:, tt, :], ps2.ap[:, 4:8], AF.Exp, [ps2, vec], [EG16], bias=V(VO["nl16"]), scale=1.0)
            W.prefetch()
            ar_base = ar.off

            preq = ar.alloc([128, 2, T + 4], BF16, "preq")
            prek = ar.alloc([128, 2, T + 4], BF16, "prek")
            dblm = []
            for i_ in range(2):
                dblm.append(dict(
                    qT=ar.alloc([128, 2, T], BF16, "qT%d" % i_), kT=ar.alloc([128, 2, T], BF16, "kT%d" % i_),
                    kTM=ar.alloc([128, NT, 256], BF16, "kTM%d" % i_), va=ar.alloc([128, NT, 258], BF16, "va%d" % i_),
                    sigo=ar.alloc([128, NT, 256], BF16, "sigo%d" % i_)))
            hn = [ar.alloc([128, 256], BF16, "hn%d" % i) for i in range(2)]
            PTb = [ar.alloc([128, 128], BF16, "PT%d" % i) for i in range(2)]
            dg = [ar.alloc([128, 4, 128], BF16, "dg%d" % i) for i in range(2)]
            junk = ar.alloc([128, 256], BF16, "junk")

            def conv_silu_g(pre, fbase, dst):
                for mi in range(2):
                    f = fbase + mi
                    d_ = dg[mi % 2]
                    for j in range(4):
                        TS(d_.ap[:, j, :], ident_b.ap[:], V(VO["qk_cw"] + f * 4 + j), ALU.mult, [ident_b, vec], [d_])
                    for (c0, w) in subs:
                        ps = S.rot("big")
                        S.mm(ps, [(d_.ap[:, j, :], pre.ap[:, mi, c0 + j + 1:c0 + j + 1 + w]) for j in range(4)],
                             reads=[d_, pre], out_ap=ps.ap[:, 0:w])
                        ACT(dst.ap[:, mi, c0:c0 + w], ps.ap[:, 0:w], AF.Silu, [ps, vec], [dst], bias=V(VO["qk_cb"] + f), scale=1.0)
                        yield

            def proj_pre_g(tag, pre, fbase):
                slot, sv = W.next(tag)
                S.op("dve", lambda e: e.tensor_copy(out=pre.ap[:, :, 0:4], in_=prehalo.ap[:, fbase:fbase + 2, :]), [prehalo], [pre])

                def ev(mi, c0, w, ps):
                    ACT(pre.ap[:, mi, 4 + c0:4 + c0 + w], ps.ap[:, 0:w], AF.Copy, [ps], [pre])
                yield from proj_fm_g(slot, sv, 16, 2, uT.ap, UP, subs, ev)
                if snap_tt is not None:
                    st_ = (snap_tt + 1) * 128
                    S.op("dve", lambda e: e.tensor_copy(out=prehalo.ap[:, fbase:fbase + 2, :], in_=pre.ap[:, :, st_:st_ + 4]), [pre], [prehalo])

            def mlstm_A(hd):
                B_ = dblm[hd % 2]
                qT, kT, kTM, va, sigo = B_["qT"], B_["kT"], B_["kTM"], B_["va"], B_["sigo"]
                if full:
                    yield from proj_pre_g("mq%d" % hd, preq, hd * 2)
                    yield from conv_silu_g(preq, hd * 2, qT)
                yield from proj_pre_g("mk%d" % hd, prek, 8 + hd * 2)
                yield from conv_silu_g(prek, 8 + hd * 2, kT)
                for tt in range(ntt):
                    ps = S.rot("misc")
                    pv = TRANS(ps, [(kt * 128, kT.ap[:, kt, tt * 128:(tt + 1) * 128], 128) for kt in range(2)], [kT])
                    S.op("dve", lambda e, pv=pv, tt=tt: e.tensor_copy(out=kTM.ap[:, tt, :], in_=pv[:, 0:256]), [ps], [kTM])
                    if tt % 2 == 1:
                        yield
                slot, sv = W.next("mv%d" % hd)

                def ev_v(tt, ps):
                    ACT(va.ap[:, tt, 0:256], ps.ap[:, 0:256], AF.Copy, [ps, Acol], [va], scale=Acol.ap[:, tt, hd:hd + 1])
                    S.op("dve", lambda e: e.tensor_copy(out=va.ap[:, tt, 256:257], in_=Acol.ap[:, tt, hd:hd + 1]), [Acol], [va])
                yield from proj_tm_g(slot, sv, 256, ntt, ev_v)
                if full:
                    slot, sv = W.next("mo%d" % hd)

                    def ev_o(tt, ps):
                        ACT(sigo.ap[:, tt, :], ps.ap[:, 0:256], AF.Sigmoid, [ps], [sigo])
                    yield from proj_tm_g(slot, sv, 256, ntt, ev_o)

            def mlstm_B(hd):
                B_ = dblm[hd % 2]
                qT, kT, kTM, va, sigo = B_["qT"], B_["kT"], B_["kTM"], B_["va"], B_["sigo"]
                egprev = egp.ap[:, hd:hd + 1]
                egres = egp
                for tt in range(ntt):
                    tsl = slice(tt * 128, (tt + 1) * 128)
                    if full:
                        ps = S.rot("misc")
                        S.mm(ps, [(kT.ap[:, kt, tsl], qT.ap[:, kt, tsl]) for kt in range(2)], reads=[kT, qT], out_ap=ps.ap[:, 0:128])
                        PT = PTb[tt % 2]
                        TT(PT.ap[:], ps.ap[:, 0:128], mask16.ap[:], ALU.mult, [ps, mask16], [PT])
                        yield
                        psx = pxy
                        S.mm(psx, [(PT.ap[:], va.ap[:, tt, 0:257])] + [(qT.ap[:, kt, tsl], Cbf.ap[:, hd, kt, 0:257]) for kt in range(2)],
                             reads=[PT, va, qT, Cbf], out_ap=psx.ap[:, 0:257])
                    if tt <= upd_last:
                        S.pe_group([
                            lambda pe, kt=kt, tt=tt: pe.matmul(wide.ap[:, kt * 512:kt * 512 + 257], kTM.ap[:, tt, kt * 128:(kt + 1) * 128],
                                                               va.ap[:, tt, 0:257], start=True, stop=True)
                            for kt in range(2)], reads=[kTM, va], writes=[wide])
                        for kt in range(2):
                            STT(Tst.ap[:, hd, kt, :], Tst.ap[:, hd, kt, :], egprev, wide.ap[:, kt * 512:kt * 512 + 257],
                                ALU.mult, ALU.add, [Tst, wide, egres], [Tst])
                        if full or tt == upd_last:
                            for kt in range(2):
                                ACT(Cbf.ap[:, hd, kt, 0:257], Tst.ap[:, hd, kt, :], AF.Copy, [Tst, EG16], [Cbf], scale=EG16.ap[:, tt, hd:hd + 1])
                        egprev = EG.ap[:, tt, hd:hd + 1]
                        egres = EG
                    yield
                    if full:
                        ACT(sm.ap[:, 0:1], psx.ap[:, 256:257], AF.Abs, [psx], [sm])
                        TT(sm.ap[:, 0:1], sm.ap[:, 0:1], EMB.ap[:, tt, hd:hd + 1], ALU.max, [sm, EMB], [sm])
                        RECIP(sm.ap[:, 1:2], sm.ap[:, 0:1], [sm], [sm])
                        ACT(junk.ap[:], psx.ap[:, 0:256], AF.Square, [psx, sm], [junk, sm], scale=sm.ap[:, 1:2], accum=sm.ap[:, 2:3])
                        yield
                        ACT(sm.ap[:, 3:4], sm.ap[:, 2:3], AF.Sqrt, [sm, vec], [sm], bias=V(VO["eps"]), scale=1.0 / 256)
                        RECIP(sm.ap[:, 4:5], sm.ap[:, 3:4], [sm], [sm])
                        TT(sm.ap[:, 5:6], sm.ap[:, 4:5], sm.ap[:, 1:2], ALU.mult, [sm], [sm])
                        h_ = hn[tt % 2]
                        STT(h_.ap[:], psx.ap[:, 0:256], sm.ap[:, 5:6], sigo.ap[:, tt, :], ALU.mult, ALU.mult, [psx, sm, sigo], [h_])
                        yield
                        ps3 = S.rot("misc")
                        pv = TRANS(ps3, [(vt * 128, h_.ap[:, vt * 128:(vt + 1) * 128], 128) for vt in range(2)], [h_])
                        for vt in range(2):
                            ACT(mixT.ap[:, hd * 2 + vt, tsl], pv[:, vt * 128:(vt + 1) * 128], AF.Copy, [ps3, vec], [MP[hd * 2 + vt]],
                                scale=V(VO["ml_g"] + hd * 2 + vt))
                        yield
                if snap_tt is not None:
                    S.op("dve", lambda e: e.tensor_copy(out=egp.ap[:, hd:hd + 1], in_=EG.ap[:, snap_tt, hd:hd + 1]), [EG], [egp])

            def drive(*gens):
                gens = [g for g in gens if g is not None]
                while gens:
                    for g in list(gens):
                        try:
                            next(g)
                        except StopIteration:
                            gens.remove(g)

            drive(mlstm_A(0))
            for hd in range(4):
                drive(mlstm_B(hd), mlstm_A(hd + 1) if hd < 3 else None)

            S.barrier()
            if attn:
                ar.off = ar_base
                dbla = []
                for i_ in range(2):
                    dbla.append(dict(
                        qTb=[ar.alloc([128, T], BF16, "qTb%d_%d" % (i_, i)) for i in range(2)],
                        kbuf=[ar.alloc([128, 512 + T], BF16, "kbuf%d_%d" % (i_, i)) for i in range(2)],
                        vbuf=[ar.alloc([128, 4 + NT, 128], BF16, "vbuf%d_%d" % (i_, i)) for i in range(2)],
                        b2=[ar.alloc([128, 640], F32, "b2_%d_%d" % (i_, i)) for i in range(2)]))
                sc = [ar.alloc([128, 640], F32, "sc%d" % i) for i in range(2)]
                pex = [ar.alloc([128, 640], F32, "pex%d" % i) for i in range(2)]
                pn = [ar.alloc([128, 640], BF16, "pn%d" % i) for i in range(2)]
                pTt = [ar.alloc([128, 640], BF16, "pT%d" % i) for i in range(2)]
                asubs = subs if full else [s_ for s_ in subs if s_[0] >= 512]
                att0 = 0 if full else 4

                def attn_A(pr):
                    B_ = dbla[pr % 2]
                    qTb, kbuf, vbuf, b2 = B_["qTb"], B_["kbuf"], B_["vbuf"], B_["b2"]
                    for hh in range(2):
                        h = pr * 2 + hh
                        S.op("dve", lambda e, hh=hh, h=h: e.tensor_copy(out=kbuf[hh].ap[:, 0:512], in_=KVk.ap[:, h, :]), [KVk], [kbuf[hh]])
                        S.op("dve", lambda e, hh=hh, h=h: e.tensor_copy(out=vbuf[hh].ap[:, 0:4, :], in_=KVv.ap[:, h, :, :]), [KVv], [vbuf[hh]])
                        if full:
                            S.dma("sp", b2[hh].ap[:], b2_d[h], writes=[b2[hh]])
                    if full:
                        slot, sv = W.next("aq%d" % pr)

                        def ev_q(mi, c0, w, ps):
                            ACT(qTb[mi].ap[:, c0:c0 + w], ps.ap[:, 0:w], AF.Copy, [ps], [qTb[mi]], scale=float(128 ** -0.5))
                        yield from proj_fm_g(slot, sv, 16, 2, uT.ap, UP, subs, ev_q)
                    slot, sv = W.next("ak%d" % pr)

                    def ev_k(mi, c0, w, ps):
                        S.op("dve", lambda e: e.tensor_copy(out=kbuf[mi].ap[:, 512 + c0:512 + c0 + w], in_=ps.ap[:, 0:w]), [ps], [kbuf[mi]])
                    yield from proj_fm_g(slot, sv, 16, 2, uT.ap, UP, asubs, ev_k)
                    slot, sv = W.next("av%d" % pr)
                    for tt in range(att0, ntt):
                        ps = S.rot("big")
                        S.mm(ps, [(uT.ap[:, k, tt * 128:(tt + 1) * 128], sv[:, k, 0:256]) for k in range(16)],
                             reads=[slot] + UP, out_ap=ps.ap[:, 0:256])
                        for hh in range(2):
                            ACT(vbuf[hh].ap[:, 4 + tt, :], ps.ap[:, hh * 128:(hh + 1) * 128], AF.Copy, [ps], [vbuf[hh]])
                        yield
                    W.prefetch()

                def attn_B(pr):
                    B_ = dbla[pr % 2]
                    qTb, kbuf, vbuf, b2 = B_["qTb"], B_["kbuf"], B_["vbuf"], B_["b2"]
                    if full:
                        def unit(u, hh):
                            h = pr * 2 + hh
                            i2 = (u * 2 + hh) % 2
                            q_ = qTb[hh].ap[:, u * 128:(u + 1) * 128]
                            S.pe_group([
                                lambda pe: pe.matmul(wide.ap[:, 0:512], q_, kbuf[hh].ap[:, u * 128:u * 128 + 512], start=True, stop=True),
                                lambda pe: pe.matmul(wide.ap[:, 512:640], q_, kbuf[hh].ap[:, u * 128 + 512:u * 128 + 640], start=True, stop=True),
                            ], reads=[qTb[hh], kbuf[hh]], writes=[wide])
                            s_ = sc[i2]
                            TT(s_.ap[:], wide.ap[:, 0:640], b2[hh].ap[:], ALU.add, [wide, b2[hh]], [s_])
                            if is_A and u <= 4:
                                nm_ = 640 - u * 128
                                TS(s_.ap[:, 0:nm_], s_.ap[:, 0:nm_], V(VO["maskc"]), ALU.add, [s_, vec], [s_])
                            yield
                            m_ = S.rot("sm2")
                            S.op("dve", lambda e: e.tensor_reduce(out=m_.ap[:, 0:1], in_=s_.ap[:], axis=AX.X, op=ALU.max, negate=True), [s_], [m_])
                            p_ = pex[i2]
                            ACT(p_.ap[:], s_.ap[:], AF.Exp, [s_, m_], [p_, m_], bias=m_.ap[:, 0:1], scale=1.0, accum=m_.ap[:, 1:2])
                            yield
                            RECIP(m_.ap[:, 2:3], m_.ap[:, 1:2], [m_], [m_])
                            n_ = pn[i2]
                            TS(n_.ap[:], p_.ap[:], m_.ap[:, 2:3], ALU.mult, [p_, m_], [n_])
                            yield
                            psT = S.rot("misc")
                            pv = TRANS(psT, [(j * 128, n_.ap[:, j * 128:(j + 1) * 128], 128) for j in range(5)], [n_])
                            t_ = pTt[i2]
                            ACT(t_.ap[:], pv[:, 0:640], AF.Copy, [psT], [t_])
                            yield
                            pso = S.rot("misc")
                            S.mm(pso, [(vbuf[hh].ap[:, u + j, :], t_.ap[:, j * 128:(j + 1) * 128]) for j in range(5)],
                                 reads=[vbuf[hh], t_], out_ap=pso.ap[:, 0:128])
                            S.op("dve", lambda e: e.tensor_copy(out=mixT.ap[:, 8 + h, u * 128:(u + 1) * 128], in_=pso.ap[:, 0:128]), [pso], [MP[8 + h]])
                            yield
                        units = [unit(u, hh) for u in range(ntt) for hh in range(2)]
                        active = []
                        it = iter(units)
                        while True:
                            while len(active) < 2:
                                g = next(it, None)
                                if g is None:
                                    break
                                active.append(g)
                            if not active:
                                break
                            for g in list(active):
                                try:
                                    next(g)
                                except StopIteration:
                                    active.remove(g)
                            yield
                    if snap_tt is not None:
                        sk = (snap_tt + 1) * 128
                        for hh in range(2):
                            h = pr * 2 + hh
                            S.op("dve", lambda e, hh=hh, h=h: e.tensor_copy(out=KVk.ap[:, h, :], in_=kbuf[hh].ap[:, sk:sk + 512]), [kbuf[hh]], [KVk])
                            S.op("dve", lambda e, hh=hh, h=h: e.tensor_copy(out=KVv.ap[:, h, :, :], in_=vbuf[hh].ap[:, snap_tt + 1:snap_tt + 5, :]), [vbuf[hh]], [KVv])

                drive(attn_A(0))
                for pr in range(4):
                    drive(attn_B(pr), attn_A(pr + 1) if pr < 3 else None)

            if not full:
                return
            S.barrier()
            if dbg == 2:
                S.dma("pool", dbg_d[dbg_i], mixT.ap[:], reads=MP, final=True, semres=mixT)
                return
            aru = Arena(R_u, 8 * T * 4)
            xs2 = [aru.alloc([128, 2048], F32, "xsb%d" % i) for i in range(2)]
            S.pool_of("xs2", xs2)
            for tt in range(ntt):
                load_xT(tok0, tt, hT.ap, HP, tt * 128, "xs2")
            for s_ in range(8):
                slot, sv = W.next("wo%d" % s_)

                def ev_o2(mi, c0, w, ps, s_=s_):
                    d_ = s_ * 2 + mi
                    TT(hT.ap[:, d_, c0:c0 + w], ps.ap[:, 0:w], hT.ap[:, d_, c0:c0 + w], ALU.add, [ps, HP[d_]], [HP[d_]])
                proj_fm(slot, sv, 16, 2, mixT.ap, MP, subs, ev_o2)
            S.barrier()
            if dbg_i is not None and dbg_d is not None:
                S.dma("sp", dbg_d[dbg_i], hT.ap[:], reads=HP, final=True, semres=hT)
            mlp(0, 0)
            for c0 in range(0, T, 256):
                w = min(256, T - c0)
                norm_fm(hT.ap, HP, c0, w, VO["g_mix1"], uT.ap, UP, c0)
            S.barrier()
            zT = mixT
            for s_ in range(8):
                slotA, svA = W.next("p1a%d" % s_)
                slotG, svG = W.next("p1g%d" % s_)
                for mi in range(2):
                    f = s_ * 2 + mi
                    for (c0, w) in subs:
                        psA = S.rot("big")
                        S.mm(psA, [(svA[:, k, mi * 128:(mi + 1) * 128], uT.ap[:, k, c0:c0 + w]) for k in range(16)], reads=[slotA] + UP, out_ap=psA.ap[:, 0:w])
                        psG = S.rot("big")
                        S.mm(psG, [(svG[:, k, mi * 128:(mi + 1) * 128], uT.ap[:, k, c0:c0 + w]) for k in range(16)], reads=[slotG] + UP, out_ap=psG.ap[:, 0:w])
                        sg = S.rot("rl")
                        ACT(sg.ap[:, 0:w], psG.ap[:, 0:w], AF.Sigmoid, [psG, vec], [sg], bias=V(VO["pw1_b"] + 16 + f), scale=1.0)
                        STT(zT.ap[:, f, c0:c0 + w], psA.ap[:, 0:w], V(VO["pw1_b"] + f), sg.ap[:, 0:w], ALU.add, ALU.mult, [psA, sg, vec], [MP[f]])
                W.prefetch()
            if is_A:
                TS(zT.ap[:, :, 96:128], zT.ap[:, :, 96:128], V(VO["flag"]), ALU.mult, MP + [vec], MP)
            S.barrier()
            yT = uT
            csubs = subs_from(128)
            for f in range(16):
                for j in range(31):
                    TS(dg31.ap[:, j, :], ident_b.ap[:], V(VO["dw_w"] + f * 31 + j), ALU.mult, [ident_b, vec], [dg31],
                       eng="dve")
                for (c0, w) in csubs:
                    ps = S.rot("big")
                    S.mm(ps, [(dg31.ap[:, j, :], zT.ap[:, f, c0 - 30 + j:c0 - 30 + j + w]) for j in range(31)], reads=[dg31, MP[f]], out_ap=ps.ap[:, 0:w])
                    ACT(yT.ap[:, f, c0:c0 + w], ps.ap[:, 0:w], AF.Identity, [ps, vec], [UP[f]], bias=V(VO["dw_b"] + f), scale=1.0)
            S.barrier()
            sT = mixT
            for c0 in range(128, T, 256):
                w = 256
                ACT(sq.ap[:, :, 0:w], yT.ap[:, :, c0:c0 + w], AF.Square, UP, [sq])
                psm = S.rot("misc")
                S.mm(psm, [(ones_b.ap[:], yT.ap[:, k, c0:c0 + w]) for k in range(16)], reads=UP + [ones_b], out_ap=psm.ap[:, 0:w])
                psq = S.rot("misc")
                S.mm(psq, [(ones_b.ap[:], sq.ap[:, k, 0:w]) for k in range(16)], reads=[sq, ones_b], out_ap=psq.ap[:, 0:w])
                TS(st1.ap[:, 0:w], psm.ap[:, 0:w], 1.0 / D, ALU.mult, [psm], [st1])
                TT(st2.ap[:, 0:w], st1.ap[:, 0:w], st1.ap[:, 0:w], ALU.mult, [st1], [st2])
                STT(st2.ap[:, 0:w], psq.ap[:, 0:w], 1.0 / D, st2.ap[:, 0:w], ALU.mult, ALU.subtract, [psq, st2], [st2])
                ACT(st2.ap[:, 0:w], st2.ap[:, 0:w], AF.Sqrt, [st2, vec], [st2], bias=V(VO["eps"]), scale=1.0)
                RECIP(st2.ap[:, 0:w], st2.ap[:, 0:w], [st2], [st2])
                for f in range(16):
                    t_ = S.rot("rl")
                    TT(t_.ap[:, 0:w], yT.ap[:, f, c0:c0 + w], st1.ap[:, 0:w], ALU.subtract, [UP[f], st1], [t_])
                    TT(t_.ap[:, 0:w], t_.ap[:, 0:w], st2.ap[:, 0:w], ALU.mult, [t_, st2], [t_])
                    ACT(sT.ap[:, f, c0:c0 + w], t_.ap[:, 0:w], AF.Silu, [t_, vec], [MP[f]], bias=V(VO["ln_b"] + f), scale=V(VO["ln_g"] + f))
            S.barrier()
            for s_ in range(8):
                slot, sv = W.next("p2%d" % s_)

                def ev_p2(mi, c0, w, ps, s_=s_):
                    d_ = s_ * 2 + mi
                    STT(hT.ap[:, d_, c0:c0 + w], ps.ap[:, 0:w], V(VO["pw2_b"] + d_), hT.ap[:, d_, c0:c0 + w], ALU.add, ALU.add, [ps, HP[d_], vec], [HP[d_]])
                proj_fm(slot, sv, 16, 2, sT.ap, MP, csubs, ev_p2)
            S.barrier()
            mlp(1, 128)
            S.barrier()
            aru = Arena(R_u, 8 * T * 4)
            oT = [aru.alloc([128, 16, 128], F32, "oT%d" % i) for i in range(2)]
            ob = [aru.alloc([128, 2048], F32, "ob%d" % i) for i in range(2)]
            for tt in range(1, ntt):
                o_ = oT[tt % 2]
                norm_fm(hT.ap, HP, tt * 128, 128, VO["g_fin"], o_.ap, [o_], 0)
                b_ = ob[tt % 2]
                for q4 in range(4):
                    ps = S.rot("misc")
                    TRANS(ps, [(j * 128, o_.ap[:, q4 * 4 + j, :], 128) for j in range(4)], [o_], f32=True)
                    if q4 % 2 == 0:
                        ACT(b_.ap[:, q4 * 512:(q4 + 1) * 512], ps.ap[:, 0:512], AF.Copy, [ps], [b_])
                    else:
                        S.op("dve", lambda e, b_=b_, ps=ps, q4=q4: e.tensor_copy(out=b_.ap[:, q4 * 512:(q4 + 1) * 512], in_=ps.ap[:, 0:512]), [ps], [b_])
                r0 = out0 + (tt - 1) * 128
                S.dma("sp", out_d[r0:r0 + 128, :], b_.ap[:], reads=[b_], final=True)

        def mlp(l, c_lo):
            msubs = subs_from(c_lo)
            gofs = VO["g_mlp0"] if l == 0 else VO["g_mlp1"]
            for c0 in range(c_lo, T, 256):
                norm_fm(hT.ap, HP, c0, min(256, T - c0), gofs, uT.ap, UP, c0)
            S.barrier()
            hid = [view(R_m, 0, [128, 8, T], BF16, "hid0"), view(R_m, 8 * T * 2, [128, 8, T], BF16, "hid1")]
            for g in range(8):
                hb = hid[g % 2]
                for s4 in range(4):
                    slot, sv = W.next("w1_%d_%d_%d" % (l, g, s4))

                    def ev1(mi, c0, w, ps, s4=s4, hb=hb):
                        r_ = S.rot("rl")
                        ACT(r_.ap[:, 0:w], ps.ap[:, 0:w], AF.Relu, [ps], [r_])
                        TT(hb.ap[:, s4 * 2 + mi, c0:c0 + w], ps.ap[:, 0:w], r_.ap[:, 0:w], ALU.mult, [ps, r_], [hb])
                    proj_fm(slot, sv, 16, 2, uT.ap, UP, msubs, ev1)
                for s4 in range(4):
                    slot, sv = W.next("w2_%d_%d_%d" % (l, g, s4))

                    def ev2(mi, c0, w, ps, s4=s4):
                        d_ = s4 * 4 + mi
                        TT(hT.ap[:, d_, c0:c0 + w], ps.ap[:, 0:w], hT.ap[:, d_, c0:c0 + w], ALU.add, [ps, HP[d_]], [HP[d_]])
                    proj_fm(slot, sv, 8, 4, hb.ap, [hb], msubs, ev2)

        plan_tile(False, False)
        plan_tile(False, True)
        plan_tile(True, True)
        if dbg != 2:
            plan_tile(True, True)
        tile(0, 6, False, False, 5, 5, set(range(6)), False, None)
        tile(768, 9, False, True, 8, 8, set(range(9)), False, None)
        tile(1920, 9, True, True, 7, 7, {0}, True, 0, dbg_i=0)
        if dbg != 2:
            tile(2944, 9, True, True, None, 7, set(), False, 1024, dbg_i=1)
        S.barrier(("sp",))
        S.finish()
        assert W.taken == len(W.plan), (W.taken, len(W.plan))
    return nc


def _host_consts():
    cm = np.zeros((128, 4, 128), np.float32)
    cm[:, 0, :] = np.eye(128, dtype=np.float32)
    cm[:, 1, :] = 1.0
    u = np.triu(np.ones((128, 128), np.float32))
    cm[:, 2, :] = u
    cm[:, 3, :] = u * (1.0 / 16.0)
    return cm


def _bias2(rel_bias):
    qi = np.arange(128)
    kj = np.arange(640)
    qc = qi // 64
    kc = kj // 64
    qpos = qi
    kpos = (kc - 8) * 64 + (kj % 64)
    dist = np.clip(qpos[:, None] - kpos[None, :], -256, 256) + 256
    vis = (kc[None, :] >= qc[:, None]) & (kc[None, :] <= qc[:, None] + 8)
    out = np.empty((8, 128, 640), np.float32)
    for h in range(8):
        out[h] = np.where(vis, rel_bias[h][dist], np.float32(NEG))
    return out


_NC_CACHE = {}


def kernel(**inputs):
    dbg = inputs.pop("_dbg", False)
    x = np.asarray(inputs["x"], np.float32)
    f = lambda n: np.asarray(inputs[n], np.float32)
    vecs = np.zeros((128, NV), np.float32)

    def put(name, arr):
        arr = np.asarray(arr, np.float32)
        vecs[:, VO[name]:VO[name] + arr.shape[1]] = arr

    put("g_mix0", _fm(f("mixer_norm_g")[0]))
    put("g_mix1", _fm(f("mixer_norm_g")[1]))
    put("g_mlp0", _fm(f("mlp_norm_g")[0]))
    put("g_mlp1", _fm(f("mlp_norm_g")[1]))
    put("g_fin", _fm(f("final_norm_g")))
    put("qk_cb", _fm(f("qk_conv_b")[0]))
    cw = f("qk_conv_w")[0]
    put("qk_cw", np.ascontiguousarray(cw.T.reshape(16, 128, 4).transpose(1, 0, 2)).reshape(128, 64))
    put("ml_g", _fm(f("mlstm_norm_g")[0]))
    put("pw1_b", _fm(f("conv_pw1_b")[0]))
    dw = f("conv_dw_w")[0]
    put("dw_w", np.ascontiguousarray(dw.T.reshape(16, 128, 31).transpose(1, 0, 2)).reshape(128, 496))
    put("dw_b", _fm(f("conv_dw_b")[0]))
    put("ln_g", _fm(f("conv_ln_g")[0]))
    put("ln_b", _fm(f("conv_ln_b")[0]))
    put("pw2_b", _fm(f("conv_pw2_b")[0]))
    gb = np.concatenate([f("igate_b")[0], f("fgate_b")[0]])[None, :]
    put("gate_b", np.broadcast_to(gb, (128, 8)))
    vecs[:, VO["eps"]] = EPS
    vecs[:, VO["one"]] = 1.0
    vecs[:, VO["nl16"]] = -np.log(16.0)
    vecs[:, VO["zero"]] = 0.0
    cm = _host_consts()
    b2 = _bias2(f("rel_bias")[0])
    w_in = np.ascontiguousarray(f("mix_w_in")[0])
    w_out = np.ascontiguousarray(f("mix_w_out")[0])
    pw1 = np.ascontiguousarray(f("conv_pw1_w")[0])
    pw2 = np.ascontiguousarray(f("conv_pw2_w")[0])
    w1 = np.ascontiguousarray(f("mlp_w1"))
    w2 = np.ascontiguousarray(f("mlp_w2"))
    in_maps = []
    for c in range(8):
        b, half = c // 2, c % 2
        xe = np.zeros((VS, D), np.float32)
        if half == 0:
            xe[2048:] = x[b, :2048]
        else:
            xe[:] = x[b]
        v = vecs.copy()
        v[:, VO["flag"]] = float(half)
        v[:, VO["maskc"]] = 0.0 if half == 1 else NEG
        in_maps.append({"x_ext": xe, "vecs": v, "cmat": cm, "bias2": b2, "w_in": w_in, "w_out": w_out,
                        "pw1": pw1, "pw2": pw2, "w1": w1, "w2": w2})
    key = dbg
    if key not in _NC_CACHE:
        _NC_CACHE[key] = build_program(dbg)
    nc = _NC_CACHE[key]
    res = run_bass_kernel_spmd(nc, in_maps, core_ids=list(range(8)))
    out = np.empty((4, 4096, D), np.float32)
    for c in range(8):
        b, half = c // 2, c % 2
        out[b, half * 2048:(half + 1) * 2048] = res.results[c]["out"]
    if dbg:
        return out, [res.results[c]["dbg"] for c in range(8)]
    return out
```

```python
import numpy as np
from contextlib import ExitStack
import concourse.bass as bass
import concourse.mybir as mybir
from concourse.bass_utils import run_bass_kernel_spmd

F32 = mybir.dt.float32
BF16 = mybir.dt.bfloat16
AF = mybir.ActivationFunctionType
ALU = mybir.AluOpType
AX = mybir.AxisListType


class Res:
    __slots__ = ("name", "w", "r", "dsem")

    def __init__(self, name):
        self.name = name
        self.w = None
        self.r = {}
        self.dsem = None


class Buf(Res):
    __slots__ = ("ap", "parts", "nbytes")

    def __init__(self, name, ap):
        super().__init__(name)
        self.ap = ap
        self.parts = {}

    def part(self, key):
        p = self.parts.get(key)
        if p is None:
            p = Res("%s/%s" % (self.name, key))
            self.parts[key] = p
        return p


class Sched:
    ENG = ("pe", "act", "dve", "pool", "sp")

    def __init__(self, nc, es):
        self.nc = nc
        self.es = es
        self.eng = {"pe": nc.tensor, "act": nc.scalar, "dve": nc.vector, "pool": nc.gpsimd, "sp": nc.sync}
        self.sem = {}
        self.cnt = {}
        for e in ("pe", "act", "dve", "pool"):
            self.sem[e] = es.enter_context(nc.semaphore("s_" + e))
            self.cnt[e] = 0
        self.known = {e: {} for e in self.ENG}
        self.dsems = {}
        self.finals = []
        self.ninst = 0
        self._rot = {}

    def sb(self, name, shape, dtype):
        t = self.es.enter_context(self.nc.sbuf_tensor(name, list(shape), dtype))
        return Buf(name, t)

    def psum(self, name, shape, dtype=F32):
        t = self.es.enter_context(self.nc.psum_tensor(name, list(shape), dtype))
        return Buf(name, t)

    def pool_of(self, name, bufs):
        self._rot[name] = [bufs, 0]

    def rot(self, name):
        p = self._rot[name]
        b = p[0][p[1] % len(p[0])]
        p[1] += 1
        return b

    def _dsem(self, res):
        if res.dsem is None:
            nm = "d%d" % len(self.dsems)
            s = self.es.enter_context(self.nc.semaphore(nm))
            res.dsem = [nm, s, 0]
            self.dsems[nm] = res.dsem
        return res.dsem

    def _semof(self, key):
        return self.sem[key] if key in self.sem else self.dsems[key][1]

    def _waits(self, e, reads, writes):
        need = {}
        for r in reads:
            if r.w is not None:
                k, v = r.w
                if need.get(k, 0) < v:
                    need[k] = v
        for w in writes:
            if w.w is not None:
                k, v = w.w
                if need.get(k, 0) < v:
                    need[k] = v
            for k, v in w.r.items():
                if need.get(k, 0) < v:
                    need[k] = v
        kn = self.known[e]
        eng = self.eng[e]
        for k, v in need.items():
            if kn.get(k, 0) < v:
                eng.wait_ge(self._semof(k), v)
                kn[k] = v
                self.ninst += 1

    def _mark(self, key, val, reads, writes):
        for r in reads:
            if r.r.get(key, 0) < val:
                r.r[key] = val
        for w in writes:
            w.w = (key, val)
            w.r = {}

    def op(self, e, fn, reads=(), writes=()):
        self._waits(e, reads, writes)
        ins = fn(self.eng[e])
        self.cnt[e] += 1
        ins.then_inc(self.sem[e], 1)
        self._mark(e, self.cnt[e], reads, writes)
        self.ninst += 1
        return ins

    def pe_group(self, fns, reads=(), writes=()):
        self._waits("pe", reads, writes)
        ins = None
        for fn in fns:
            ins = fn(self.eng["pe"])
            self.ninst += 1
        self.cnt["pe"] += 1
        ins.then_inc(self.sem["pe"], 1)
        self._mark("pe", self.cnt["pe"], reads, writes)

    def mm(self, out, pairs, reads=(), out_ap=None):
        oap = out.ap[:] if out_ap is None else out_ap
        n = len(pairs)
        fns = []
        for i, (l, r) in enumerate(pairs):
            fns.append(lambda pe, l=l, r=r, i=i: pe.matmul(oap, l, r, start=(i == 0), stop=(i == n - 1)))
        self.pe_group(fns, reads=reads, writes=[out])

    def dma(self, q, out_ap, in_ap, reads=(), writes=(), final=False, semres=None):
        self._waits(q, reads, writes)
        sr = semres if semres is not None else (writes[0] if writes else reads[0])
        ds = self._dsem(sr)
        self.eng[q].dma_start(out=out_ap, in_=in_ap).then_inc(ds[1], 16)
        ds[2] += 16
        self._mark(ds[0], ds[2], reads, writes)
        self.ninst += 1
        if final:
            self.finals.append((ds[0], ds[2]))

    def finish(self):
        kn = self.known["sp"]
        for k, v in self.finals:
            if kn.get(k, 0) < v:
                self.eng["sp"].wait_ge(self._semof(k), v)
                kn[k] = v


    def barrier(self, engs=("pe", "act", "dve", "sp")):
        for e in engs:
            kn = self.known[e]
            for k in ("pe", "act", "dve", "pool"):
                v = self.cnt[k]
                if k != e and v > 0 and kn.get(k, 0) < v:
                    self.eng[e].wait_ge(self.sem[k], v)
                    kn[k] = v
            for nm, ds in self.dsems.items():
                if ds[2] > 0 and kn.get(nm, 0) < ds[2]:
                    self.eng[e].wait_ge(ds[1], ds[2])
                    kn[nm] = ds[2]


D = 2048
KC = 16
DFF = 8192
T = 1152
NT = 9
VS = 4096
DIN = 7176
EPS = 1e-6
NEG = -30000.0

VO = {}
_o = 0
for _n, _w in (("g_mix0", 16), ("g_mix1", 16), ("g_mlp0", 16), ("g_mlp1", 16), ("g_fin", 16),
               ("qk_cb", 16), ("qk_cw", 64), ("ml_g", 8), ("pw1_b", 32), ("dw_w", 496),
               ("dw_b", 16), ("ln_g", 16), ("ln_b", 16), ("pw2_b", 16), ("gate_b", 8),
               ("flag", 1), ("maskc", 1), ("eps", 1), ("one", 1), ("nl16", 1), ("zero", 1)):
    VO[_n] = _o
    _o += _w
NV = _o


def _fm(v):
    v = np.asarray(v, np.float32)
    return np.ascontiguousarray(v.reshape(-1, 128).T)


def run_interleaved(gens, depth):
    active = []
    it = iter(gens)
    while True:
        while len(active) < depth:
            g = next(it, None)
            if g is None:
                break
            active.append(g)
        if not active:
            break
        for g in list(active):
            try:
                next(g)
            except StopIteration:
                active.remove(g)


def build_program(dbg=False):
    nc = bass.Bass("TRN2", target_bir_lowering=False)

    def din(name, shape):
        return nc.dram_tensor(name, list(shape), F32, kind="ExternalInput").ap()

    x_d = din("x_ext", [VS, D])
    vec_d = din("vecs", [128, NV])
    cm_d = din("cmat", [128, 4, 128])
    b2_d = din("bias2", [8, 128, 640])
    win_d = din("w_in", [D, DIN])
    wout_d = din("w_out", [D, D])
    pw1_d = din("pw1", [D, 2 * D])
    pw2_d = din("pw2", [D, D])
    w1_d = din("w1", [2, D, DFF])
    w2_d = din("w2", [2, DFF, D])
    out_d = nc.dram_tensor("out", [2048, D], F32, kind="ExternalOutput").ap()
    dbg_d = None
    if dbg:
        dbg_d = nc.dram_tensor("dbg", [2, 128, 16, T], F32, kind="ExternalOutput").ap()

    winv = win_d.rearrange("(k p) n -> p k n", p=128)
    woutv = wout_d.rearrange("(k p) n -> p k n", p=128)
    pw1v = pw1_d.rearrange("(k p) n -> p k n", p=128)
    pw2v = pw2_d.rearrange("(k p) n -> p k n", p=128)
    w1v = [w1_d[l].rearrange("(k p) n -> p k n", p=128) for l in range(2)]
    w2v = [w2_d[l].rearrange("(k p) n -> p k n", p=128) for l in range(2)]

    es = ExitStack()
    with es:
        S = Sched(nc, es)
        vec = S.sb("vec", [128, NV], F32)
        ident_f = S.sb("ident_f", [128, 128], F32)
        ones_f = S.sb("ones_f", [128, 128], F32)
        U_f = S.sb("U_f", [128, 128], F32)
        mask16 = S.sb("mask16", [128, 128], F32)
        ident_b = S.sb("ident_b", [128, 128], BF16)
        ones_b = S.sb("ones_b", [128, 128], BF16)
        KVk = S.sb("KVk", [128, 8, 512], BF16)
        KVv = S.sb("KVv", [128, 8, 4, 128], BF16)
        Tst = S.sb("Tst", [128, 4, 2, 257], F32)
        Cbf = S.sb("Cbf", [128, 4, 2, 258], BF16)
        egp = S.sb("egp", [128, 4], F32)
        prehalo = S.sb("prehalo", [128, 16, 4], BF16)
        st1 = S.sb("st1", [128, 256], F32)
        st2 = S.sb("st2", [128, 256], F32)
        sm = S.sb("sm", [128, 8], F32)
        sm2 = [S.sb("sm2_%d" % i, [128, 4], F32) for i in range(3)]
        NSLOT = 2
        wslots = [S.sb("wslot%d" % i, [128, 4096], BF16) for i in range(NSLOT)]
        R_h = S.sb("R_h", [128, 16 * T], F32)
        R_u = S.sb("R_u", [128, 8 * T], F32)
        R_m = S.sb("R_m", [128, 8 * T], F32)
        R_s = S.sb("R_s", [128, 2048], F32)
        big = [S.psum("pbig%d" % i, [128, 512]) for i in range(3)]
        wide = S.psum("pwide", [128, 1024])
        misc = [S.psum("pmisc%d" % i, [128, 512]) for i in range(2)]
        pxy = S.psum("pxy", [128, 512])
        S.pool_of("big", big)
        S.pool_of("misc", misc)
        S.pool_of("sm2", sm2)

        def V(c):
            return vec.ap[:, c:c + 1]

        def view(reg, off, shape, dt, name):
            nel = 1
            for d_ in shape[1:]:
                nel *= d_
            if dt == BF16:
                assert off % 4 == 0
                words = (nel + 1) // 2
                a = reg.ap[:, off // 4: off // 4 + words].bitcast(BF16)[:, 0:nel]
                nb = words * 4
            else:
                a = reg.ap[:, off // 4: off // 4 + nel]
                nb = nel * 4
            if len(shape) == 3:
                a = a.rearrange("p (a b) -> p a b", a=shape[1])
            elif len(shape) == 4:
                a = a.rearrange("p (a b c) -> p a b c", a=shape[1], b=shape[2])
            b = Buf(name, a)
            b.nbytes = nb
            return b

        class Arena:
            def __init__(self, reg, size):
                self.reg, self.size, self.off = reg, size, 0

            def alloc(self, shape, dt, name):
                b = view(self.reg, self.off, shape, dt, name)
                self.off += (b.nbytes + 31) // 32 * 32
                assert self.off <= self.size, (name, self.off, self.size)
                return b

        hT = view(R_h, 0, [128, 16, T], F32, "hT")
        uT = view(R_u, 0, [128, 16, T], BF16, "uT")
        mixT = view(R_m, 0, [128, 16, T], BF16, "mixT")
        sq = view(R_s, 0, [128, 16, 256], BF16, "sq")
        rl = [view(R_s, 0, [128, 512], F32, "rl0"), view(R_s, 2048, [128, 512], F32, "rl1")]
        dgA = view(R_s, 0, [128, 16, 128], BF16, "dgA")
        dgB = view(R_s, 4096, [128, 15, 128], BF16, "dgB")
        sqh = [view(R_s, 0, [128, 16, 128], BF16, "sqh0"), view(R_s, 4096, [128, 16, 128], BF16, "sqh1")]
        nrot = [0]
        S.pool_of("rl", rl)

        def hp(k):
            return hT.part(k)

        HP = [hT.part(k) for k in range(16)]
        UP = [uT.part(k) for k in range(16)]
        MP = [mixT.part(k) for k in range(16)]

        S.dma("sp", vec.ap[:], vec_d[:, :], writes=[vec])
        S.dma("sp", ident_f.ap[:], cm_d[:, 0, :], writes=[ident_f])
        S.dma("sp", ones_f.ap[:], cm_d[:, 1, :], writes=[ones_f])
        S.dma("sp", U_f.ap[:], cm_d[:, 2, :], writes=[U_f])
        S.dma("sp", mask16.ap[:], cm_d[:, 3, :], writes=[mask16])
        S.dma("pool", ident_b.ap[:], cm_d[:, 0, :], writes=[ident_b])
        S.dma("pool", ones_b.ap[:], cm_d[:, 1, :], writes=[ones_b])
        S.op("dve", lambda e: e.memset(Tst.ap[:], 0.0), writes=[Tst])
        S.op("dve", lambda e: e.memset(Cbf.ap[:], 0.0), writes=[Cbf])
        S.op("dve", lambda e: e.memset(egp.ap[:], 1.0), writes=[egp])
        S.op("dve", lambda e: e.memset(prehalo.ap[:], 0.0), writes=[prehalo])
        S.op("dve", lambda e: e.memset(KVk.ap[:], 0.0), writes=[KVk])
        S.op("dve", lambda e: e.memset(KVv.ap[:], 0.0), writes=[KVv])

        class WStream:
            def __init__(self):
                self.plan = []
                self.issued = 0
                self.taken = 0

            def add(self, tag, ap, kc, cols):
                self.plan.append((tag, ap, kc, cols))

            def _issue(self):
                tag, ap, kc, cols = self.plan[self.issued]
                slot = wslots[self.issued % NSLOT]
                v = slot.ap[:, 0:kc * cols].rearrange("p (k c) -> p k c", k=kc)
                S.dma("pool", v, ap, writes=[slot])
                self.issued += 1

            def next(self, tag):
                while self.issued <= self.taken:
                    self._issue()
                ptag, ap, kc, cols = self.plan[self.taken]
                assert ptag == tag, (ptag, tag)
                slot = wslots[self.taken % NSLOT]
                v = slot.ap[:, 0:kc * cols].rearrange("p (k c) -> p k c", k=kc)
                self.taken += 1
                return slot, v

            def prefetch(self):
                while self.issued < min(len(self.plan), self.taken + NSLOT):
                    self._issue()

        W = WStream()

        def plan_tile(full, attn):
            W.add("gates", winv[:, :, 4096:4104], 16, 8)
            for hd in range(4):
                if full:
                    W.add("mq%d" % hd, winv[:, :, hd * 256:(hd + 1) * 256], 16, 256)
                W.add("mk%d" % hd, winv[:, :, 1024 + hd * 256:1024 + (hd + 1) * 256], 16, 256)
                W.add("mv%d" % hd, winv[:, :, 2048 + hd * 256:2048 + (hd + 1) * 256], 16, 256)
                if full:
                    W.add("mo%d" % hd, winv[:, :, 3072 + hd * 256:3072 + (hd + 1) * 256], 16, 256)
            if attn:
                for pr in range(4):
                    if full:
                        W.add("aq%d" % pr, winv[:, :, 4104 + pr * 256:4104 + (pr + 1) * 256], 16, 256)
                    W.add("ak%d" % pr, winv[:, :, 5128 + pr * 256:5128 + (pr + 1) * 256], 16, 256)
                    W.add("av%d" % pr, winv[:, :, 6152 + pr * 256:6152 + (pr + 1) * 256], 16, 256)
            if full and dbg != 2:
                for s_ in range(8):
                    W.add("wo%d" % s_, woutv[:, :, s_ * 256:(s_ + 1) * 256], 16, 256)
                plan_mlp(0)
                for s_ in range(8):
                    W.add("p1a%d" % s_, pw1v[:, :, s_ * 256:(s_ + 1) * 256], 16, 256)
                    W.add("p1g%d" % s_, pw1v[:, :, 2048 + s_ * 256:2048 + (s_ + 1) * 256], 16, 256)
                for s_ in range(8):
                    W.add("p2%d" % s_, pw2v[:, :, s_ * 256:(s_ + 1) * 256], 16, 256)
                plan_mlp(1)

        def plan_mlp(l):
            for g in range(8):
                for s4 in range(4):
                    c = g * 1024 + s4 * 256
                    W.add("w1_%d_%d_%d" % (l, g, s4), w1v[l][:, :, c:c + 256], 16, 256)
                for s4 in range(4):
                    W.add("w2_%d_%d_%d" % (l, g, s4), w2v[l][:, g * 8:(g + 1) * 8, s4 * 512:(s4 + 1) * 512], 8, 512)

        def ACT(out, in_, func, reads, writes, bias=None, scale=None, accum=None):
            kw = {}
            if bias is not None:
                kw["bias"] = bias
            if scale is not None:
                kw["scale"] = scale
            if accum is not None:
                kw["accum_out"] = accum
            S.op("act", lambda e: e.activation(out=out, in_=in_, func=func, **kw), reads, writes)

        def TT(out, in0, in1, op, reads, writes, eng="dve"):
            S.op(eng, lambda e: e.tensor_tensor(out=out, in0=in0, in1=in1, op=op), reads, writes)

        def TS(out, in0, s1, op0, reads, writes, s2=None, op1=None, eng="dve"):
            if op1 is None:
                S.op(eng, lambda e: e.tensor_scalar(out=out, in0=in0, scalar1=s1, scalar2=None, op0=op0), reads, writes)
            else:
                S.op(eng, lambda e: e.tensor_scalar(out=out, in0=in0, scalar1=s1, scalar2=s2, op0=op0, op1=op1), reads, writes)

        def STT(out, in0, scalar, in1, op0, op1, reads, writes):
            S.op("dve", lambda e: e.scalar_tensor_tensor(out=out, in0=in0, scalar=scalar, in1=in1, op0=op0, op1=op1), reads, writes)

        def RECIP(out, in_, reads, writes):
            S.op("dve", lambda e: e.reciprocal(out=out, in_=in_), reads, writes)

        def TRANS(ps, items, reads, f32=False):
            pv = ps.ap[:, 0:512] if f32 else ps.ap[:, 0:512].bitcast(BF16)
            idn = ident_f if f32 else ident_b
            fns = []
            for (c0, a, n) in items:
                fns.append(lambda pe, c0=c0, a=a, n=n: pe.transpose(pv[:, c0:c0 + n], a, idn.ap[:]))
            S.pe_group(fns, reads=list(reads) + [idn], writes=[ps])
            return pv

        def subs_from(c_lo):
            if c_lo == 0:
                return [(0, 512), (512, 512), (1024, 128)]
            return [(128, 512), (640, 512)]

        def norm_fm(src, sres, c0, w, gofs, dst, dres, dc0):
            if w <= 128:
                sq_ = sqh[nrot[0] % 2]
                st_ = (st1, st2)[nrot[0] % 2]
                nrot[0] += 1
            else:
                sq_, st_ = sq, st2
            ACT(sq_.ap[:, :, 0:w], src[:, :, c0:c0 + w], AF.Square, sres, [sq_])
            ps = S.rot("misc")
            S.mm(ps, [(ones_b.ap[:], sq_.ap[:, k, 0:w]) for k in range(16)], reads=[sq_, ones_b], out_ap=ps.ap[:, 0:w])
            ACT(st_.ap[:, 0:w], ps.ap[:, 0:w], AF.Sqrt, [ps, vec], [st_], bias=V(VO["eps"]), scale=1.0 / D)
            RECIP(st_.ap[:, 0:w], st_.ap[:, 0:w], [st_], [st_])
            for k in range(16):
                STT(dst[:, k, dc0:dc0 + w], src[:, k, c0:c0 + w], V(gofs + k), st_.ap[:, 0:w], ALU.mult, ALU.mult,
                    [sres[k] if len(sres) == 16 else sres[0], st_, vec], [dres[k] if len(dres) == 16 else dres[0]])

        def proj_fm_g(slot, sv, kc, nm, src, sreads, subs, evac, k0=0):
            for mi in range(nm):
                for (c0, w) in subs:
                    ps = S.rot("big")
                    S.mm(ps, [(sv[:, k, mi * 128:(mi + 1) * 128], src[:, k0 + k, c0:c0 + w]) for k in range(kc)],
                         reads=[slot] + list(sreads), out_ap=ps.ap[:, 0:w])
                    evac(mi, c0, w, ps)
                    yield
            W.prefetch()

        def proj_fm(*a_, **k_):
            for _ in proj_fm_g(*a_, **k_):
                pass

        def proj_tm_g(slot, sv, cols, ntt, evac):
            for tt in range(ntt):
                ps = S.rot("big")
                S.mm(ps, [(uT.ap[:, k, tt * 128:(tt + 1) * 128], sv[:, k, 0:cols]) for k in range(16)],
                     reads=[slot] + UP, out_ap=ps.ap[:, 0:cols])
                evac(tt, ps)
                yield
            W.prefetch()

        def load_xT(tok0, tt, dst, dres, dc0, xs_pool):
            xs = S.rot(xs_pool)
            S.dma("sp", xs.ap[:], x_d[tok0 + tt * 128: tok0 + (tt + 1) * 128, :], writes=[xs])
            for q4 in range(4):
                ps = S.rot("misc")
                TRANS(ps, [(j * 128, xs.ap[:, (q4 * 4 + j) * 128:(q4 * 4 + j + 1) * 128], 128) for j in range(4)], [xs], f32=True)
                o = dst[:, q4 * 4:(q4 + 1) * 4, dc0:dc0 + 128]
                i_ = ps.ap[:, 0:512].rearrange("p (a b) -> p a b", a=4)
                if q4 % 2 == 0:
                    ACT(o, i_, AF.Copy, [ps], dres[q4 * 4:(q4 + 1) * 4] if len(dres) == 16 else dres)
                else:
                    S.op("dve", lambda e, o=o, i_=i_: e.tensor_copy(out=o, in_=i_), [ps], dres[q4 * 4:(q4 + 1) * 4] if len(dres) == 16 else dres)

        def tile(tok0, ntt, full, attn, snap_tt, upd_last, flag_tts, is_A, out0, dbg_i=None):
            Tn = ntt * 128
            subs = [(c0, min(512, Tn - c0)) for c0 in range(0, Tn, 512)]
            S.barrier()
            ar = Arena(R_h, 16 * T * 4)
            xs_b = [ar.alloc([128, 2048], F32, "xs%d" % i) for i in range(2)]
            xT_b = [ar.alloc([128, 16, 128], F32, "xT%d" % i) for i in range(2)]
            S.pool_of("xs", xs_b)
            for tt in range(ntt):
                xT = xT_b[tt % 2]
                load_xT(tok0, tt, xT.ap, [xT], 0, "xs")
                norm_fm(xT.ap, [xT], 0, 128, VO["g_mix0"], uT.ap, UP, tt * 128)
            S.barrier()
            ar = Arena(R_h, 16 * T * 4)
            LF = ar.alloc([128, NT, 4], F32, "LF")
            Acol = ar.alloc([128, NT, 4], F32, "Acol")
            EMB = ar.alloc([128, NT, 4], F32, "EMB")
            EG = ar.alloc([128, NT, 4], F32, "EG")
            EG16 = ar.alloc([128, NT, 4], F32, "EG16")
            gt = ar.alloc([128, 16], F32, "gt")
            slot, sv = W.next("gates")
            for tt in range(ntt):
                ps = S.rot("misc")
                S.mm(ps, [(uT.ap[:, k, tt * 128:(tt + 1) * 128], sv[:, k, 0:8]) for k in range(16)],
                     reads=[slot] + UP, out_ap=ps.ap[:, 0:8])
                TT(gt.ap[:, 0:8], ps.ap[:, 0:8], vec.ap[:, VO["gate_b"]:VO["gate_b"] + 8], ALU.add, [ps, vec], [gt])
                ACT(gt.ap[:, 8:12], gt.ap[:, 4:8], AF.Exp, [gt], [gt], scale=-1.0)
                ACT(gt.ap[:, 8:12], gt.ap[:, 8:12], AF.Ln, [gt, vec], [gt], bias=V(VO["one"]), scale=1.0)
                TS(LF.ap[:, tt, :], gt.ap[:, 8:12], -1.0, ALU.mult, [gt], [LF])
                ps2 = S.rot("misc")
                S.pe_group([
                    lambda pe, ps2=ps2, tt=tt: pe.matmul(ps2.ap[:, 0:4], U_f.ap[:], LF.ap[:, tt, :], start=True, stop=True),
                    lambda pe, ps2=ps2, tt=tt: pe.matmul(ps2.ap[:, 4:8], ones_f.ap[:], LF.ap[:, tt, :], start=True, stop=True),
                ], reads=[U_f, ones_f, LF], writes=[ps2])
                TT(gt.ap[:, 12:16], gt.ap[:, 0:4], ps2.ap[:, 0:4], ALU.subtract, [gt, ps2], [gt])
                ACT(Acol.ap[:, tt, :], gt.ap[:, 12:16], AF.Exp, [gt], [Acol])
                if tt in flag_tts:
                    TS(Acol.ap[:, tt, :], Acol.ap[:, tt, :], V(VO["flag"]), ALU.mult, [Acol, vec], [Acol])
                ACT(EMB.ap[:, tt, :], ps2.ap[:, 0:4], AF.Exp, [ps2], [EMB], scale=-1.0)
                ACT(EG.ap[:, tt, :], ps2.ap[:, 4:8], AF.Exp, [ps2], [EG])
                ACT(EG16.ap[:, tt, :], ps2.ap[:, 4:8], AF.Exp, [ps2, vec], [EG16], bias=V(VO["nl16"]), scale=1.0)
            W.prefetch()
            ar_base = ar.off

            preq = ar.alloc([128, 2, T + 4], BF16, "preq")
            prek = ar.alloc([128, 2, T + 4], BF16, "prek")
            dblm = []
            for i_ in range(2):
                dblm.append(dict(
                    qT=ar.alloc([128, 2, T], BF16, "qT%d" % i_), kT=ar.alloc([128, 2, T], BF16, "kT%d" % i_),
                    kTM=ar.alloc([128, NT, 256], BF16, "kTM%d" % i_), va=ar.alloc([128, NT, 258], BF16, "va%d" % i_),
                    sigo=ar.alloc([128, NT, 256], BF16, "sigo%d" % i_)))
            hn = [ar.alloc([128, 256], BF16, "hn%d" % i) for i in range(2)]
            PTb = [ar.alloc([128, 128], BF16, "PT%d" % i) for i in range(2)]
            dg = [ar.alloc([128, 4, 128], BF16, "dg%d" % i) for i in range(2)]
            junk = ar.alloc([128, 256], BF16, "junk")

            def conv_silu_g(pre, fbase, dst):
                for mi in range(2):
                    f = fbase + mi
                    d_ = dg[mi % 2]
                    for j in range(4):
                        TS(d_.ap[:, j, :], ident_b.ap[:], V(VO["qk_cw"] + f * 4 + j), ALU.mult, [ident_b, vec], [d_])
                    for (c0, w) in subs:
                        ps = S.rot("big")
                        S.mm(ps, [(d_.ap[:, j, :], pre.ap[:, mi, c0 + j + 1:c0 + j + 1 + w]) for j in range(4)],
                             reads=[d_, pre], out_ap=ps.ap[:, 0:w])
                        ACT(dst.ap[:, mi, c0:c0 + w], ps.ap[:, 0:w], AF.Silu, [ps, vec], [dst], bias=V(VO["qk_cb"] + f), scale=1.0)
                        yield

            def proj_pre_g(tag, pre, fbase):
                slot, sv = W.next(tag)
                S.op("dve", lambda e: e.tensor_copy(out=pre.ap[:, :, 0:4], in_=prehalo.ap[:, fbase:fbase + 2, :]), [prehalo], [pre])

                def ev(mi, c0, w, ps):
                    ACT(pre.ap[:, mi, 4 + c0:4 + c0 + w], ps.ap[:, 0:w], AF.Copy, [ps], [pre])
                yield from proj_fm_g(slot, sv, 16, 2, uT.ap, UP, subs, ev)
                if snap_tt is not None:
                    st_ = (snap_tt + 1) * 128
                    S.op("dve", lambda e: e.tensor_copy(out=prehalo.ap[:, fbase:fbase + 2, :], in_=pre.ap[:, :, st_:st_ + 4]), [pre], [prehalo])

            def mlstm_A(hd):
                B_ = dblm[hd % 2]
                qT, kT, kTM, va, sigo = B_["qT"], B_["kT"], B_["kTM"], B_["va"], B_["sigo"]
                if full:
                    yield from proj_pre_g("mq%d" % hd, preq, hd * 2)
                    yield from conv_silu_g(preq, hd * 2, qT)
                yield from proj_pre_g("mk%d" % hd, prek, 8 + hd * 2)
                yield from conv_silu_g(prek, 8 + hd * 2, kT)
                for tt in range(ntt):
                    ps = S.rot("misc")
                    pv = TRANS(ps, [(kt * 128, kT.ap[:, kt, tt * 128:(tt + 1) * 128], 128) for kt in range(2)], [kT])
                    S.op("dve", lambda e, pv=pv, tt=tt: e.tensor_copy(out=kTM.ap[:, tt, :], in_=pv[:, 0:256]), [ps], [kTM])
                    if tt % 2 == 1:
                        yield
                slot, sv = W.next("mv%d" % hd)

                def ev_v(tt, ps):
                    ACT(va.ap[:, tt, 0:256], ps.ap[:, 0:256], AF.Copy, [ps, Acol], [va], scale=Acol.ap[:, tt, hd:hd + 1])
                    S.op("dve", lambda e: e.tensor_copy(out=va.ap[:, tt, 256:257], in_=Acol.ap[:, tt, hd:hd + 1]), [Acol], [va])
                yield from proj_tm_g(slot, sv, 256, ntt, ev_v)
                if full:
                    slot, sv = W.next("mo%d" % hd)

                    def ev_o(tt, ps):
                        ACT(sigo.ap[:, tt, :], ps.ap[:, 0:256], AF.Sigmoid, [ps], [sigo])
                    yield from proj_tm_g(slot, sv, 256, ntt, ev_o)

            def mlstm_B(hd):
                B_ = dblm[hd % 2]
                qT, kT, kTM, va, sigo = B_["qT"], B_["kT"], B_["kTM"], B_["va"], B_["sigo"]
                egprev = egp.ap[:, hd:hd + 1]
                egres = egp
                for tt in range(ntt):
                    tsl = slice(tt * 128, (tt + 1) * 128)
                    if full:
                        ps = S.rot("misc")
                        S.mm(ps, [(kT.ap[:, kt, tsl], qT.ap[:, kt, tsl]) for kt in range(2)], reads=[kT, qT], out_ap=ps.ap[:, 0:128])
                        PT = PTb[tt % 2]
                        TT(PT.ap[:], ps.ap[:, 0:128], mask16.ap[:], ALU.mult, [ps, mask16], [PT])
                        yield
                        psx = pxy
                        S.mm(psx, [(PT.ap[:], va.ap[:, tt, 0:257])] + [(qT.ap[:, kt, tsl], Cbf.ap[:, hd, kt, 0:257]) for kt in range(2)],
                             reads=[PT, va, qT, Cbf], out_ap=psx.ap[:, 0:257])
                    if tt <= upd_last:
                        S.pe_group([
                            lambda pe, kt=kt, tt=tt: pe.matmul(wide.ap[:, kt * 512:kt * 512 + 257], kTM.ap[:, tt, kt * 128:(kt + 1) * 128],
                                                               va.ap[:, tt, 0:257], start=True, stop=True)
                            for kt in range(2)], reads=[kTM, va], writes=[wide])
                        for kt in range(2):
                            STT(Tst.ap[:, hd, kt, :], Tst.ap[:, hd, kt, :], egprev, wide.ap[:, kt * 512:kt * 512 + 257],
                                ALU.mult, ALU.add, [Tst, wide, egres], [Tst])
                        if full or tt == upd_last:
                            for kt in range(2):
                                ACT(Cbf.ap[:, hd, kt, 0:257], Tst.ap[:, hd, kt, :], AF.Copy, [Tst, EG16], [Cbf], scale=EG16.ap[:, tt, hd:hd + 1])
                        egprev = EG.ap[:, tt, hd:hd + 1]
                        egres = EG
                    yield
                    if full:
                        ACT(sm.ap[:, 0:1], psx.ap[:, 256:257], AF.Abs, [psx], [sm])
                        TT(sm.ap[:, 0:1], sm.ap[:, 0:1], EMB.ap[:, tt, hd:hd + 1], ALU.max, [sm, EMB], [sm])
                        RECIP(sm.ap[:, 1:2], sm.ap[:, 0:1], [sm], [sm])
                        ACT(junk.ap[:], psx.ap[:, 0:256], AF.Square, [psx, sm], [junk, sm], scale=sm.ap[:, 1:2], accum=sm.ap[:, 2:3])
                        yield
                        ACT(sm.ap[:, 3:4], sm.ap[:, 2:3], AF.Sqrt, [sm, vec], [sm], bias=V(VO["eps"]), scale=1.0 / 256)
                        RECIP(sm.ap[:, 4:5], sm.ap[:, 3:4], [sm], [sm])
                        TT(sm.ap[:, 5:6], sm.ap[:, 4:5], sm.ap[:, 1:2], ALU.mult, [sm], [sm])
                        h_ = hn[tt % 2]
                        STT(h_.ap[:], psx.ap[:, 0:256], sm.ap[:, 5:6], sigo.ap[:, tt, :], ALU.mult, ALU.mult, [psx, sm, sigo], [h_])
                        yield
                        ps3 = S.rot("misc")
                        pv = TRANS(ps3, [(vt * 128, h_.ap[:, vt * 128:(vt + 1) * 128], 128) for vt in range(2)], [h_])
                        for vt in range(2):
                            ACT(mixT.ap[:, hd * 2 + vt, tsl], pv[:, vt * 128:(vt + 1) * 128], AF.Copy, [ps3, vec], [MP[hd * 2 + vt]],
                                scale=V(VO["ml_g"] + hd * 2 + vt))
                        yield
                if snap_tt is not None:
                    S.op("dve", lambda e: e.tensor_copy(out=egp.ap[:, hd:hd + 1], in_=EG.ap[:, snap_tt, hd:hd + 1]), [EG], [egp])

            def drive(*gens):
                gens = [g for g in gens if g is not None]
                while gens:
                    for g in list(gens):
                        try:
                            next(g)
                        except StopIteration:
                            gens.remove(g)

            drive(mlstm_A(0))
            for hd in range(4):
                drive(mlstm_B(hd), mlstm_A(hd + 1) if hd < 3 else None)

            S.barrier()
            if attn:
                ar.off = ar_base
                dbla = []
                for i_ in range(2):
                    dbla.append(dict(
                        qTb=[ar.alloc([128, T], BF16, "qTb%d_%d" % (i_, i)) for i in range(2)],
                        kbuf=[ar.alloc([128, 512 + T], BF16, "kbuf%d_%d" % (i_, i)) for i in range(2)],
                        vbuf=[ar.alloc([128, 4 + NT, 128], BF16, "vbuf%d_%d" % (i_, i)) for i in range(2)],
                        b2=[ar.alloc([128, 640], F32, "b2_%d_%d" % (i_, i)) for i in range(2)]))
                sc = [ar.alloc([128, 640], F32, "sc%d" % i) for i in range(2)]
                pex = [ar.alloc([128, 640], F32, "pex%d" % i) for i in range(2)]
                pn = [ar.alloc([128, 640], BF16, "pn%d" % i) for i in range(2)]
                pTt = [ar.alloc([128, 640], BF16, "pT%d" % i) for i in range(2)]
                asubs = subs if full else [s_ for s_ in subs if s_[0] >= 512]
                att0 = 0 if full else 4

                def attn_A(pr):
                    B_ = dbla[pr % 2]
                    qTb, kbuf, vbuf, b2 = B_["qTb"], B_["kbuf"], B_["vbuf"], B_["b2"]
                    for hh in range(2):
                        h = pr * 2 + hh
                        S.op("dve", lambda e, hh=hh, h=h: e.tensor_copy(out=kbuf[hh].ap[:, 0:512], in_=KVk.ap[:, h, :]), [KVk], [kbuf[hh]])
                        S.op("dve", lambda e, hh=hh, h=h: e.tensor_copy(out=vbuf[hh].ap[:, 0:4, :], in_=KVv.ap[:, h, :, :]), [KVv], [vbuf[hh]])
                        if full:
                            S.dma("sp", b2[hh].ap[:], b2_d[h], writes=[b2[hh]])
                    if full:
                        slot, sv = W.next("aq%d" % pr)

                        def ev_q(mi, c0, w, ps):
                            ACT(qTb[mi].ap[:, c0:c0 + w], ps.ap[:, 0:w], AF.Copy, [ps], [qTb[mi]], scale=float(128 ** -0.5))
                        yield from proj_fm_g(slot, sv, 16, 2, uT.ap, UP, subs, ev_q)
                    slot, sv = W.next("ak%d" % pr)

                    def ev_k(mi, c0, w, ps):
                        S.op("dve", lambda e: e.tensor_copy(out=kbuf[mi].ap[:, 512 + c0:512 + c0 + w], in_=ps.ap[:, 0:w]), [ps], [kbuf[mi]])
                    yield from proj_fm_g(slot, sv, 16, 2, uT.ap, UP, asubs, ev_k)
                    slot, sv = W.next("av%d" % pr)
                    for tt in range(att0, ntt):
                        ps = S.rot("big")
                        S.mm(ps, [(uT.ap[:, k, tt * 128:(tt + 1) * 128], sv[:, k, 0:256]) for k in range(16)],
                             reads=[slot] + UP, out_ap=ps.ap[:, 0:256])
                        for hh in range(2):
                            ACT(vbuf[hh].ap[:, 4 + tt, :], ps.ap[:, hh * 128:(hh + 1) * 128], AF.Copy, [ps], [vbuf[hh]])
                        yield
                    W.prefetch()

                def attn_B(pr):
                    B_ = dbla[pr % 2]
                    qTb, kbuf, vbuf, b2 = B_["qTb"], B_["kbuf"], B_["vbuf"], B_["b2"]
                    if full:
                        def unit(u, hh):
                            h = pr * 2 + hh
                            i2 = (u * 2 + hh) % 2
                            q_ = qTb[hh].ap[:, u * 128:(u + 1) * 128]
                            S.pe_group([
                                lambda pe: pe.matmul(wide.ap[:, 0:512], q_, kbuf[hh].ap[:, u * 128:u * 128 + 512], start=True, stop=True),
                                lambda pe: pe.matmul(wide.ap[:, 512:640], q_, kbuf[hh].ap[:, u * 128 + 512:u * 128 + 640], start=True, stop=True),
                            ], reads=[qTb[hh], kbuf[hh]], writes=[wide])
                            s_ = sc[i2]
                            TT(s_.ap[:], wide.ap[:, 0:640], b2[hh].ap[:], ALU.add, [wide, b2[hh]], [s_])
                            if is_A and u <= 4:
                                nm_ = 640 - u * 128
                                TS(s_.ap[:, 0:nm_], s_.ap[:, 0:nm_], V(VO["maskc"]), ALU.add, [s_, vec], [s_])
                            yield
                            m_ = S.rot("sm2")
                            S.op("dve", lambda e: e.tensor_reduce(out=m_.ap[:, 0:1], in_=s_.ap[:], axis=AX.X, op=ALU.max, negate=True), [s_], [m_])
                            p_ = pex[i2]
                            ACT(p_.ap[:], s_.ap[:], AF.Exp, [s_, m_], [p_, m_], bias=m_.ap[:, 0:1], scale=1.0, accum=m_.ap[:, 1:2])
                            yield
                            RECIP(m_.ap[:, 2:3], m_.ap[:, 1:2], [m_], [m_])
                            n_ = pn[i2]
                            TS(n_.ap[:], p_.ap[:], m_.ap[:, 2:3], ALU.mult, [p_, m_], [n_])
                            yield
                            psT = S.rot("misc")
                            pv = TRANS(psT, [(j * 128, n_.ap[:, j * 128:(j + 1) * 128], 128) for j in range(5)], [n_])
                            t_ = pTt[i2]
                            ACT(t_.ap[:], pv[:, 0:640], AF.Copy, [psT], [t_])
                            yield
                            pso = S.rot("misc")
                            S.mm(pso, [(vbuf[hh].ap[:, u + j, :], t_.ap[:, j * 128:(j + 1) * 128]) for j in range(5)],
                                 reads=[vbuf[hh], t_], out_ap=pso.ap[:, 0:128])
                            S.op("dve", lambda e: e.tensor_copy(out=mixT.ap[:, 8 + h, u * 128:(u + 1) * 128], in_=pso.ap[:, 0:128]), [pso], [MP[8 + h]])
                            yield
                        units = [unit(u, hh) for u in range(ntt) for hh in range(2)]
                        active = []
                        it = iter(units)
                        while True:
                            while len(active) < 2:
                                g = next(it, None)
                                if g is None:
                                    break
                                active.append(g)
                            if not active:
                                break
                            for g in list(active):
                                try:
                                    next(g)
                                except StopIteration:
                                    active.remove(g)
                            yield
                    if snap_tt is not None:
                        sk = (snap_tt + 1) * 128
                        for hh in range(2):
                            h = pr * 2 + hh
                            S.op("dve", lambda e, hh=hh, h=h: e.tensor_copy(out=KVk.ap[:, h, :], in_=kbuf[hh].ap[:, sk:sk + 512]), [kbuf[hh]], [KVk])
                            S.op("dve", lambda e, hh=hh, h=h: e.tensor_copy(out=KVv.ap[:, h, :, :], in_=vbuf[hh].ap[:, snap_tt + 1:snap_tt + 5, :]), [vbuf[hh]], [KVv])

                drive(attn_A(0))
                for pr in range(4):
                    drive(attn_B(pr), attn_A(pr + 1) if pr < 3 else None)

            if not full:
                return
            S.barrier()
            if dbg == 2:
                S.dma("pool", dbg_d[dbg_i], mixT.ap[:], reads=MP, final=True, semres=mixT)
                return
            aru = Arena(R_u, 8 * T * 4)
            xs2 = [aru.alloc([128, 2048], F32, "xsb%d" % i) for i in range(2)]
            S.pool_of("xs2", xs2)
            for tt in range(ntt):
                load_xT(tok0, tt, hT.ap, HP, tt * 128, "xs2")
            for s_ in range(8):
                slot, sv = W.next("wo%d" % s_)

                def ev_o2(mi, c0, w, ps, s_=s_):
                    d_ = s_ * 2 + mi
                    TT(hT.ap[:, d_, c0:c0 + w], ps.ap[:, 0:w], hT.ap[:, d_, c0:c0 + w], ALU.add, [ps, HP[d_]], [HP[d_]])
                proj_fm(slot, sv, 16, 2, mixT.ap, MP, subs, ev_o2)
            S.barrier()
            if dbg_i is not None and dbg_d is not None:
                S.dma("sp", dbg_d[dbg_i], hT.ap[:], reads=HP, final=True, semres=hT)
            mlp(0, 0)
            for c0 in range(0, T, 256):
                w = min(256, T - c0)
                norm_fm(hT.ap, HP, c0, w, VO["g_mix1"], uT.ap, UP, c0)
            S.barrier()
            zT = mixT
            for s_ in range(8):
                slotA, svA = W.next("p1a%d" % s_)
                slotG, svG = W.next("p1g%d" % s_)
                for mi in range(2):
                    f = s_ * 2 + mi
                    for (c0, w) in subs:
                        psA = S.rot("big")
                        S.mm(psA, [(svA[:, k, mi * 128:(mi + 1) * 128], uT.ap[:, k, c0:c0 + w]) for k in range(16)], reads=[slotA] + UP, out_ap=psA.ap[:, 0:w])
                        psG = S.rot("big")
                        S.mm(psG, [(svG[:, k, mi * 128:(mi + 1) * 128], uT.ap[:, k, c0:c0 + w]) for k in range(16)], reads=[slotG] + UP, out_ap=psG.ap[:, 0:w])
                        sg = S.rot("rl")
                        ACT(sg.ap[:, 0:w], psG.ap[:, 0:w], AF.Sigmoid, [psG, vec], [sg], bias=V(VO["pw1_b"] + 16 + f), scale=1.0)
                        STT(zT.ap[:, f, c0:c0 + w], psA.ap[:, 0:w], V(VO["pw1_b"] + f), sg.ap[:, 0:w], ALU.add, ALU.mult, [psA, sg, vec], [MP[f]])
                W.prefetch()
            if is_A:
                TS(zT.ap[:, :, 96:128], zT.ap[:, :, 96:128], V(VO["flag"]), ALU.mult, MP + [vec], MP)
            S.barrier()
            yT = uT
            csubs = subs_from(128)
            for f in range(16):
                for j in range(31):
                    dgx, jj = (dgA, j) if j < 16 else (dgB, j - 16)
                    if j % 2 == 0:
                        TS(dgx.ap[:, jj, :], ident_b.ap[:], V(VO["dw_w"] + f * 31 + j), ALU.mult, [ident_b, vec], [dgx])
                    else:
                        ACT(dgx.ap[:, jj, :], ident_b.ap[:], AF.Copy, [ident_b, vec], [dgx], scale=V(VO["dw_w"] + f * 31 + j))
                pss = [S.rot("big") for _ in csubs]
                for half in range(2):
                    jr = range(0, 16) if half == 0 else range(16, 31)
                    dgx = dgA if half == 0 else dgB
                    for si, (c0, w) in enumerate(csubs):
                        ps = pss[si]
                        S.pe_group([
                            (lambda pe, j=j, ps=ps, c0=c0, w=w, dgx=dgx: pe.matmul(
                                ps.ap[:, 0:w], dgx.ap[:, j if j < 16 else j - 16, :], zT.ap[:, f, c0 - 30 + j:c0 - 30 + j + w],
                                start=(j == 0), stop=(j == 30)))
                            for j in jr], reads=[dgx, MP[f]], writes=[ps])
                for si, (c0, w) in enumerate(csubs):
                    ps = pss[si]
                    ACT(yT.ap[:, f, c0:c0 + w], ps.ap[:, 0:w], AF.Identity, [ps, vec], [UP[f]], bias=V(VO["dw_b"] + f), scale=1.0)
            S.barrier()
            sT = mixT
            for c0 in range(128, T, 256):
                w = 256
                ACT(sq.ap[:, :, 0:w], yT.ap[:, :, c0:c0 + w], AF.Square, UP, [sq])
                psm = S.rot("misc")
                S.mm(psm, [(ones_b.ap[:], yT.ap[:, k, c0:c0 + w]) for k in range(16)], reads=UP + [ones_b], out_ap=psm.ap[:, 0:w])
                psq = S.rot("misc")
                S.mm(psq, [(ones_b.ap[:], sq.ap[:, k, 0:w]) for k in range(16)], reads=[sq, ones_b], out_ap=psq.ap[:, 0:w])
                TS(st1.ap[:, 0:w], psm.ap[:, 0:w], 1.0 / D, ALU.mult, [psm], [st1])
                TT(st2.ap[:, 0:w], st1.ap[:, 0:w], st1.ap[:, 0:w], ALU.mult, [st1], [st2])
                STT(st2.ap[:, 0:w], psq.ap[:, 0:w], 1.0 / D, st2.ap[:, 0:w], ALU.mult, ALU.subtract, [psq, st2], [st2])
                ACT(st2.ap[:, 0:w], st2.ap[:, 0:w], AF.Sqrt, [st2, vec], [st2], bias=V(VO["eps"]), scale=1.0)
                RECIP(st2.ap[:, 0:w], st2.ap[:, 0:w], [st2], [st2])
                for f in range(16):
                    t_ = S.rot("rl")
                    TT(t_.ap[:, 0:w], yT.ap[:, f, c0:c0 + w], st1.ap[:, 0:w], ALU.subtract, [UP[f], st1], [t_])
                    TT(t_.ap[:, 0:w], t_.ap[:, 0:w], st2.ap[:, 0:w], ALU.mult, [t_, st2], [t_])
                    ACT(sT.ap[:, f, c0:c0 + w], t_.ap[:, 0:w], AF.Silu, [t_, vec], [MP[f]], bias=V(VO["ln_b"] + f), scale=V(VO["ln_g"] + f))
            S.barrier()
            for s_ in range(8):
                slot, sv = W.next("p2%d" % s_)

                def ev_p2(mi, c0, w, ps, s_=s_):
                    d_ = s_ * 2 + mi
                    STT(hT.ap[:, d_, c0:c0 + w], ps.ap[:, 0:w], V(VO["pw2_b"] + d_), hT.ap[:, d_, c0:c0 + w], ALU.add, ALU.add, [ps, HP[d_], vec], [HP[d_]])
                proj_fm(slot, sv, 16, 2, sT.ap, MP, csubs, ev_p2)
            S.barrier()
            mlp(1, 128)
            S.barrier()
            aru = Arena(R_u, 8 * T * 4)
            oT = [aru.alloc([128, 16, 128], F32, "oT%d" % i) for i in range(2)]
            ob = [aru.alloc([128, 2048], F32, "ob%d" % i) for i in range(2)]
            for tt in range(1, ntt):
                o_ = oT[tt % 2]
                norm_fm(hT.ap, HP, tt * 128, 128, VO["g_fin"], o_.ap, [o_], 0)
                b_ = ob[tt % 2]
                for q4 in range(4):
                    ps = S.rot("misc")
                    TRANS(ps, [(j * 128, o_.ap[:, q4 * 4 + j, :], 128) for j in range(4)], [o_], f32=True)
                    if q4 % 2 == 0:
                        ACT(b_.ap[:, q4 * 512:(q4 + 1) * 512], ps.ap[:, 0:512], AF.Copy, [ps], [b_])
                    else:
                        S.op("dve", lambda e, b_=b_, ps=ps, q4=q4: e.tensor_copy(out=b_.ap[:, q4 * 512:(q4 + 1) * 512], in_=ps.ap[:, 0:512]), [ps], [b_])
                r0 = out0 + (tt - 1) * 128
                S.dma("sp", out_d[r0:r0 + 128, :], b_.ap[:], reads=[b_], final=True)

        def mlp(l, c_lo):
            msubs = subs_from(c_lo)
            gofs = VO["g_mlp0"] if l == 0 else VO["g_mlp1"]
            for c0 in range(c_lo, T, 256):
                norm_fm(hT.ap, HP, c0, min(256, T - c0), gofs, uT.ap, UP, c0)
            S.barrier()
            hid = [view(R_m, 0, [128, 8, T], BF16, "hid0"), view(R_m, 8 * T * 2, [128, 8, T], BF16, "hid1")]
            for g in range(8):
                hb = hid[g % 2]
                for s4 in range(4):
                    slot, sv = W.next("w1_%d_%d_%d" % (l, g, s4))

                    def ev1(mi, c0, w, ps, s4=s4, hb=hb):
                        r_ = S.rot("rl")
                        ACT(r_.ap[:, 0:w], ps.ap[:, 0:w], AF.Relu, [ps], [r_])
                        TT(hb.ap[:, s4 * 2 + mi, c0:c0 + w], ps.ap[:, 0:w], r_.ap[:, 0:w], ALU.mult, [ps, r_], [hb])
                    proj_fm(slot, sv, 16, 2, uT.ap, UP, msubs, ev1)
                for s4 in range(4):
                    slot, sv = W.next("w2_%d_%d_%d" % (l, g, s4))

                    def ev2(mi, c0, w, ps, s4=s4):
                        d_ = s4 * 4 + mi
                        TT(hT.ap[:, d_, c0:c0 + w], ps.ap[:, 0:w], hT.ap[:, d_, c0:c0 + w], ALU.add, [ps, HP[d_]], [HP[d_]])
                    proj_fm(slot, sv, 8, 4, hb.ap, [hb], msubs, ev2)

        plan_tile(False, False)
        plan_tile(False, True)
        plan_tile(True, True)
        if dbg != 2:
            plan_tile(True, True)
        tile(0, 6, False, False, 5, 5, set(range(6)), False, None)
        tile(768, 9, False, True, 8, 8, set(range(9)), False, None)
        tile(1920, 9, True, True, 7, 7, {0}, True, 0, dbg_i=0)
        if dbg != 2:
            tile(2944, 9, True, True, None, 7, set(), False, 1024, dbg_i=1)
        S.barrier(("sp",))
        S.finish()
        assert W.taken == len(W.plan), (W.taken, len(W.plan))
    return nc


def _host_consts():
    cm = np.zeros((128, 4, 128), np.float32)
    cm[:, 0, :] = np.eye(128, dtype=np.float32)
    cm[:, 1, :] = 1.0
    u = np.triu(np.ones((128, 128), np.float32))
    cm[:, 2, :] = u
    cm[:, 3, :] = u * (1.0 / 16.0)
    return cm


def _bias2(rel_bias):
    qi = np.arange(128)
    kj = np.arange(640)
    qc = qi // 64
    kc = kj // 64
    qpos = qi
    kpos = (kc - 8) * 64 + (kj % 64)
    dist = np.clip(qpos[:, None] - kpos[None, :], -256, 256) + 256
    vis = (kc[None, :] >= qc[:, None]) & (kc[None, :] <= qc[:, None] + 8)
    out = np.empty((8, 128, 640), np.float32)
    for h in range(8):
        out[h] = np.where(vis, rel_bias[h][dist], np.float32(NEG))
    return out


_NC_CACHE = {}


def kernel(**inputs):
    dbg = inputs.pop("_dbg", False)
    x = np.asarray(inputs["x"], np.float32)
    f = lambda n: np.asarray(inputs[n], np.float32)
    vecs = np.zeros((128, NV), np.float32)

    def put(name, arr):
        arr = np.asarray(arr, np.float32)
        vecs[:, VO[name]:VO[name] + arr.shape[1]] = arr

    put("g_mix0", _fm(f("mixer_norm_g")[0]))
    put("g_mix1", _fm(f("mixer_norm_g")[1]))
    put("g_mlp0", _fm(f("mlp_norm_g")[0]))
    put("g_mlp1", _fm(f("mlp_norm_g")[1]))
    put("g_fin", _fm(f("final_norm_g")))
    put("qk_cb", _fm(f("qk_conv_b")[0]))
    cw = f("qk_conv_w")[0]
    put("qk_cw", np.ascontiguousarray(cw.T.reshape(16, 128, 4).transpose(1, 0, 2)).reshape(128, 64))
    put("ml_g", _fm(f("mlstm_norm_g")[0]))
    put("pw1_b", _fm(f("conv_pw1_b")[0]))
    dw = f("conv_dw_w")[0]
    put("dw_w", np.ascontiguousarray(dw.T.reshape(16, 128, 31).transpose(1, 0, 2)).reshape(128, 496))
    put("dw_b", _fm(f("conv_dw_b")[0]))
    put("ln_g", _fm(f("conv_ln_g")[0]))
    put("ln_b", _fm(f("conv_ln_b")[0]))
    put("pw2_b", _fm(f("conv_pw2_b")[0]))
    gb = np.concatenate([f("igate_b")[0], f("fgate_b")[0]])[None, :]
    put("gate_b", np.broadcast_to(gb, (128, 8)))
    vecs[:, VO["eps"]] = EPS
    vecs[:, VO["one"]] = 1.0
    vecs[:, VO["nl16"]] = -np.log(16.0)
    vecs[:, VO["zero"]] = 0.0
    cm = _host_consts()
    b2 = _bias2(f("rel_bias")[0])
    w_in = np.ascontiguousarray(f("mix_w_in")[0])
    w_out = np.ascontiguousarray(f("mix_w_out")[0])
    pw1 = np.ascontiguousarray(f("conv_pw1_w")[0])
    pw2 = np.ascontiguousarray(f("conv_pw2_w")[0])
    w1 = np.ascontiguousarray(f("mlp_w1"))
    w2 = np.ascontiguousarray(f("mlp_w2"))
    in_maps = []
    for c in range(8):
        b, half = c // 2, c % 2
        xe = np.zeros((VS, D), np.float32)
        if half == 0:
            xe[2048:] = x[b, :2048]
        else:
            xe[:] = x[b]
        v = vecs.copy()
        v[:, VO["flag"]] = float(half)
        v[:, VO["maskc"]] = 0.0 if half == 1 else NEG
        in_maps.append({"x_ext": xe, "vecs": v, "cmat": cm, "bias2": b2, "w_in": w_in, "w_out": w_out,
                        "pw1": pw1, "pw2": pw2, "w1": w1, "w2": w2})
    key = dbg
    if key not in _NC_CACHE:
        _NC_CACHE[key] = build_program(dbg)
    nc = _NC_CACHE[key]
    res = run_bass_kernel_spmd(nc, in_maps, core_ids=list(range(8)))
    out = np.empty((4, 4096, D), np.float32)
    for c in range(8):
        b, half = c // 2, c % 2
        out[b, half * 2048:(half + 1) * 2048] = res.results[c]["out"]
    if dbg:
        return out, [res.results[c]["dbg"] for c in range(8)]
    return out
```

```python
import numpy as np
from contextlib import ExitStack
import concourse.bass as bass
import concourse.mybir as mybir
from concourse.bass_utils import run_bass_kernel_spmd

F32 = mybir.dt.float32
BF16 = mybir.dt.bfloat16
AF = mybir.ActivationFunctionType
ALU = mybir.AluOpType
AX = mybir.AxisListType


class Res:
    __slots__ = ("name", "w", "r", "dsem")

    def __init__(self, name):
        self.name = name
        self.w = None
        self.r = {}
        self.dsem = None


class Buf(Res):
    __slots__ = ("ap", "parts", "nbytes")

    def __init__(self, name, ap):
        super().__init__(name)
        self.ap = ap
        self.parts = {}

    def part(self, key):
        p = self.parts.get(key)
        if p is None:
            p = Res("%s/%s" % (self.name, key))
            self.parts[key] = p
        return p


class Sched:
    ENG = ("pe", "act", "dve", "pool", "sp")

    def __init__(self, nc, es):
        self.nc = nc
        self.es = es
        self.eng = {"pe": nc.tensor, "act": nc.scalar, "dve": nc.vector, "pool": nc.gpsimd, "sp": nc.sync}
        self.sem = {}
        self.cnt = {}
        for e in ("pe", "act", "dve", "pool"):
            self.sem[e] = es.enter_context(nc.semaphore("s_" + e))
            self.cnt[e] = 0
        self.known = {e: {} for e in self.ENG}
        self.dsems = {}
        self.finals = []
        self.ninst = 0
        self._rot = {}

    def sb(self, name, shape, dtype):
        t = self.es.enter_context(self.nc.sbuf_tensor(name, list(shape), dtype))
        return Buf(name, t)

    def psum(self, name, shape, dtype=F32):
        t = self.es.enter_context(self.nc.psum_tensor(name, list(shape), dtype))
        return Buf(name, t)

    def pool_of(self, name, bufs):
        self._rot[name] = [bufs, 0]

    def rot(self, name):
        p = self._rot[name]
        b = p[0][p[1] % len(p[0])]
        p[1] += 1
        return b

    def _dsem(self, res):
        if res.dsem is None:
            nm = "d%d" % len(self.dsems)
            s = self.es.enter_context(self.nc.semaphore(nm))
            res.dsem = [nm, s, 0]
            self.dsems[nm] = res.dsem
        return res.dsem

    def _semof(self, key):
        return self.sem[key] if key in self.sem else self.dsems[key][1]

    def _waits(self, e, reads, writes):
        need = {}
        for r in reads:
            if r.w is not None:
                k, v = r.w
                if need.get(k, 0) < v:
                    need[k] = v
        for w in writes:
            if w.w is not None:
                k, v = w.w
                if need.get(k, 0) < v:
                    need[k] = v
            for k, v in w.r.items():
                if need.get(k, 0) < v:
                    need[k] = v
        kn = self.known[e]
        eng = self.eng[e]
        for k, v in need.items():
            if kn.get(k, 0) < v:
                eng.wait_ge(self._semof(k), v)
                kn[k] = v
                self.ninst += 1

    def _mark(self, key, val, reads, writes):
        for r in reads:
            if r.r.get(key, 0) < val:
                r.r[key] = val
        for w in writes:
            w.w = (key, val)
            w.r = {}

    def op(self, e, fn, reads=(), writes=()):
        self._waits(e, reads, writes)
        ins = fn(self.eng[e])
        self.cnt[e] += 1
        ins.then_inc(self.sem[e], 1)
        self._mark(e, self.cnt[e], reads, writes)
        self.ninst += 1
        return ins

    def pe_group(self, fns, reads=(), writes=()):
        self._waits("pe", reads, writes)
        ins = None
        for fn in fns:
            ins = fn(self.eng["pe"])
            self.ninst += 1
        self.cnt["pe"] += 1
        ins.then_inc(self.sem["pe"], 1)
        self._mark("pe", self.cnt["pe"], reads, writes)

    def mm(self, out, pairs, reads=(), out_ap=None):
        oap = out.ap[:] if out_ap is None else out_ap
        n = len(pairs)
        fns = []
        for i, (l, r) in enumerate(pairs):
            fns.append(lambda pe, l=l, r=r, i=i: pe.matmul(oap, l, r, start=(i == 0), stop=(i == n - 1)))
        self.pe_group(fns, reads=reads, writes=[out])

    def dma(self, q, out_ap, in_ap, reads=(), writes=(), final=False, semres=None):
        self._waits(q, reads, writes)
        sr = semres if semres is not None else (writes[0] if writes else reads[0])
        ds = self._dsem(sr)
        self.eng[q].dma_start(out=out_ap, in_=in_ap).then_inc(ds[1], 16)
        ds[2] += 16
        self._mark(ds[0], ds[2], reads, writes)
        self.ninst += 1
        if final:
            self.finals.append((ds[0], ds[2]))

    def finish(self):
        kn = self.known["sp"]
        for k, v in self.finals:
            if kn.get(k, 0) < v:
                self.eng["sp"].wait_ge(self._semof(k), v)
                kn[k] = v


    def barrier(self, engs=("pe", "act", "dve", "sp")):
        for e in engs:
            kn = self.known[e]
            for k in ("pe", "act", "dve", "pool"):
                v = self.cnt[k]
                if k != e and v > 0 and kn.get(k, 0) < v:
                    self.eng[e].wait_ge(self.sem[k], v)
                    kn[k] = v
            for nm, ds in self.dsems.items():
                if ds[2] > 0 and kn.get(nm, 0) < ds[2]:
                    self.eng[e].wait_ge(ds[1], ds[2])
                    kn[nm] = ds[2]


D = 2048
KC = 16
DFF = 8192
T = 1152
NT = 9
VS = 4096
DIN = 7176
EPS = 1e-6
NEG = -30000.0

VO = {}
_o = 0
for _n, _w in (("g_mix0", 16), ("g_mix1", 16), ("g_mlp0", 16), ("g_mlp1", 16), ("g_fin", 16),
               ("qk_cb", 16), ("qk_cw", 64), ("ml_g", 8), ("pw1_b", 32), ("dw_w", 496),
               ("dw_b", 16), ("ln_g", 16), ("ln_b", 16), ("pw2_b", 16), ("gate_b", 8),
               ("flag", 1), ("maskc", 1), ("eps", 1), ("one", 1), ("nl16", 1), ("zero", 1)):
    VO[_n] = _o
    _o += _w
NV = _o


def _fm(v):
    v = np.asarray(v, np.float32)
    return np.ascontiguousarray(v.reshape(-1, 128).T)


def run_interleaved(gens, depth):
    active = []
    it = iter(gens)
    while True:
        while len(active) < depth:
            g = next(it, None)
            if g is None:
                break
            active.append(g)
        if not active:
            break
        for g in list(active):
            try:
                next(g)
            except StopIteration:
                active.remove(g)


def build_program(dbg=False):
    nc = bass.Bass("TRN2", target_bir_lowering=False)

    def din(name, shape):
        return nc.dram_tensor(name, list(shape), F32, kind="ExternalInput").ap()

    x_d = din("x_ext", [VS, D])
    vec_d = din("vecs", [128, NV])
    cm_d = din("cmat", [128, 4, 128])
    b2_d = din("bias2", [8, 128, 640])
    win_d = din("w_in", [D, DIN])
    wout_d = din("w_out", [D, D])
    pw1_d = din("pw1", [D, 2 * D])
    pw2_d = din("pw2", [D, D])
    w1_d = din("w1", [2, D, DFF])
    w2_d = din("w2", [2, DFF, D])
    out_d = nc.dram_tensor("out", [2048, D], F32, kind="ExternalOutput").ap()
    dbg_d = None
    if dbg:
        dbg_d = nc.dram_tensor("dbg", [2, 128, 16, T], F32, kind="ExternalOutput").ap()

    winv = win_d.rearrange("(k p) n -> p k n", p=128)
    woutv = wout_d.rearrange("(k p) n -> p k n", p=128)
    pw1v = pw1_d.rearrange("(k p) n -> p k n", p=128)
    pw2v = pw2_d.rearrange("(k p) n -> p k n", p=128)
    w1v = [w1_d[l].rearrange("(k p) n -> p k n", p=128) for l in range(2)]
    w2v = [w2_d[l].rearrange("(k p) n -> p k n", p=128) for l in range(2)]

    es = ExitStack()
    with es:
        S = Sched(nc, es)
        vec = S.sb("vec", [128, NV], F32)
        ident_f = S.sb("ident_f", [128, 128], F32)
        ones_f = S.sb("ones_f", [128, 128], F32)
        U_f = S.sb("U_f", [128, 128], F32)
        mask16 = S.sb("mask16", [128, 128], F32)
        ident_b = S.sb("ident_b", [128, 128], BF16)
        ones_b = S.sb("ones_b", [128, 128], BF16)
        KVk = S.sb("KVk", [128, 8, 512], BF16)
        KVv = S.sb("KVv", [128, 8, 4, 128], BF16)
        Tst = S.sb("Tst", [128, 4, 2, 257], F32)
        Cbf = S.sb("Cbf", [128, 4, 2, 258], BF16)
        egp = S.sb("egp", [128, 4], F32)
        prehalo = S.sb("prehalo", [128, 16, 4], BF16)
        st1 = S.sb("st1", [128, 256], F32)
        st2 = S.sb("st2", [128, 256], F32)
        sm = S.sb("sm", [128, 8], F32)
        sm2 = [S.sb("sm2_%d" % i, [128, 4], F32) for i in range(3)]
        NSLOT = 2
        wslots = [S.sb("wslot%d" % i, [128, 4096], BF16) for i in range(NSLOT)]
        R_h = S.sb("R_h", [128, 16 * T], F32)
        R_u = S.sb("R_u", [128, 8 * T], F32)
        R_m = S.sb("R_m", [128, 8 * T], F32)
        R_s = S.sb("R_s", [128, 2048], F32)
        big = [S.psum("pbig%d" % i, [128, 512]) for i in range(3)]
        wide = S.psum("pwide", [128, 1024])
        misc = [S.psum("pmisc%d" % i, [128, 512]) for i in range(2)]
        pxy = S.psum("pxy", [128, 512])
        S.pool_of("big", big)
        S.pool_of("misc", misc)
        S.pool_of("sm2", sm2)

        def V(c):
            return vec.ap[:, c:c + 1]

        def view(reg, off, shape, dt, name):
            nel = 1
            for d_ in shape[1:]:
                nel *= d_
            if dt == BF16:
                assert off % 4 == 0
                words = (nel + 1) // 2
                a = reg.ap[:, off // 4: off // 4 + words].bitcast(BF16)[:, 0:nel]
                nb = words * 4
            else:
                a = reg.ap[:, off // 4: off // 4 + nel]
                nb = nel * 4
            if len(shape) == 3:
                a = a.rearrange("p (a b) -> p a b", a=shape[1])
            elif len(shape) == 4:
                a = a.rearrange("p (a b c) -> p a b c", a=shape[1], b=shape[2])
            b = Buf(name, a)
            b.nbytes = nb
            return b

        class Arena:
            def __init__(self, reg, size):
                self.reg, self.size, self.off = reg, size, 0

            def alloc(self, shape, dt, name):
                b = view(self.reg, self.off, shape, dt, name)
                self.off += (b.nbytes + 31) // 32 * 32
                assert self.off <= self.size, (name, self.off, self.size)
                return b

        hT = view(R_h, 0, [128, 16, T], F32, "hT")
        uT = view(R_u, 0, [128, 16, T], BF16, "uT")
        mixT = view(R_m, 0, [128, 16, T], BF16, "mixT")
        sq = view(R_s, 0, [128, 16, 256], BF16, "sq")
        rl = [view(R_s, 0, [128, 512], F32, "rl0"), view(R_s, 2048, [128, 512], F32, "rl1")]
        dgA = view(R_s, 0, [128, 16, 128], BF16, "dgA")
        dgB = view(R_s, 4096, [128, 15, 128], BF16, "dgB")
        sqh = [view(R_s, 0, [128, 16, 128], BF16, "sqh0"), view(R_s, 4096, [128, 16, 128], BF16, "sqh1")]
        nrot = [0]
        S.pool_of("rl", rl)

        def hp(k):
            return hT.part(k)

        HP = [hT.part(k) for k in range(16)]
        UP = [uT.part(k) for k in range(16)]
        MP = [mixT.part(k) for k in range(16)]

        S.dma("sp", vec.ap[:], vec_d[:, :], writes=[vec])
        S.dma("sp", ident_f.ap[:], cm_d[:, 0, :], writes=[ident_f])
        S.dma("sp", ones_f.ap[:], cm_d[:, 1, :], writes=[ones_f])
        S.dma("sp", U_f.ap[:], cm_d[:, 2, :], writes=[U_f])
        S.dma("sp", mask16.ap[:], cm_d[:, 3, :], writes=[mask16])
        S.dma("pool", ident_b.ap[:], cm_d[:, 0, :], writes=[ident_b])
        S.dma("pool", ones_b.ap[:], cm_d[:, 1, :], writes=[ones_b])
        S.op("dve", lambda e: e.memset(Tst.ap[:], 0.0), writes=[Tst])
        S.op("dve", lambda e: e.memset(Cbf.ap[:], 0.0), writes=[Cbf])
        S.op("dve", lambda e: e.memset(egp.ap[:], 1.0), writes=[egp])
        S.op("dve", lambda e: e.memset(prehalo.ap[:], 0.0), writes=[prehalo])
        S.op("dve", lambda e: e.memset(KVk.ap[:], 0.0), writes=[KVk])
        S.op("dve", lambda e: e.memset(KVv.ap[:], 0.0), writes=[KVv])

        class WStream:
            def __init__(self):
                self.plan = []
                self.issued = 0
                self.taken = 0

            def add(self, tag, ap, kc, cols):
                self.plan.append((tag, ap, kc, cols))

            def _issue(self):
                tag, ap, kc, cols = self.plan[self.issued]
                slot = wslots[self.issued % NSLOT]
                v = slot.ap[:, 0:kc * cols].rearrange("p (k c) -> p k c", k=kc)
                S.dma("pool", v, ap, writes=[slot])
                self.issued += 1

            def next(self, tag):
                while self.issued <= self.taken:
                    self._issue()
                ptag, ap, kc, cols = self.plan[self.taken]
                assert ptag == tag, (ptag, tag)
                slot = wslots[self.taken % NSLOT]
                v = slot.ap[:, 0:kc * cols].rearrange("p (k c) -> p k c", k=kc)
                self.taken += 1
                return slot, v

            def prefetch(self):
                while self.issued < min(len(self.plan), self.taken + NSLOT):
                    self._issue()

        W = WStream()

        def plan_tile(full, attn):
            W.add("gates", winv[:, :, 4096:4104], 16, 8)
            for hd in range(4):
                if full:
                    W.add("mq%d" % hd, winv[:, :, hd * 256:(hd + 1) * 256], 16, 256)
                W.add("mk%d" % hd, winv[:, :, 1024 + hd * 256:1024 + (hd + 1) * 256], 16, 256)
                W.add("mv%d" % hd, winv[:, :, 2048 + hd * 256:2048 + (hd + 1) * 256], 16, 256)
                if full:
                    W.add("mo%d" % hd, winv[:, :, 3072 + hd * 256:3072 + (hd + 1) * 256], 16, 256)
            if attn:
                for pr in range(4):
                    if full:
                        W.add("aq%d" % pr, winv[:, :, 4104 + pr * 256:4104 + (pr + 1) * 256], 16, 256)
                    W.add("ak%d" % pr, winv[:, :, 5128 + pr * 256:5128 + (pr + 1) * 256], 16, 256)
                    W.add("av%d" % pr, winv[:, :, 6152 + pr * 256:6152 + (pr + 1) * 256], 16, 256)
            if full and dbg != 2:
                for s_ in range(8):
                    W.add("wo%d" % s_, woutv[:, :, s_ * 256:(s_ + 1) * 256], 16, 256)
                plan_mlp(0)
                for s_ in range(8):
                    W.add("p1a%d" % s_, pw1v[:, :, s_ * 256:(s_ + 1) * 256], 16, 256)
                    W.add("p1g%d" % s_, pw1v[:, :, 2048 + s_ * 256:2048 + (s_ + 1) * 256], 16, 256)
                for s_ in range(8):
                    W.add("p2%d" % s_, pw2v[:, :, s_ * 256:(s_ + 1) * 256], 16, 256)
                plan_mlp(1)

        def plan_mlp(l):
            for g in range(8):
                for s4 in range(4):
                    c = g * 1024 + s4 * 256
                    W.add("w1_%d_%d_%d" % (l, g, s4), w1v[l][:, :, c:c + 256], 16, 256)
                for s4 in range(4):
                    W.add("w2_%d_%d_%d" % (l, g, s4), w2v[l][:, g * 8:(g + 1) * 8, s4 * 512:(s4 + 1) * 512], 8, 512)

        def ACT(out, in_, func, reads, writes, bias=None, scale=None, accum=None):
            kw = {}
            if bias is not None:
                kw["bias"] = bias
            if scale is not None:
                kw["scale"] = scale
            if accum is not None:
                kw["accum_out"] = accum
            S.op("act", lambda e: e.activation(out=out, in_=in_, func=func, **kw), reads, writes)

        def TT(out, in0, in1, op, reads, writes, eng="dve"):
            S.op(eng, lambda e: e.tensor_tensor(out=out, in0=in0, in1=in1, op=op), reads, writes)

        def TS(out, in0, s1, op0, reads, writes, s2=None, op1=None, eng="dve"):
            if op1 is None:
                S.op(eng, lambda e: e.tensor_scalar(out=out, in0=in0, scalar1=s1, scalar2=None, op0=op0), reads, writes)
            else:
                S.op(eng, lambda e: e.tensor_scalar(out=out, in0=in0, scalar1=s1, scalar2=s2, op0=op0, op1=op1), reads, writes)

        def STT(out, in0, scalar, in1, op0, op1, reads, writes):
            S.op("dve", lambda e: e.scalar_tensor_tensor(out=out, in0=in0, scalar=scalar, in1=in1, op0=op0, op1=op1), reads, writes)

        def RECIP(out, in_, reads, writes):
            S.op("dve", lambda e: e.reciprocal(out=out, in_=in_), reads, writes)

        def TRANS(ps, items, reads, f32=False):
            pv = ps.ap[:, 0:512] if f32 else ps.ap[:, 0:512].bitcast(BF16)
            idn = ident_f if f32 else ident_b
            fns = []
            for (c0, a, n) in items:
                fns.append(lambda pe, c0=c0, a=a, n=n: pe.transpose(pv[:, c0:c0 + n], a, idn.ap[:]))
            S.pe_group(fns, reads=list(reads) + [idn], writes=[ps])
            return pv

        def subs_from(c_lo):
            if c_lo == 0:
                return [(0, 512), (512, 512), (1024, 128)]
            return [(128, 512), (640, 512)]

        def norm_fm(src, sres, c0, w, gofs, dst, dres, dc0):
            if w <= 128:
                sq_ = sqh[nrot[0] % 2]
                st_ = (st1, st2)[nrot[0] % 2]
                nrot[0] += 1
            else:
                sq_, st_ = sq, st2
            ACT(sq_.ap[:, :, 0:w], src[:, :, c0:c0 + w], AF.Square, sres, [sq_])
            ps = S.rot("misc")
            S.mm(ps, [(ones_b.ap[:], sq_.ap[:, k, 0:w]) for k in range(16)], reads=[sq_, ones_b], out_ap=ps.ap[:, 0:w])
            ACT(st_.ap[:, 0:w], ps.ap[:, 0:w], AF.Sqrt, [ps, vec], [st_], bias=V(VO["eps"]), scale=1.0 / D)
            RECIP(st_.ap[:, 0:w], st_.ap[:, 0:w], [st_], [st_])
            for k in range(16):
                STT(dst[:, k, dc0:dc0 + w], src[:, k, c0:c0 + w], V(gofs + k), st_.ap[:, 0:w], ALU.mult, ALU.mult,
                    [sres[k] if len(sres) == 16 else sres[0], st_, vec], [dres[k] if len(dres) == 16 else dres[0]])

        def proj_fm_g(slot, sv, kc, nm, src, sreads, subs, evac, k0=0):
            for mi in range(nm):
                for (c0, w) in subs:
                    ps = S.rot("big")
                    S.mm(ps, [(sv[:, k, mi * 128:(mi + 1) * 128], src[:, k0 + k, c0:c0 + w]) for k in range(kc)],
                         reads=[slot] + list(sreads), out_ap=ps.ap[:, 0:w])
                    evac(mi, c0, w, ps)
                    yield
            W.prefetch()

        def proj_fm(*a_, **k_):
            for _ in proj_fm_g(*a_, **k_):
                pass

        def proj_tm_g(slot, sv, cols, ntt, evac):
            for tt in range(ntt):
                ps = S.rot("big")
                S.mm(ps, [(uT.ap[:, k, tt * 128:(tt + 1) * 128], sv[:, k, 0:cols]) for k in range(16)],
                     reads=[slot] + UP, out_ap=ps.ap[:, 0:cols])
                evac(tt, ps)
                yield
            W.prefetch()

        def load_xT(tok0, tt, dst, dres, dc0, xs_pool, all_act=False):
            xs = S.rot(xs_pool)
            S.dma("sp", xs.ap[:], x_d[tok0 + tt * 128: tok0 + (tt + 1) * 128, :], writes=[xs])
            for q4 in range(4):
                ps = S.rot("misc")
                TRANS(ps, [(j * 128, xs.ap[:, (q4 * 4 + j) * 128:(q4 * 4 + j + 1) * 128], 128) for j in range(4)], [xs], f32=True)
                o = dst[:, q4 * 4:(q4 + 1) * 4, dc0:dc0 + 128]
                i_ = ps.ap[:, 0:512].rearrange("p (a b) -> p a b", a=4)
                if all_act or q4 % 2 == 0:
                    ACT(o, i_, AF.Copy, [ps], dres[q4 * 4:(q4 + 1) * 4] if len(dres) == 16 else dres)
                else:
                    S.op("dve", lambda e, o=o, i_=i_: e.tensor_copy(out=o, in_=i_), [ps], dres[q4 * 4:(q4 + 1) * 4] if len(dres) == 16 else dres)

        def tile(tok0, ntt, full, attn, snap_tt, upd_last, flag_tts, is_A, out0, dbg_i=None):
            Tn = ntt * 128
            subs = [(c0, min(512, Tn - c0)) for c0 in range(0, Tn, 512)]
            S.barrier()
            ar = Arena(R_h, 16 * T * 4)
            xs_b = [ar.alloc([128, 2048], F32, "xs%d" % i) for i in range(2)]
            xT_b = [ar.alloc([128, 16, 128], F32, "xT%d" % i) for i in range(2)]
            S.pool_of("xs", xs_b)
            for tt in range(ntt):
                xT = xT_b[tt % 2]
                load_xT(tok0, tt, xT.ap, [xT], 0, "xs", all_act=True)
                norm_fm(xT.ap, [xT], 0, 128, VO["g_mix0"], uT.ap, UP, tt * 128)
            S.barrier()
            ar = Arena(R_h, 16 * T * 4)
            LF = ar.alloc([128, NT, 4], F32, "LF")
            Acol = ar.alloc([128, NT, 4], F32, "Acol")
            EMB = ar.alloc([128, NT, 4], F32, "EMB")
            EG = ar.alloc([128, NT, 4], F32, "EG")
            EG16 = ar.alloc([128, NT, 4], F32, "EG16")
            gt = ar.alloc([128, 16], F32, "gt")
            slot, sv = W.next("gates")
            for tt in range(ntt):
                ps = S.rot("misc")
                S.mm(ps, [(uT.ap[:, k, tt * 128:(tt + 1) * 128], sv[:, k, 0:8]) for k in range(16)],
                     reads=[slot] + UP, out_ap=ps.ap[:, 0:8])
                TT(gt.ap[:, 0:8], ps.ap[:, 0:8], vec.ap[:, VO["gate_b"]:VO["gate_b"] + 8], ALU.add, [ps, vec], [gt])
                ACT(gt.ap[:, 8:12], gt.ap[:, 4:8], AF.Exp, [gt], [gt], scale=-1.0)
                ACT(gt.ap[:, 8:12], gt.ap[:, 8:12], AF.Ln, [gt, vec], [gt], bias=V(VO["one"]), scale=1.0)
                TS(LF.ap[:, tt, :], gt.ap[:, 8:12], -1.0, ALU.mult, [gt], [LF])
                ps2 = S.rot("misc")
                S.pe_group([
                    lambda pe, ps2=ps2, tt=tt: pe.matmul(ps2.ap[:, 0:4], U_f.ap[:], LF.ap[:, tt, :], start=True, stop=True),
                    lambda pe, ps2=ps2, tt=tt: pe.matmul(ps2.ap[:, 4:8], ones_f.ap[:], LF.ap[:, tt, :], start=True, stop=True),
                ], reads=[U_f, ones_f, LF], writes=[ps2])
                TT(gt.ap[:, 12:16], gt.ap[:, 0:4], ps2.ap[:, 0:4], ALU.subtract, [gt, ps2], [gt])
                ACT(Acol.ap[:, tt, :], gt.ap[:, 12:16], AF.Exp, [gt], [Acol])
                if tt in flag_tts:
                    TS(Acol.ap[:, tt, :], Acol.ap[:, tt, :], V(VO["flag"]), ALU.mult, [Acol, vec], [Acol])
                ACT(EMB.ap[:, tt, :], ps2.ap[:, 0:4], AF.Exp, [ps2], [EMB], scale=-1.0)
                ACT(EG.ap[:, tt, :], ps2.ap[:, 4:8], AF.Exp, [ps2], [EG])
                ACT(EG16.ap[:, tt, :], ps2.ap[:, 4:8], AF.Exp, [ps2, vec], [EG16], bias=V(VO["nl16"]), scale=1.0)
            W.prefetch()
            ar_base = ar.off

            preq = ar.alloc([128, 2, T + 4], BF16, "preq")
            prek = ar.alloc([128, 2, T + 4], BF16, "prek")
            dblm = []
            for i_ in range(2):
                dblm.append(dict(
                    qT=ar.alloc([128, 2, T], BF16, "qT%d" % i_), kT=ar.alloc([128, 2, T], BF16, "kT%d" % i_),
                    kTM=ar.alloc([128, NT, 256], BF16, "kTM%d" % i_), va=ar.alloc([128, NT, 258], BF16, "va%d" % i_),
                    sigo=ar.alloc([128, NT, 256], BF16, "sigo%d" % i_)))
            hn = [ar.alloc([128, 256], BF16, "hn%d" % i) for i in range(2)]
            PTb = [ar.alloc([128, 128], BF16, "PT%d" % i) for i in range(2)]
            dg = [ar.alloc([128, 4, 128], BF16, "dg%d" % i) for i in range(2)]
            junk = ar.alloc([128, 256], BF16, "junk")

            def conv_silu_g(pre, fbase, dst):
                for mi in range(2):
                    f = fbase + mi
                    d_ = dg[mi % 2]
                    for j in range(4):
                        TS(d_.ap[:, j, :], ident_b.ap[:], V(VO["qk_cw"] + f * 4 + j), ALU.mult, [ident_b, vec], [d_])
                    for (c0, w) in subs:
                        ps = S.rot("big")
                        S.mm(ps, [(d_.ap[:, j, :], pre.ap[:, mi, c0 + j + 1:c0 + j + 1 + w]) for j in range(4)],
                             reads=[d_, pre], out_ap=ps.ap[:, 0:w])
                        ACT(dst.ap[:, mi, c0:c0 + w], ps.ap[:, 0:w], AF.Silu, [ps, vec], [dst], bias=V(VO["qk_cb"] + f), scale=1.0)
                        yield

            def proj_pre_g(tag, pre, fbase):
                slot, sv = W.next(tag)
                S.op("dve", lambda e: e.tensor_copy(out=pre.ap[:, :, 0:4], in_=prehalo.ap[:, fbase:fbase + 2, :]), [prehalo], [pre])

                def ev(mi, c0, w, ps):
                    ACT(pre.ap[:, mi, 4 + c0:4 + c0 + w], ps.ap[:, 0:w], AF.Copy, [ps], [pre])
                yield from proj_fm_g(slot, sv, 16, 2, uT.ap, UP, subs, ev)
                if snap_tt is not None:
                    st_ = (snap_tt + 1) * 128
                    S.op("dve", lambda e: e.tensor_copy(out=prehalo.ap[:, fbase:fbase + 2, :], in_=pre.ap[:, :, st_:st_ + 4]), [pre], [prehalo])

            def mlstm_A(hd):
                B_ = dblm[hd % 2]
                qT, kT, kTM, va, sigo = B_["qT"], B_["kT"], B_["kTM"], B_["va"], B_["sigo"]
                if full:
                    yield from proj_pre_g("mq%d" % hd, preq, hd * 2)
                    yield from conv_silu_g(preq, hd * 2, qT)
                yield from proj_pre_g("mk%d" % hd, prek, 8 + hd * 2)
                yield from conv_silu_g(prek, 8 + hd * 2, kT)
                for tt in range(ntt):
                    ps = S.rot("misc")
                    pv = TRANS(ps, [(kt * 128, kT.ap[:, kt, tt * 128:(tt + 1) * 128], 128) for kt in range(2)], [kT])
                    S.op("dve", lambda e, pv=pv, tt=tt: e.tensor_copy(out=kTM.ap[:, tt, :], in_=pv[:, 0:256]), [ps], [kTM])
                    if tt % 2 == 1:
                        yield
                slot, sv = W.next("mv%d" % hd)

                def ev_v(tt, ps):
                    ACT(va.ap[:, tt, 0:256], ps.ap[:, 0:256], AF.Copy, [ps, Acol], [va], scale=Acol.ap[:, tt, hd:hd + 1])
                    S.op("dve", lambda e: e.tensor_copy(out=va.ap[:, tt, 256:257], in_=Acol.ap[:, tt, hd:hd + 1]), [Acol], [va])
                yield from proj_tm_g(slot, sv, 256, ntt, ev_v)
                if full:
                    slot, sv = W.next("mo%d" % hd)

                    def ev_o(tt, ps):
                        ACT(sigo.ap[:, tt, :], ps.ap[:, 0:256], AF.Sigmoid, [ps], [sigo])
                    yield from proj_tm_g(slot, sv, 256, ntt, ev_o)

            def mlstm_B(hd):
                B_ = dblm[hd % 2]
                qT, kT, kTM, va, sigo = B_["qT"], B_["kT"], B_["kTM"], B_["va"], B_["sigo"]
                egprev = egp.ap[:, hd:hd + 1]
                egres = egp
                for tt in range(ntt):
                    tsl = slice(tt * 128, (tt + 1) * 128)
                    if full:
                        ps = S.rot("misc")
                        S.mm(ps, [(kT.ap[:, kt, tsl], qT.ap[:, kt, tsl]) for kt in range(2)], reads=[kT, qT], out_ap=ps.ap[:, 0:128])
                        PT = PTb[tt % 2]
                        TT(PT.ap[:], ps.ap[:, 0:128], mask16.ap[:], ALU.mult, [ps, mask16], [PT])
                        yield
                        psx = pxy
                        S.mm(psx, [(PT.ap[:], va.ap[:, tt, 0:257])] + [(qT.ap[:, kt, tsl], Cbf.ap[:, hd, kt, 0:257]) for kt in range(2)],
                             reads=[PT, va, qT, Cbf], out_ap=psx.ap[:, 0:257])
                    if tt <= upd_last:
                        S.pe_group([
                            lambda pe, kt=kt, tt=tt: pe.matmul(wide.ap[:, kt * 512:kt * 512 + 257], kTM.ap[:, tt, kt * 128:(kt + 1) * 128],
                                                               va.ap[:, tt, 0:257], start=True, stop=True)
                            for kt in range(2)], reads=[kTM, va], writes=[wide])
                        for kt in range(2):
                            STT(Tst.ap[:, hd, kt, :], Tst.ap[:, hd, kt, :], egprev, wide.ap[:, kt * 512:kt * 512 + 257],
                                ALU.mult, ALU.add, [Tst, wide, egres], [Tst])
                        if full or tt == upd_last:
                            for kt in range(2):
                                ACT(Cbf.ap[:, hd, kt, 0:257], Tst.ap[:, hd, kt, :], AF.Copy, [Tst, EG16], [Cbf], scale=EG16.ap[:, tt, hd:hd + 1])
                        egprev = EG.ap[:, tt, hd:hd + 1]
                        egres = EG
                    yield
                    if full:
                        ACT(sm.ap[:, 0:1], psx.ap[:, 256:257], AF.Abs, [psx], [sm])
                        TT(sm.ap[:, 0:1], sm.ap[:, 0:1], EMB.ap[:, tt, hd:hd + 1], ALU.max, [sm, EMB], [sm])
                        RECIP(sm.ap[:, 1:2], sm.ap[:, 0:1], [sm], [sm])
                        ACT(junk.ap[:], psx.ap[:, 0:256], AF.Square, [psx, sm], [junk, sm], scale=sm.ap[:, 1:2], accum=sm.ap[:, 2:3])
                        yield
                        ACT(sm.ap[:, 3:4], sm.ap[:, 2:3], AF.Sqrt, [sm, vec], [sm], bias=V(VO["eps"]), scale=1.0 / 256)
                        RECIP(sm.ap[:, 4:5], sm.ap[:, 3:4], [sm], [sm])
                        TT(sm.ap[:, 5:6], sm.ap[:, 4:5], sm.ap[:, 1:2], ALU.mult, [sm], [sm])
                        h_ = hn[tt % 2]
                        STT(h_.ap[:], psx.ap[:, 0:256], sm.ap[:, 5:6], sigo.ap[:, tt, :], ALU.mult, ALU.mult, [psx, sm, sigo], [h_])
                        yield
                        ps3 = S.rot("misc")
                        pv = TRANS(ps3, [(vt * 128, h_.ap[:, vt * 128:(vt + 1) * 128], 128) for vt in range(2)], [h_])
                        for vt in range(2):
                            ACT(mixT.ap[:, hd * 2 + vt, tsl], pv[:, vt * 128:(vt + 1) * 128], AF.Copy, [ps3, vec], [MP[hd * 2 + vt]],
                                scale=V(VO["ml_g"] + hd * 2 + vt))
                        yield
                if snap_tt is not None:
                    S.op("dve", lambda e: e.tensor_copy(out=egp.ap[:, hd:hd + 1], in_=EG.ap[:, snap_tt, hd:hd + 1]), [EG], [egp])

            def drive(*gens):
                gens = [g for g in gens if g is not None]
                while gens:
                    for g in list(gens):
                        try:
                            next(g)
                        except StopIteration:
                            gens.remove(g)

            drive(mlstm_A(0))
            for hd in range(4):
                drive(mlstm_B(hd), mlstm_A(hd + 1) if hd < 3 else None)

            S.barrier()
            if attn:
                ar.off = ar_base
                dbla = []
                for i_ in range(2):
                    dbla.append(dict(
                        qTb=[ar.alloc([128, T], BF16, "qTb%d_%d" % (i_, i)) for i in range(2)],
                        kbuf=[ar.alloc([128, 512 + T], BF16, "kbuf%d_%d" % (i_, i)) for i in range(2)],
                        vbuf=[ar.alloc([128, 4 + NT, 128], BF16, "vbuf%d_%d" % (i_, i)) for i in range(2)],
                        b2=[ar.alloc([128, 640], F32, "b2_%d_%d" % (i_, i)) for i in range(2)]))
                sc = [ar.alloc([128, 640], F32, "sc%d" % i) for i in range(2)]
                pex = [ar.alloc([128, 640], F32, "pex%d" % i) for i in range(2)]
                pn = [ar.alloc([128, 640], BF16, "pn%d" % i) for i in range(2)]
                pTt = [ar.alloc([128, 640], BF16, "pT%d" % i) for i in range(2)]
                asubs = subs if full else [s_ for s_ in subs if s_[0] >= 512]
                att0 = 0 if full else 4

                def attn_A(pr):
                    B_ = dbla[pr % 2]
                    qTb, kbuf, vbuf, b2 = B_["qTb"], B_["kbuf"], B_["vbuf"], B_["b2"]
                    for hh in range(2):
                        h = pr * 2 + hh
                        S.op("dve", lambda e, hh=hh, h=h: e.tensor_copy(out=kbuf[hh].ap[:, 0:512], in_=KVk.ap[:, h, :]), [KVk], [kbuf[hh]])
                        S.op("dve", lambda e, hh=hh, h=h: e.tensor_copy(out=vbuf[hh].ap[:, 0:4, :], in_=KVv.ap[:, h, :, :]), [KVv], [vbuf[hh]])
                        if full:
                            S.dma("sp", b2[hh].ap[:], b2_d[h], writes=[b2[hh]])
                    if full:
                        slot, sv = W.next("aq%d" % pr)

                        def ev_q(mi, c0, w, ps):
                            ACT(qTb[mi].ap[:, c0:c0 + w], ps.ap[:, 0:w], AF.Copy, [ps], [qTb[mi]], scale=float(128 ** -0.5))
                        yield from proj_fm_g(slot, sv, 16, 2, uT.ap, UP, subs, ev_q)
                    slot, sv = W.next("ak%d" % pr)

                    def ev_k(mi, c0, w, ps):
                        S.op("dve", lambda e: e.tensor_copy(out=kbuf[mi].ap[:, 512 + c0:512 + c0 + w], in_=ps.ap[:, 0:w]), [ps], [kbuf[mi]])
                    yield from proj_fm_g(slot, sv, 16, 2, uT.ap, UP, asubs, ev_k)
                    slot, sv = W.next("av%d" % pr)
                    for tt in range(att0, ntt):
                        ps = S.rot("big")
                        S.mm(ps, [(uT.ap[:, k, tt * 128:(tt + 1) * 128], sv[:, k, 0:256]) for k in range(16)],
                             reads=[slot] + UP, out_ap=ps.ap[:, 0:256])
                        for hh in range(2):
                            ACT(vbuf[hh].ap[:, 4 + tt, :], ps.ap[:, hh * 128:(hh + 1) * 128], AF.Copy, [ps], [vbuf[hh]])
                        yield
                    W.prefetch()

                def attn_B(pr):
                    B_ = dbla[pr % 2]
                    qTb, kbuf, vbuf, b2 = B_["qTb"], B_["kbuf"], B_["vbuf"], B_["b2"]
                    if full:
                        def unit(u, hh):
                            h = pr * 2 + hh
                            i2 = (u * 2 + hh) % 2
                            q_ = qTb[hh].ap[:, u * 128:(u + 1) * 128]
                            S.pe_group([
                                lambda pe: pe.matmul(wide.ap[:, 0:512], q_, kbuf[hh].ap[:, u * 128:u * 128 + 512], start=True, stop=True),
                                lambda pe: pe.matmul(wide.ap[:, 512:640], q_, kbuf[hh].ap[:, u * 128 + 512:u * 128 + 640], start=True, stop=True),
                            ], reads=[qTb[hh], kbuf[hh]], writes=[wide])
                            s_ = sc[i2]
                            TT(s_.ap[:], wide.ap[:, 0:640], b2[hh].ap[:], ALU.add, [wide, b2[hh]], [s_])
                            if is_A and u <= 4:
                                nm_ = 640 - u * 128
                                TS(s_.ap[:, 0:nm_], s_.ap[:, 0:nm_], V(VO["maskc"]), ALU.add, [s_, vec], [s_])
                            yield
                            m_ = S.rot("sm2")
                            S.op("dve", lambda e: e.tensor_reduce(out=m_.ap[:, 0:1], in_=s_.ap[:], axis=AX.X, op=ALU.max, negate=True), [s_], [m_])
                            p_ = pex[i2]
                            ACT(p_.ap[:], s_.ap[:], AF.Exp, [s_, m_], [p_, m_], bias=m_.ap[:, 0:1], scale=1.0, accum=m_.ap[:, 1:2])
                            yield
                            RECIP(m_.ap[:, 2:3], m_.ap[:, 1:2], [m_], [m_])
                            n_ = pn[i2]
                            TS(n_.ap[:], p_.ap[:], m_.ap[:, 2:3], ALU.mult, [p_, m_], [n_])
                            yield
                            psT = S.rot("misc")
                            pv = TRANS(psT, [(j * 128, n_.ap[:, j * 128:(j + 1) * 128], 128) for j in range(5)], [n_])
                            t_ = pTt[i2]
                            ACT(t_.ap[:], pv[:, 0:640], AF.Copy, [psT], [t_])
                            yield
                            pso = S.rot("misc")
                            S.mm(pso, [(vbuf[hh].ap[:, u + j, :], t_.ap[:, j * 128:(j + 1) * 128]) for j in range(5)],
                                 reads=[vbuf[hh], t_], out_ap=pso.ap[:, 0:128])
                            S.op("dve", lambda e: e.tensor_copy(out=mixT.ap[:, 8 + h, u * 128:(u + 1) * 128], in_=pso.ap[:, 0:128]), [pso], [MP[8 + h]])
                            yield
                        units = [unit(u, hh) for u in range(ntt) for hh in range(2)]
                        active = []
                        it = iter(units)
                        while True:
                            while len(active) < 2:
                                g = next(it, None)
                                if g is None:
                                    break
                                active.append(g)
                            if not active:
                                break
                            for g in list(active):
                                try:
                                    next(g)
                                except StopIteration:
                                    active.remove(g)
                            yield
                    if snap_tt is not None:
                        sk = (snap_tt + 1) * 128
                        for hh in range(2):
                            h = pr * 2 + hh
                            S.op("dve", lambda e, hh=hh, h=h: e.tensor_copy(out=KVk.ap[:, h, :], in_=kbuf[hh].ap[:, sk:sk + 512]), [kbuf[hh]], [KVk])
                            S.op("dve", lambda e, hh=hh, h=h: e.tensor_copy(out=KVv.ap[:, h, :, :], in_=vbuf[hh].ap[:, snap_tt + 1:snap_tt + 5, :]), [vbuf[hh]], [KVv])

                drive(attn_A(0))
                for pr in range(4):
                    drive(attn_B(pr), attn_A(pr + 1) if pr < 3 else None)

            if not full:
                return
            S.barrier()
            if dbg == 2:
                S.dma("pool", dbg_d[dbg_i], mixT.ap[:], reads=MP, final=True, semres=mixT)
                return
            aru = Arena(R_u, 8 * T * 4)
            xs2 = [aru.alloc([128, 2048], F32, "xsb%d" % i) for i in range(2)]
            S.pool_of("xs2", xs2)
            for tt in range(ntt):
                load_xT(tok0, tt, hT.ap, HP, tt * 128, "xs2")
            for s_ in range(8):
                slot, sv = W.next("wo%d" % s_)

                def ev_o2(mi, c0, w, ps, s_=s_):
                    d_ = s_ * 2 + mi
                    TT(hT.ap[:, d_, c0:c0 + w], ps.ap[:, 0:w], hT.ap[:, d_, c0:c0 + w], ALU.add, [ps, HP[d_]], [HP[d_]])
                proj_fm(slot, sv, 16, 2, mixT.ap, MP, subs, ev_o2)
            S.barrier()
            if dbg_i is not None and dbg_d is not None:
                S.dma("sp", dbg_d[dbg_i], hT.ap[:], reads=HP, final=True, semres=hT)
            mlp(0, 0)
            for c0 in range(0, T, 256):
                w = min(256, T - c0)
                norm_fm(hT.ap, HP, c0, w, VO["g_mix1"], uT.ap, UP, c0)
            S.barrier()
            zT = mixT
            for s_ in range(8):
                slotA, svA = W.next("p1a%d" % s_)
                slotG, svG = W.next("p1g%d" % s_)
                for mi in range(2):
                    f = s_ * 2 + mi
                    for (c0, w) in subs:
                        psA = S.rot("big")
                        S.mm(psA, [(svA[:, k, mi * 128:(mi + 1) * 128], uT.ap[:, k, c0:c0 + w]) for k in range(16)], reads=[slotA] + UP, out_ap=psA.ap[:, 0:w])
                        psG = S.rot("big")
                        S.mm(psG, [(svG[:, k, mi * 128:(mi + 1) * 128], uT.ap[:, k, c0:c0 + w]) for k in range(16)], reads=[slotG] + UP, out_ap=psG.ap[:, 0:w])
                        sg = S.rot("rl")
                        ACT(sg.ap[:, 0:w], psG.ap[:, 0:w], AF.Sigmoid, [psG, vec], [sg], bias=V(VO["pw1_b"] + 16 + f), scale=1.0)
                        STT(zT.ap[:, f, c0:c0 + w], psA.ap[:, 0:w], V(VO["pw1_b"] + f), sg.ap[:, 0:w], ALU.add, ALU.mult, [psA, sg, vec], [MP[f]])
                W.prefetch()
            if is_A:
                TS(zT.ap[:, :, 96:128], zT.ap[:, :, 96:128], V(VO["flag"]), ALU.mult, MP + [vec], MP)
            S.barrier()
            yT = uT
            csubs = subs_from(128)
            for f in range(16):
                for j in range(31):
                    dgx, jj = (dgA, j) if j < 16 else (dgB, j - 16)
                    if True:
                        TS(dgx.ap[:, jj, :], ident_b.ap[:], V(VO["dw_w"] + f * 31 + j), ALU.mult, [ident_b, vec], [dgx])
                    else:
                        ACT(dgx.ap[:, jj, :], ident_b.ap[:], AF.Copy, [ident_b, vec], [dgx], scale=V(VO["dw_w"] + f * 31 + j))
                pss = [S.rot("big") for _ in csubs]
                for half in range(2):
                    jr = range(0, 16) if half == 0 else range(16, 31)
                    dgx = dgA if half == 0 else dgB
                    for si, (c0, w) in enumerate(csubs):
                        ps = pss[si]
                        S.pe_group([
                            (lambda pe, j=j, ps=ps, c0=c0, w=w, dgx=dgx: pe.matmul(
                                ps.ap[:, 0:w], dgx.ap[:, j if j < 16 else j - 16, :], zT.ap[:, f, c0 - 30 + j:c0 - 30 + j + w],
                                start=(j == 0), stop=(j == 30)))
                            for j in jr], reads=[dgx, MP[f]], writes=[ps])
                for si, (c0, w) in enumerate(csubs):
                    ps = pss[si]
                    ACT(yT.ap[:, f, c0:c0 + w], ps.ap[:, 0:w], AF.Identity, [ps, vec], [UP[f]], bias=V(VO["dw_b"] + f), scale=1.0)
            S.barrier()
            sT = mixT
            for c0 in range(128, T, 256):
                w = 256
                ACT(sq.ap[:, :, 0:w], yT.ap[:, :, c0:c0 + w], AF.Square, UP, [sq])
                psm = S.rot("misc")
                S.mm(psm, [(ones_b.ap[:], yT.ap[:, k, c0:c0 + w]) for k in range(16)], reads=UP + [ones_b], out_ap=psm.ap[:, 0:w])
                psq = S.rot("misc")
                S.mm(psq, [(ones_b.ap[:], sq.ap[:, k, 0:w]) for k in range(16)], reads=[sq, ones_b], out_ap=psq.ap[:, 0:w])
                TS(st1.ap[:, 0:w], psm.ap[:, 0:w], 1.0 / D, ALU.mult, [psm], [st1])
                TT(st2.ap[:, 0:w], st1.ap[:, 0:w], st1.ap[:, 0:w], ALU.mult, [st1], [st2])
                STT(st2.ap[:, 0:w], psq.ap[:, 0:w], 1.0 / D, st2.ap[:, 0:w], ALU.mult, ALU.subtract, [psq, st2], [st2])
                ACT(st2.ap[:, 0:w], st2.ap[:, 0:w], AF.Sqrt, [st2, vec], [st2], bias=V(VO["eps"]), scale=1.0)
                RECIP(st2.ap[:, 0:w], st2.ap[:, 0:w], [st2], [st2])
                for f in range(16):
                    t_ = S.rot("rl")
                    TT(t_.ap[:, 0:w], yT.ap[:, f, c0:c0 + w], st1.ap[:, 0:w], ALU.subtract, [UP[f], st1], [t_])
                    TT(t_.ap[:, 0:w], t_.ap[:, 0:w], st2.ap[:, 0:w], ALU.mult, [t_, st2], [t_])
                    ACT(sT.ap[:, f, c0:c0 + w], t_.ap[:, 0:w], AF.Silu, [t_, vec], [MP[f]], bias=V(VO["ln_b"] + f), scale=V(VO["ln_g"] + f))
            S.barrier()
            for s_ in range(8):
                slot, sv = W.next("p2%d" % s_)

                def ev_p2(mi, c0, w, ps, s_=s_):
                    d_ = s_ * 2 + mi
                    STT(hT.ap[:, d_, c0:c0 + w], ps.ap[:, 0:w], V(VO["pw2_b"] + d_), hT.ap[:, d_, c0:c0 + w], ALU.add, ALU.add, [ps, HP[d_], vec], [HP[d_]])
                proj_fm(slot, sv, 16, 2, sT.ap, MP, csubs, ev_p2)
            S.barrier()
            mlp(1, 128)
            S.barrier()
            aru = Arena(R_u, 8 * T * 4)
            oT = [aru.alloc([128, 16, 128], F32, "oT%d" % i) for i in range(2)]
            ob = [aru.alloc([128, 2048], F32, "ob%d" % i) for i in range(2)]
            for tt in range(1, ntt):
                o_ = oT[tt % 2]
                norm_fm(hT.ap, HP, tt * 128, 128, VO["g_fin"], o_.ap, [o_], 0)
                b_ = ob[tt % 2]
                for q4 in range(4):
                    ps = S.rot("misc")
                    TRANS(ps, [(j * 128, o_.ap[:, q4 * 4 + j, :], 128) for j in range(4)], [o_], f32=True)
                    if True:
                        ACT(b_.ap[:, q4 * 512:(q4 + 1) * 512], ps.ap[:, 0:512], AF.Copy, [ps], [b_])
                    else:
                        S.op("dve", lambda e, b_=b_, ps=ps, q4=q4: e.tensor_copy(out=b_.ap[:, q4 * 512:(q4 + 1) * 512], in_=ps.ap[:, 0:512]), [ps], [b_])
                r0 = out0 + (tt - 1) * 128
                S.dma("sp", out_d[r0:r0 + 128, :], b_.ap[:], reads=[b_], final=True)

        def mlp(l, c_lo):
            msubs = subs_from(c_lo)
            gofs = VO["g_mlp0"] if l == 0 else VO["g_mlp1"]
            for c0 in range(c_lo, T, 256):
                norm_fm(hT.ap, HP, c0, min(256, T - c0), gofs, uT.ap, UP, c0)
            S.barrier()
            hid = [view(R_m, 0, [128, 8, T], BF16, "hid0"), view(R_m, 8 * T * 2, [128, 8, T], BF16, "hid1")]
            for g in range(8):
                hb = hid[g % 2]
                for s4 in range(4):
                    slot, sv = W.next("w1_%d_%d_%d" % (l, g, s4))

                    def ev1(mi, c0, w, ps, s4=s4, hb=hb):
                        r_ = S.rot("rl")
                        ACT(r_.ap[:, 0:w], ps.ap[:, 0:w], AF.Relu, [ps], [r_])
                        TT(hb.ap[:, s4 * 2 + mi, c0:c0 + w], ps.ap[:, 0:w], r_.ap[:, 0:w], ALU.mult, [ps, r_], [hb])
                    proj_fm(slot, sv, 16, 2, uT.ap, UP, msubs, ev1)
                for s4 in range(4):
                    slot, sv = W.next("w2_%d_%d_%d" % (l, g, s4))

                    def ev2(mi, c0, w, ps, s4=s4):
                        d_ = s4 * 4 + mi
                        TT(hT.ap[:, d_, c0:c0 + w], ps.ap[:, 0:w], hT.ap[:, d_, c0:c0 + w], ALU.add, [ps, HP[d_]], [HP[d_]])
                    proj_fm(slot, sv, 8, 4, hb.ap, [hb], msubs, ev2)

        plan_tile(False, False)
        plan_tile(False, True)
        plan_tile(True, True)
        if dbg != 2:
            plan_tile(True, True)
        tile(0, 6, False, False, 5, 5, set(range(6)), False, None)
        tile(768, 9, False, True, 8, 8, set(range(9)), False, None)
        tile(1920, 9, True, True, 7, 7, {0}, True, 0, dbg_i=0)
        if dbg != 2:
            tile(2944, 9, True, True, None, 7, set(), False, 1024, dbg_i=1)
        S.barrier(("sp",))
        S.finish()
        assert W.taken == len(W.plan), (W.taken, len(W.plan))
    return nc


def _host_consts():
    cm = np.zeros((128, 4, 128), np.float32)
    cm[:, 0, :] = np.eye(128, dtype=np.float32)
    cm[:, 1, :] = 1.0
    u = np.triu(np.ones((128, 128), np.float32))
    cm[:, 2, :] = u
    cm[:, 3, :] = u * (1.0 / 16.0)
    return cm


def _bias2(rel_bias):
    qi = np.arange(128)
    kj = np.arange(640)
    qc = qi // 64
    kc = kj // 64
    qpos = qi
    kpos = (kc - 8) * 64 + (kj % 64)
    dist = np.clip(qpos[:, None] - kpos[None, :], -256, 256) + 256
    vis = (kc[None, :] >= qc[:, None]) & (kc[None, :] <= qc[:, None] + 8)
    out = np.empty((8, 128, 640), np.float32)
    for h in range(8):
        out[h] = np.where(vis, rel_bias[h][dist], np.float32(NEG))
    return out


_NC_CACHE = {}


def kernel(**inputs):
    dbg = inputs.pop("_dbg", False)
    x = np.asarray(inputs["x"], np.float32)
    f = lambda n: np.asarray(inputs[n], np.float32)
    vecs = np.zeros((128, NV), np.float32)

    def put(name, arr):
        arr = np.asarray(arr, np.float32)
        vecs[:, VO[name]:VO[name] + arr.shape[1]] = arr

    put("g_mix0", _fm(f("mixer_norm_g")[0]))
    put("g_mix1", _fm(f("mixer_norm_g")[1]))
    put("g_mlp0", _fm(f("mlp_norm_g")[0]))
    put("g_mlp1", _fm(f("mlp_norm_g")[1]))
    put("g_fin", _fm(f("final_norm_g")))
    put("qk_cb", _fm(f("qk_conv_b")[0]))
    cw = f("qk_conv_w")[0]
    put("qk_cw", np.ascontiguousarray(cw.T.reshape(16, 128, 4).transpose(1, 0, 2)).reshape(128, 64))
    put("ml_g", _fm(f("mlstm_norm_g")[0]))
    put("pw1_b", _fm(f("conv_pw1_b")[0]))
    dw = f("conv_dw_w")[0]
    put("dw_w", np.ascontiguousarray(dw.T.reshape(16, 128, 31).transpose(1, 0, 2)).reshape(128, 496))
    put("dw_b", _fm(f("conv_dw_b")[0]))
    put("ln_g", _fm(f("conv_ln_g")[0]))
    put("ln_b", _fm(f("conv_ln_b")[0]))
    put("pw2_b", _fm(f("conv_pw2_b")[0]))
    gb = np.concatenate([f("igate_b")[0], f("fgate_b")[0]])[None, :]
    put("gate_b", np.broadcast_to(gb, (128, 8)))
    vecs[:, VO["eps"]] = EPS
    vecs[:, VO["one"]] = 1.0
    vecs[:, VO["nl16"]] = -np.log(16.0)
    vecs[:, VO["zero"]] = 0.0
    cm = _host_consts()
    b2 = _bias2(f("rel_bias")[0])
    w_in = np.ascontiguousarray(f("mix_w_in")[0])
    w_out = np.ascontiguousarray(f("mix_w_out")[0])
    pw1 = np.ascontiguousarray(f("conv_pw1_w")[0])
    pw2 = np.ascontiguousarray(f("conv_pw2_w")[0])
    w1 = np.ascontiguousarray(f("mlp_w1"))
    w2 = np.ascontiguousarray(f("mlp_w2"))
    in_maps = []
    for c in range(8):
        b, half = c // 2, c % 2
        xe = np.zeros((VS, D), np.float32)
        if half == 0:
            xe[2048:] = x[b, :2048]
        else:
            xe[:] = x[b]
        v = vecs.copy()
        v[:, VO["flag"]] = float(half)
        v[:, VO["maskc"]] = 0.0 if half == 1 else NEG
        in_maps.append({"x_ext": xe, "vecs": v, "cmat": cm, "bias2": b2, "w_in": w_in, "w_out": w_out,
                        "pw1": pw1, "pw2": pw2, "w1": w1, "w2": w2})
    key = dbg
    if key not in _NC_CACHE:
        _NC_CACHE[key] = build_program(dbg)
    nc = _NC_CACHE[key]
    res = run_bass_kernel_spmd(nc, in_maps, core_ids=list(range(8)))
    out = np.empty((4, 4096, D), np.float32)
    for c in range(8):
        b, half = c // 2, c % 2
        out[b, half * 2048:(half + 1) * 2048] = res.results[c]["out"]
    if dbg:
        return out, [res.results[c]["dbg"] for c in range(8)]
    return out
```
